# Optimizing a Trainium2 kernel written in Bass

```python
import math
import jax, jax.numpy as jnp
from jax import lax
import numpy as np

D_MODEL = 1024
BATCH = 8
SEQ = 4096
DEPTH = 2

NORM_EPS = 1e-6
GLA_HEADS = 4
GLA_DV = D_MODEL // 8
GLA_DK = GLA_DV // 2
GLA_LOWRANK = 16
GLA_TAU = 16.0
GLA_CHUNK = 64
NSA_HEADS = 8
NSA_KV_GROUPS = 2
NSA_HEAD_DIM = D_MODEL // 16
CMP_BLOCK = 32
CMP_STRIDE = 16
CMP_HIDDEN = 4 * NSA_HEAD_DIM
SEL_BLOCK = 64
N_SELECT = 8
WINDOW = 512
Q_BLOCK = 128
D_INNER = 2 * D_MODEL
SSM_HEAD_DIM = 64
SSM_HEADS = D_INNER // SSM_HEAD_DIM
SSM_GROUPS = 4
SSM_STATE = 128
CONV_K = 4
SSM_CHUNK = 64
CONV_CH = D_INNER + 2 * SSM_GROUPS * SSM_STATE
FFN_DENSE = ((8 * D_MODEL // 3 + 127) // 128) * 128
N_EXPERTS = 8
TOP_K = 2
FFN_EXPERT = 7 * D_MODEL // 2
N_EVEN = (DEPTH + 1) // 2
N_ODD = DEPTH // 2
HYB_SPLITS = (GLA_HEADS * GLA_DK, GLA_HEADS * GLA_DK, GLA_HEADS * GLA_DV, GLA_LOWRANK, GLA_HEADS * GLA_DV,
              NSA_HEADS * NSA_HEAD_DIM, 6 * NSA_KV_GROUPS * NSA_HEAD_DIM, 3 * NSA_HEADS)
HYB_IN = sum(HYB_SPLITS)
HYB_MIX = GLA_HEADS * GLA_DV + NSA_HEADS * NSA_HEAD_DIM
SSM_SPLITS = (D_INNER, CONV_CH, SSM_HEADS)
SSM_IN = sum(SSM_SPLITS)

kernel_name = 'hybrid_gla_nsa_ssd_moe_trunk'


def rmsnorm(x, w):
    xf = x.astype(jnp.float32)
    y = xf * lax.rsqrt(jnp.mean(xf * xf, axis=-1, keepdims=True) + NORM_EPS)
    return (y * w.astype(jnp.float32)).astype(x.dtype)


def split_cols(a, sizes):
    return jnp.split(a, [int(s) for s in np.cumsum(sizes)[:-1]], axis=-1)


def adaln(c, w, b):
    mod = jax.nn.silu(c) @ w + b
    shift, scale, gate = jnp.split(mod[:, None, :], 3, axis=-1)
    return shift, scale, gate


def modulate(x, norm_w, shift, scale):
    return rmsnorm(x, norm_w) * (1 + scale) + shift


def masked_softmax(s, mask):
    s = jnp.where(mask, s.astype(jnp.float32), -jnp.inf)
    m = jnp.max(s, axis=-1, keepdims=True)
    m = jnp.where(jnp.isfinite(m), m, 0.0)
    p = jnp.where(mask, jnp.exp(s - m), 0.0)
    return p / jnp.maximum(jnp.sum(p, axis=-1, keepdims=True), 1e-20)


def alibi_slopes(n):
    return 2.0 ** (-8.0 * jnp.arange(1, n + 1, dtype=jnp.float32) / n)


def swiglu(h, w_gu, w_down):
    g, u = jnp.split(h @ w_gu, 2, axis=-1)
    return (jax.nn.silu(g) * u) @ w_down


def gla_mixer(q, k, v, lowrank, r, gk_up, gk_bias, out_norm):
    B, T, _ = q.shape
    H, DK, DV, C = GLA_HEADS, GLA_DK, GLA_DV, GLA_CHUNK
    NC = T // C

    def chunks(a, d):
        return a.astype(jnp.float32).reshape(B, NC, C, H, d).transpose(0, 3, 1, 2, 4)

    log_alpha = jax.nn.log_sigmoid((lowrank @ gk_up + gk_bias).astype(jnp.float32)) / GLA_TAU
    b = jnp.cumsum(chunks(log_alpha, DK), axis=3)
    qc = chunks(q, DK) * DK ** -0.5
    kc = chunks(k, DK)
    vc = chunks(v, DV)
    q_dec = qc * jnp.exp(b)
    k_dec = kc * jnp.exp(-b)
    causal = jnp.tril(jnp.ones((C, C), dtype=bool))
    att = jnp.where(causal, jnp.einsum('bhncd,bhnsd->bhncs', q_dec, k_dec), 0.0)
    o_intra = jnp.einsum('bhncs,bhnsv->bhncv', att, vc)
    b_end = b[:, :, :, -1]
    k_to_end = kc * jnp.exp(b_end[:, :, :, None] - b)

    def step(S, inp):
        qd, kd, vv, decay = inp
        o = jnp.einsum('bhcd,bhdv->bhcv', qd, S)
        S = jnp.exp(decay)[..., None] * S + jnp.einsum('bhcd,bhcv->bhdv', kd, vv)
        return S, o

    S0 = jnp.zeros((B, H, DK, DV), jnp.float32)
    xs = tuple(jnp.moveaxis(a, 2, 0) for a in (q_dec, k_to_end, vc, b_end))
    _, o_inter = lax.scan(step, S0, xs)
    o = o_intra + jnp.moveaxis(o_inter, 0, 2)
    o = o.transpose(0, 2, 3, 1, 4).reshape(B, T, H, DV)
    o = rmsnorm(o, out_norm) * jax.nn.silu(r.astype(jnp.float32).reshape(B, T, H, DV))
    return o.reshape(B, T, H * DV)


def nsa_mixer(q, kv, gate_logits, cmp_pe, cmp_w1, cmp_w2):
    B, T, _ = q.shape
    G, Dh = NSA_KV_GROUPS, NSA_HEAD_DIM
    R = NSA_HEADS // G
    qh = q.astype(jnp.float32).reshape(B, T, G, R, Dh).transpose(0, 2, 3, 1, 4) * Dh ** -0.5
    kvh = kv.astype(jnp.float32).reshape(B, T, 6, G, Dh).transpose(2, 0, 3, 1, 4)
    k_cmp, v_cmp, k_slc, v_slc, k_win, v_win = (kvh[j] for j in range(6))
    gates = jax.nn.sigmoid(gate_logits.astype(jnp.float32)).reshape(B, T, G, R, 3).transpose(0, 2, 3, 1, 4)

    n_cmp = (T - CMP_BLOCK) // CMP_STRIDE + 1
    blk_idx = np.arange(n_cmp)[:, None] * CMP_STRIDE + np.arange(CMP_BLOCK)[None, :]

    def compress(a, j):
        blocks = a[:, :, blk_idx] + cmp_pe[j]
        h = jax.nn.gelu(blocks.reshape(B, G, n_cmp, CMP_BLOCK * Dh) @ cmp_w1[j])
        return h @ cmp_w2[j]

    kc = compress(k_cmp, 0)
    vc = compress(v_cmp, 1)
    cmp_end = jnp.asarray(blk_idx[:, -1], jnp.int32)
    cmp_center = jnp.asarray(blk_idx.mean(axis=-1), jnp.float32)

    n_sel = T // SEL_BLOCK
    n_top = min(N_SELECT, n_sel)
    sel_start_np = np.arange(n_sel) * SEL_BLOCK
    overlap = jnp.asarray((blk_idx[:, :1] < sel_start_np[None, :] + SEL_BLOCK)
                          & (blk_idx[:, -1:] >= sel_start_np[None, :]), jnp.float32)
    sel_start = jnp.asarray(sel_start_np, jnp.int32)
    kb = k_slc.reshape(B, G, n_sel, SEL_BLOCK, Dh)
    vb = v_slc.reshape(B, G, n_sel, SEL_BLOCK, Dh)
    bi = jnp.arange(B)[:, None, None, None]
    gi = jnp.arange(G)[None, :, None, None]

    k_wp = jnp.pad(k_win, ((0, 0), (0, 0), (WINDOW, 0), (0, 0)))
    v_wp = jnp.pad(v_win, ((0, 0), (0, 0), (WINDOW, 0), (0, 0)))
    slopes = alibi_slopes(NSA_HEADS).reshape(G, R, 1, 1)

    def block_fn(i):
        q0 = i * Q_BLOCK
        qb = lax.dynamic_slice_in_dim(qh, q0, Q_BLOCK, axis=3)
        gb = lax.dynamic_slice_in_dim(gates, q0, Q_BLOCK, axis=3)
        t = q0 + jnp.arange(Q_BLOCK, dtype=jnp.int32)
        tf = t.astype(jnp.float32)
        s = jnp.einsum('bgrqd,bgkd->bgrqk', qb, kc) - slopes * (tf[:, None] - cmp_center[None, :])
        p_cmp = masked_softmax(s, cmp_end[None, :] <= t[:, None])
        o_cmp = jnp.einsum('bgrqk,bgkd->bgrqd', p_cmp, vc)
        imp = jnp.einsum('bgqk,ks->bgqs', jnp.sum(p_cmp, axis=2), overlap)
        blk_t = t // SEL_BLOCK
        ids = jnp.arange(n_sel)[None, :]
        forced = (ids == 0) | (ids == blk_t[:, None]) | (ids == blk_t[:, None] - 1)
        future = sel_start[None, :] > t[:, None]
        imp = jnp.where(forced, jnp.inf, jnp.where(future, -jnp.inf, imp))
        _, idx = lax.top_k(imp, n_top)
        ks_ = kb[bi, gi, idx].reshape(B, G, Q_BLOCK, n_top * SEL_BLOCK, Dh)
        vs_ = vb[bi, gi, idx].reshape(B, G, Q_BLOCK, n_top * SEL_BLOCK, Dh)
        pos = (idx[..., None] * SEL_BLOCK + jnp.arange(SEL_BLOCK)).reshape(B, G, Q_BLOCK, n_top * SEL_BLOCK)
        dist = (t[None, None, :, None] - pos)[:, :, None]
        s = jnp.einsum('bgrqd,bgqkd->bgrqk', qb, ks_) - slopes * dist.astype(jnp.float32)
        p_slc = masked_softmax(s, dist >= 0)
        o_slc = jnp.einsum('bgrqk,bgqkd->bgrqd', p_slc, vs_)
        kw = lax.dynamic_slice_in_dim(k_wp, q0, WINDOW + Q_BLOCK, axis=2)
        vw = lax.dynamic_slice_in_dim(v_wp, q0, WINDOW + Q_BLOCK, axis=2)
        s_pos = q0 - WINDOW + jnp.arange(WINDOW + Q_BLOCK, dtype=jnp.int32)
        d = t[:, None] - s_pos[None, :]
        s = jnp.einsum('bgrqd,bgkd->bgrqk', qb, kw) - slopes * d.astype(jnp.float32)
        p_win = masked_softmax(s, (d >= 0) & (d < WINDOW) & (s_pos[None, :] >= 0))
        o_win = jnp.einsum('bgrqk,bgkd->bgrqd', p_win, vw)
        o = gb[..., 0:1] * o_cmp + gb[..., 1:2] * o_slc + gb[..., 2:3] * o_win
        return o.transpose(0, 3, 1, 2, 4).reshape(B, Q_BLOCK, NSA_HEADS * Dh)

    out = lax.map(block_fn, jnp.arange(T // Q_BLOCK))
    return out.transpose(1, 0, 2, 3).reshape(B, T, NSA_HEADS * Dh)


def hybrid_mixer(h, w_in, gk_up, gk_bias, gla_norm, cmp_pe, cmp_w1, cmp_w2, w_out):
    q_a, k_a, v_a, lr_a, r_a, q_b, kv_b, g_b = split_cols(h @ w_in, HYB_SPLITS)
    o_a = gla_mixer(q_a, k_a, v_a, lr_a, r_a, gk_up, gk_bias, gla_norm)
    o_b = nsa_mixer(q_b, kv_b, g_b, cmp_pe, cmp_w1, cmp_w2)
    return jnp.concatenate([o_a, o_b], axis=-1).astype(h.dtype) @ w_out


def mamba2_mixer(h, w_in, conv_w, conv_b, dt_bias, a_log, d_skip, norm_w, w_out):
    B, T, _ = h.shape
    H, P, G, N, Q = SSM_HEADS, SSM_HEAD_DIM, SSM_GROUPS, SSM_STATE, SSM_CHUNK
    R = H // G
    NC = T // Q
    z, xbc, dt = split_cols(h @ w_in, SSM_SPLITS)
    xbc = lax.conv_general_dilated(xbc, conv_w[:, None, :].astype(xbc.dtype), window_strides=(1,),
                                   padding=((CONV_K - 1, 0),), dimension_numbers=('NWC', 'WIO', 'NWC'),
                                   feature_group_count=CONV_CH) + conv_b
    xbc = jax.nn.silu(xbc.astype(jnp.float32))
    xs_, Bm, Cm = split_cols(xbc, (D_INNER, G * N, G * N))
    dt = jax.nn.softplus(dt.astype(jnp.float32) + dt_bias)
    A = -jnp.exp(a_log.astype(jnp.float32))
    x = xs_.reshape(B, T, H, P)
    xdt = (x * dt[..., None]).reshape(B, NC, Q, G, R, P)
    Bc = Bm.reshape(B, NC, Q, G, N)
    Cc = Cm.reshape(B, NC, Q, G, N)
    a = (dt * A).reshape(B, NC, Q, G, R).transpose(0, 3, 4, 1, 2)
    cum = jnp.cumsum(a, axis=-1)
    seg = cum[..., :, None] - cum[..., None, :]
    causal = jnp.tril(jnp.ones((Q, Q), dtype=bool))
    L = jnp.exp(jnp.where(causal, seg, -jnp.inf))
    cb = jnp.einsum('bclgn,bcsgn->bgcls', Cc, Bc)
    y_diag = jnp.einsum('bgcls,bgrcls,bcsgrp->bclgrp', cb, L, xdt)
    decay_to_end = jnp.exp(cum[..., -1:] - cum)
    decay_from_start = jnp.exp(cum)
    chunk_decay = jnp.exp(cum[..., -1])

    def step(S, inp):
        Bn, Cn, xn, dte, dfs, cd = inp
        y = jnp.einsum('blgn,bgrpn,bgrl->blgrp', Cn, S, dfs)
        S = cd[..., None, None] * S + jnp.einsum('bsgn,bgrs,bsgrp->bgrpn', Bn, dte, xn)
        return S, y

    S0 = jnp.zeros((B, G, R, P, N), jnp.float32)
    xs = (jnp.moveaxis(Bc, 1, 0), jnp.moveaxis(Cc, 1, 0), jnp.moveaxis(xdt, 1, 0),
          jnp.moveaxis(decay_to_end, 3, 0), jnp.moveaxis(decay_from_start, 3, 0), jnp.moveaxis(chunk_decay, 3, 0))
    _, y_off = lax.scan(step, S0, xs)
    y = (y_diag + jnp.moveaxis(y_off, 0, 1)).reshape(B, T, H, P) + d_skip[:, None] * x
    y = y.reshape(B, T, D_INNER) * jax.nn.silu(z.astype(jnp.float32))
    y = rmsnorm(y.reshape(B, T, G, D_INNER // G), norm_w.reshape(G, D_INNER // G)).reshape(B, T, D_INNER)
    return y.astype(h.dtype) @ w_out


def moe_ffn(h, router, w_gu, w_down):
    logits = (h @ router).astype(jnp.float32)
    top_val, top_idx = lax.top_k(logits, TOP_K)
    weights = jax.nn.softmax(top_val, axis=-1)
    gate = jnp.sum(jax.nn.one_hot(top_idx, N_EXPERTS, dtype=jnp.float32) * weights[..., None], axis=-2)
    out = jnp.zeros(h.shape, jnp.float32)
    for e in range(N_EXPERTS):
        out = out + gate[..., e:e + 1] * swiglu(h, w_gu[e], w_down[e])
    return out


def setup_inputs(seed: int = 0) -> dict:
    key = jax.random.key(seed)
    ks = jax.random.split(key, 40)
    f32 = jnp.float32

    def nrm(i, shape, scale):
        return scale * jax.random.normal(ks[i], shape, f32)

    def gain(i, shape):
        return 1.0 + 0.1 * jax.random.normal(ks[i], shape, f32)

    dt = jnp.exp(jax.random.uniform(ks[30], (N_ODD, SSM_HEADS), f32,
                                    math.log(0.001), math.log(0.1)))
    return {
        'x': nrm(0, (BATCH, SEQ, D_MODEL), 1.0),
        'c': nrm(1, (BATCH, D_MODEL), 1.0),
        'hyb_norm': gain(2, (N_EVEN, D_MODEL)),
        'hyb_mod_w': nrm(3, (N_EVEN, D_MODEL, 3 * D_MODEL), 0.5 * D_MODEL ** -0.5),
        'hyb_mod_b': nrm(4, (N_EVEN, 3 * D_MODEL), 0.02),
        'hyb_w_in': nrm(5, (N_EVEN, D_MODEL, HYB_IN), D_MODEL ** -0.5),
        'gla_gk_up': nrm(6, (N_EVEN, GLA_LOWRANK, GLA_HEADS * GLA_DK), GLA_LOWRANK ** -0.5),
        'gla_gk_bias': nrm(7, (N_EVEN, GLA_HEADS * GLA_DK), 0.1),
        'gla_out_norm': gain(8, (N_EVEN, GLA_DV)),
        'nsa_cmp_pe': nrm(9, (N_EVEN, 2, CMP_BLOCK, NSA_HEAD_DIM), 0.1),
        'nsa_cmp_w1': nrm(10, (N_EVEN, 2, CMP_BLOCK * NSA_HEAD_DIM, CMP_HIDDEN), (CMP_BLOCK * NSA_HEAD_DIM) ** -0.5),
        'nsa_cmp_w2': nrm(11, (N_EVEN, 2, CMP_HIDDEN, NSA_HEAD_DIM), CMP_HIDDEN ** -0.5),
        'hyb_w_out': nrm(12, (N_EVEN, HYB_MIX, D_MODEL), HYB_MIX ** -0.5),
        'dense_norm': gain(13, (N_EVEN, D_MODEL)),
        'dense_mod_w': nrm(14, (N_EVEN, D_MODEL, 3 * D_MODEL), 0.5 * D_MODEL ** -0.5),
        'dense_mod_b': nrm(15, (N_EVEN, 3 * D_MODEL), 0.02),
        'dense_w_gu': nrm(16, (N_EVEN, D_MODEL, 2 * FFN_DENSE), D_MODEL ** -0.5),
        'dense_w_down': nrm(17, (N_EVEN, FFN_DENSE, D_MODEL), FFN_DENSE ** -0.5),
        'ssm_norm': gain(18, (N_ODD, D_MODEL)),
        'ssm_mod_w': nrm(19, (N_ODD, D_MODEL, 3 * D_MODEL), 0.5 * D_MODEL ** -0.5),
        'ssm_mod_b': nrm(20, (N_ODD, 3 * D_MODEL), 0.02),
        'ssm_w_in': nrm(21, (N_ODD, D_MODEL, SSM_IN), D_MODEL ** -0.5),
        'ssm_conv_w': nrm(22, (N_ODD, CONV_K, CONV_CH), CONV_K ** -0.5),
        'ssm_conv_b': nrm(23, (N_ODD, CONV_CH), 0.02),
        'ssm_dt_bias': dt + jnp.log(-jnp.expm1(-dt)),
        'ssm_a_log': jnp.log(jax.random.uniform(ks[24], (N_ODD, SSM_HEADS), f32, 1.0, 16.0)),
        'ssm_d': gain(25, (N_ODD, SSM_HEADS)),
        'ssm_gate_norm': gain(26, (N_ODD, D_INNER)),
        'ssm_w_out': nrm(27, (N_ODD, D_INNER, D_MODEL), D_INNER ** -0.5),
        'moe_norm': gain(28, (N_ODD, D_MODEL)),
        'moe_mod_w': nrm(29, (N_ODD, D_MODEL, 3 * D_MODEL), 0.5 * D_MODEL ** -0.5),
        'moe_mod_b': nrm(31, (N_ODD, 3 * D_MODEL), 0.02),
        'moe_router': nrm(32, (N_ODD, D_MODEL, N_EXPERTS), D_MODEL ** -0.5),
        'moe_w_gu': nrm(33, (N_ODD, N_EXPERTS, D_MODEL, 2 * FFN_EXPERT), D_MODEL ** -0.5),
        'moe_w_down': nrm(34, (N_ODD, N_EXPERTS, FFN_EXPERT, D_MODEL), FFN_EXPERT ** -0.5),
        'final_norm': gain(35, (D_MODEL,)),
    }


def reference(x, c, hyb_norm, hyb_mod_w, hyb_mod_b, hyb_w_in, gla_gk_up, gla_gk_bias, gla_out_norm,
              nsa_cmp_pe, nsa_cmp_w1, nsa_cmp_w2, hyb_w_out, dense_norm, dense_mod_w, dense_mod_b,
              dense_w_gu, dense_w_down, ssm_norm, ssm_mod_w, ssm_mod_b, ssm_w_in, ssm_conv_w, ssm_conv_b,
              ssm_dt_bias, ssm_a_log, ssm_d, ssm_gate_norm, ssm_w_out, moe_norm, moe_mod_w, moe_mod_b,
              moe_router, moe_w_gu, moe_w_down, final_norm):
    for layer in range(DEPTH):
        i = layer // 2
        if layer % 2 == 0:
            shift, scale, gate = adaln(c, hyb_mod_w[i], hyb_mod_b[i])
            mix = hybrid_mixer(modulate(x, hyb_norm[i], shift, scale), hyb_w_in[i], gla_gk_up[i], gla_gk_bias[i],
                               gla_out_norm[i], nsa_cmp_pe[i], nsa_cmp_w1[i], nsa_cmp_w2[i], hyb_w_out[i])
            x = x + (gate * mix).astype(x.dtype)
            shift, scale, gate = adaln(c, dense_mod_w[i], dense_mod_b[i])
            ffn = swiglu(modulate(x, dense_norm[i], shift, scale), dense_w_gu[i], dense_w_down[i])
            x = x + (gate * ffn).astype(x.dtype)
        else:
            shift, scale, gate = adaln(c, ssm_mod_w[i], ssm_mod_b[i])
            mix = mamba2_mixer(modulate(x, ssm_norm[i], shift, scale), ssm_w_in[i], ssm_conv_w[i], ssm_conv_b[i],
                               ssm_dt_bias[i], ssm_a_log[i], ssm_d[i], ssm_gate_norm[i], ssm_w_out[i])
            x = x + (gate * mix).astype(x.dtype)
            shift, scale, gate = adaln(c, moe_mod_w[i], moe_mod_b[i])
            ffn = moe_ffn(modulate(x, moe_norm[i], shift, scale), moe_router[i], moe_w_gu[i], moe_w_down[i])
            x = x + (gate * ffn).astype(x.dtype)
    return rmsnorm(x, final_norm)
```

```python
import numpy as np
import ml_dtypes
import concourse.bass as bass
import concourse.mybir as mybir
from concourse.bass_utils import run_bass_kernel_spmd
from contextlib import ExitStack

F32 = mybir.dt.float32
BF16 = mybir.dt.bfloat16
ALU = mybir.AluOpType
AF = mybir.ActivationFunctionType
AX = mybir.AxisListType

T = 4096
D = 1024
NT = 8
TT = 512
EPS = 1e-6


class Buf:
    __slots__ = ("name", "w", "r", "dsem", "dcnt", "excl", "rg")

    def __init__(self, name):
        self.name = name
        self.rg = None
        self.excl = False
        self.w = None
        self.r = {}
        self.dsem = None
        self.dcnt = 0


class Tl:
    __slots__ = ("t", "b")

    def __init__(self, t, name):
        self.t = t
        self.b = Buf(name)

    def __getitem__(self, k):
        return self.t[k]


class Prog:
    CENG = ["pe", "act", "dve", "pool"]
    ALLQ = ["pe", "act", "dve", "pool", "sp"]

    def __init__(self, nc, es):
        self.nc = nc
        self.es = es
        self.q = {e: [] for e in self.ALLQ}
        self.cnt = {e: 0 for e in self.CENG}
        self.sems = {}
        for e in self.CENG:
            self.sems["E" + e] = es.enter_context(nc.semaphore("sem_" + e))
        self.known = {e: {} for e in self.ALLQ}
        self.ndsem = 0
        self.totals = {}
        self.free_dsems = []
        self.dma_bufs = []

    def _collect(self, eng, reads, writes, rg=None):
        deps = {}
        force = set()
        if eng == "pe":
            for b in writes:
                if b.w is not None and b.w[2] == "pe" and b.rg != rg:
                    force.add(b.w[0])

        def add(key, val, src):
            if deps.get(key, (0,))[0] < val:
                deps[key] = (val, src)
        for b in reads:
            if b.w is not None:
                add(*b.w)
            if b.excl:
                for key, (val, src) in b.r.items():
                    if src != eng:
                        add(key, val, src)
        for b in writes:
            if b.w is not None:
                add(*b.w)
            for key, (val, src) in b.r.items():
                if src == eng and eng != "pool":
                    continue
                add(key, val, src)
        waits = []
        kn = self.known[eng]
        for key, (val, src) in deps.items():
            if src == eng and eng == "pe" and key not in force:
                continue
            if kn.get(key, 0) >= val:
                continue
            kn[key] = val
            waits.append((key, val))
        return waits

    def op(self, eng, fn, reads=(), writes=(), rg=None):
        reads = [x.b if isinstance(x, Tl) else x for x in reads]
        writes = [x.b if isinstance(x, Tl) else x for x in writes]
        waits = self._collect(eng, reads, writes, rg)
        if eng == "pe":
            for b in writes:
                b.rg = rg
        self.cnt[eng] += 1
        key = "E" + eng
        n = self.cnt[eng]
        self.totals[key] = n
        self.q[eng].append((waits, fn, (key, 1)))
        for b in reads:
            b.r[key] = (n, eng)
        for b in writes:
            b.w = (key, n, eng)
            b.r = {}

    def _dsem(self, b):
        if b.dsem is None:
            if self.free_dsems:
                b.dsem = self.free_dsems.pop()
                b.dcnt = self.totals.get(b.dsem, 0)
            else:
                b.dsem = "D%d" % self.ndsem
                self.sems[b.dsem] = self.es.enter_context(self.nc.semaphore("dsem%d" % self.ndsem))
                self.ndsem += 1
                b.dcnt = 0
            self.dma_bufs.append(b)
        return b.dsem

    def load(self, fn, dst, q="sp"):
        dst = dst.b if isinstance(dst, Tl) else dst
        waits = self._collect(q, (), (dst,))
        key = self._dsem(dst)
        dst.dcnt += 16
        self.totals[key] = dst.dcnt
        self.q[q].append((waits, fn, (key, 16)))
        dst.w = (key, dst.dcnt, "dma")
        dst.r = {}

    def store(self, fn, src, q="sp"):
        src = src.b if isinstance(src, Tl) else src
        waits = self._collect(q, (src,), ())
        key = self._dsem(src)
        src.dcnt += 16
        self.totals[key] = src.dcnt
        self.q[q].append((waits, fn, (key, 16)))
        src.r[key] = (src.dcnt, "dma")

    def barrier(self):
        for e in self.ALLQ:
            waits = []
            kn = self.known[e]
            for key, tot in self.totals.items():
                if kn.get(key, 0) >= tot:
                    continue
                kn[key] = tot
                waits.append((key, tot))
            if waits:
                self.q[e].append((waits, None, None))
        for b in self.dma_bufs:
            self.free_dsems.append(b.dsem)
            b.dsem = None
        self.dma_bufs = []

    def replay(self):
        nc = self.nc
        sems = self.sems
        qs = self.q

        def run(name, e):
            for waits, fn, inc in qs[name]:
                for key, val in waits:
                    e.wait_ge(sems[key], val)
                if fn is not None:
                    ins = fn(e)
                    ins.then_inc(sems[inc[0]], inc[1])

        with nc.Block() as block:
            @block.tensor
            def _(e):
                run("pe", e)

            @block.scalar
            def _(e):
                run("act", e)

            @block.vector
            def _(e):
                run("dve", e)

            @block.gpsimd
            def _(e):
                run("pool", e)

            @block.sync
            def _(e):
                run("sp", e)


class KB:
    def __init__(self, nc, P):
        self.nc = nc
        self.P = P
        self.rr = 0

    def sb(self, es, name, shape, dt):
        t = es.enter_context(self.nc.sbuf_tensor(name, list(shape), dt))
        return Tl(t, name)

    def mm(self, out, lhsT, rhs, start=True, stop=True, r=(), w=(), rg=0):
        self.P.op("pe", lambda e: e.matmul(out, lhsT, rhs, start=start, stop=stop), r, w, rg=rg)

    def tr(self, out, in_, ident, r=(), w=()):
        self.P.op("pe", lambda e: e.transpose(out, in_, ident), r, w)

    def act(self, out, in_, func, bias=None, scale=None, accum=None, r=(), w=()):
        kw = {}
        if bias is not None:
            kw["bias"] = bias
        if scale is not None:
            kw["scale"] = scale
        if accum is not None:
            kw["accum_out"] = accum
        self.P.op("act", lambda e: e.activation(out, in_, func, **kw), r, w)

    def cp(self, eng, out, in_, r=(), w=()):
        if eng == "act":
            self.P.op("act", lambda e: e.copy(out, in_), r, w)
        else:
            self.P.op(eng, lambda e: e.tensor_copy(out, in_), r, w)

    def tt(self, eng, out, a, b, op, r=(), w=()):
        self.P.op(eng, lambda e: e.tensor_tensor(out, a, b, op), r, w)

    def ts(self, eng, out, a, s1, s2, op0, op1=None, r=(), w=()):
        if op1 is None:
            self.P.op(eng, lambda e: e.tensor_scalar(out, a, s1, None, op0), r, w)
        else:
            self.P.op(eng, lambda e: e.tensor_scalar(out, a, s1, s2, op0, op1), r, w)

    def stt(self, eng, out, a, s, b, op0, op1, r=(), w=()):
        self.P.op(eng, lambda e: e.scalar_tensor_tensor(out, a, s, b, op0, op1), r, w)

    def memset(self, eng, out, v, w=()):
        self.P.op(eng, lambda e: e.memset(out, v), (), w)

    def recip(self, out, in_, r=(), w=()):
        self.P.op("dve", lambda e: e.reciprocal(out, in_), r, w)

    def ld(self, out, in_, dst, q="sp"):
        self.P.load(lambda e: e.dma_start(out=out, in_=in_), dst, q)

    def st(self, out, in_, src, q="sp"):
        self.P.store(lambda e: e.dma_start(out=out, in_=in_), src, q)

    def alt(self):
        self.rr += 1
        return ("dve", "act")[self.rr % 2]


def make_consts():
    c = {}
    c["ident_f"] = np.eye(128, dtype=np.float32)
    c["ident_b"] = np.eye(128, dtype=np.float32).astype(ml_dtypes.bfloat16)
    c["ones_b"] = np.ones((128, 128), np.float32).astype(ml_dtypes.bfloat16)
    rm = np.ones((128, TT), np.float32)
    rm[:, ::64] = 0.0
    c["resetm"] = rm
    sp_, s_ = np.meshgrid(np.arange(64), np.arange(64), indexing="ij")
    c["u64"] = (sp_ > s_).astype(np.float32)
    c["tri64"] = (sp_ <= s_).astype(np.float32)
    m = (sp_ <= s_).astype(np.float32)
    c["cmask64"] = np.tile(m, (1, 8)).astype(np.float32)
    bf = ml_dtypes.bfloat16
    t = np.arange(T)
    slopes = 2.0 ** (-8.0 * np.arange(1, 9) / 8)
    qaug = np.zeros((8, 4, T), np.float32)
    for h in range(8):
        qaug[h, 0] = -slopes[h] * 64.0 * (t // 64)
        qaug[h, 1] = -slopes[h] * (t % 64)
        qaug[h, 2] = slopes[h]
        qaug[h, 3] = slopes[h]
    c["qaug"] = qaug.astype(bf)
    kaug = np.zeros((4, T), np.float32)
    kaug[0] = 1.0
    kaug[1] = 1.0
    kaug[2] = 64.0 * (t // 64)
    kaug[3] = t % 64
    c["kaug"] = kaug.astype(bf)
    n = np.arange(256)
    kaugc = np.zeros((4, 256), np.float32)
    kaugc[0] = 1.0
    kaugc[1] = 1.0
    kaugc[2] = 16.0 * n
    kaugc[3] = 15.5
    kaugc[:, 255] = 0.0
    c["kaugc"] = kaugc.astype(bf)
    c["esel"] = (t[None, :] // 64 == np.arange(64)[:, None]).astype(np.float32).astype(bf)
    p = np.arange(128)[:, None]
    f = np.arange(512)[None, :]
    winm = np.zeros((8, 128, 512), np.float32)
    for i in range(8):
        off = -512 + 128 * i
        dd = f - p - off
        winm[i] = (((dd >= 0) & (dd < 512)).astype(np.float32) - 1.0) * 30000.0
    c["winm"] = np.ascontiguousarray(winm.transpose(1, 0, 2)).astype(bf)
    cm = np.zeros((8, 128, 2, 512), np.float32)
    for qt in range(8):
        for ck in range(2):
            nn = 128 * ck + p
            cm[qt, :, ck, :] = (((16 * nn + 31 <= 512 * qt + f) & (nn < 255)).astype(np.float32) - 1.0) * 30000.0
    c["cmpm"] = cm.astype(bf)
    ovl = np.zeros((128, 2, 65), np.float32)
    for ck in range(2):
        nn = 128 * ck + np.arange(128)
        j = np.arange(64)
        o = ((16 * nn[:, None] < 64 * j[None, :] + 64) & (16 * nn[:, None] + 31 >= 64 * j[None, :])).astype(np.float32)
        o[nn >= 255] = 0.0
        ovl[:, ck, 0:64] = o
        ovl[:, ck, 64] = (nn < 255).astype(np.float32)
    c["ovl"] = ovl.astype(bf)
    M1 = np.zeros((128, 32, 64), np.float32)
    M2 = np.zeros((128, 32, 64), np.float32)
    for tile in range(32):
        tt_ = 128 * tile + np.arange(128)
        blk = tt_ // 64
        j = np.arange(64)[None, :]
        forced = (j == 0) | (j == blk[:, None]) | (j == blk[:, None] - 1)
        future = j > blk[:, None]
        M1[:, tile, :] = (~(forced | future)).astype(np.float32)
        M2[:, tile, :] = np.where(forced, 1e30 * (1 + j / 100.0), np.where(future, -1e30 * (1 + j / 100.0), 0.0))
    c["selm1"] = M1
    c["selm2"] = M2
    gs = np.zeros((24, 24 * 64), np.float32)
    for k in range(24):
        gs[k, k * 64:(k + 1) * 64] = 1.0
    c["gsel"] = gs
    return c


CONST_SPECS = None


class Ctx:
    pass


def build_program(phases, dbg_out=()):
    nc = bass.Bass("TRN2", target_bir_lowering=False)
    ext_in = {}
    scratch = {}

    def din(name, shape, dt=F32):
        ext_in[name] = (list(shape), dt)
        return nc.dram_tensor(name, list(shape), dt, kind="ExternalInput").ap()

    def dscr(name, shape, dt):
        kind = "ExternalOutput" if name in dbg_out else "Internal"
        if ("in:" + name) in dbg_out:
            kind = "ExternalInput"
            ext_in[name] = (list(shape), dt)
        t = nc.dram_tensor(name, list(shape), dt, kind=kind).ap()
        scratch[name] = t
        return t

    consts = make_consts()
    X = Ctx()
    X.dbg_stop = [d[5:] for d in dbg_out if d.startswith("stop:")]
    X.dbg_stop = X.dbg_stop[0] if X.dbg_stop else None
    X.nc = nc
    X.cd = {k: din("c_" + k, v.shape, BF16 if v.dtype == ml_dtypes.bfloat16 else F32) for k, v in consts.items()}

    X.xT = din("xT", [D, T])
    X.cvec = din("cvec", [128, 8])
    X.mod_w = [din("mod_w%d" % j, [D, 3 * D]) for j in range(4)]
    X.mod_b = [din("mod_b%d" % j, [128, 24]) for j in range(4)]
    X.norm_w = [din("norm_w%d" % j, [128, 8]) for j in range(4)]
    X.hyb_w_in = din("hyb_w_in", [D, 2856])
    X.gk_up = din("gk_up", [16, 256])
    X.gk_bias_col = din("gk_bias_col", [128, 2])
    X.gk_bias_rep = din("gk_bias_rep", [64, 256])
    X.gla_norm = din("gla_norm", [128, 1])
    X.hyb_w_out = din("hyb_w_out", [D, D])
    X.dense_w_gu = din("dense_w_gu", [D, 5632])
    X.dense_w_down = din("dense_w_down", [2816, D])
    X.ssm_w_in = din("ssm_w_in", [D, 5152])
    X.ssm_w_out = din("ssm_w_out", [2048, D])
    X.conv_w_l = din("conv_w_l", [128, 24, 4])
    X.conv_b_l = din("conv_b_l", [128, 24])
    X.dt_bias_rep = din("dt_bias_rep", [128, 32])
    X.a_log_rep = din("a_log_rep", [64, 32])
    X.d_rep = din("d_rep", [64, 2048])
    X.gate_norm_rep = din("gate_norm_rep", [128, 2048])
    X.router = din("router", [D, 8])
    X.moe_w_gu = din("moe_w_gu", [8, D, 7168])
    X.moe_w_down = din("moe_w_down", [8, 3584, D])
    X.final_w_rep = din("final_w_rep", [128, D])
    X.out = nc.dram_tensor("out", [T, D], F32, kind="ExternalOutput").ap()
    X.cmp_peT = din("cmp_peT", [2, 64, 32])
    X.cmp_w1 = din("cmp_w1", [2, 2048, 256])
    X.cmp_w2 = din("cmp_w2", [2, 256, 64])

    X.gqT = dscr("gqT", [256, T], F32)
    X.gkT = dscr("gkT", [256, T], F32)
    X.grT = dscr("grT", [512, T], F32)
    X.glrT = dscr("glrT", [16, T], F32)
    X.gk_tok = dscr("gk_tok", [T, 256], F32)
    X.gv_tok = dscr("gv_tok", [T, 512], BF16)
    X.nqT = dscr("nqT", [512, T], BF16)
    X.nkT = dscr("nkT", [4, 128, T], BF16)
    X.nv_tok = dscr("nv_tok", [2, T, 128], BF16)
    X.ngT = dscr("ngT", [24, T], F32)
    X.mixT = dscr("mixT", [D, T], BF16)
    X.x1T = dscr("x1T", [D, T], F32)
    X.x2T = dscr("x2T", [D, T], F32)
    X.xbcT = dscr("xbcT", [3072, T], F32)
    X.z_tok = dscr("z_tok", [T, 2048], F32)
    X.dt_tok = dscr("dt_tok", [T, 32], F32)
    X.x_tok = dscr("x_tok", [T, 2048], F32)
    X.B_tok = dscr("B_tok", [T, 512], BF16)
    X.BT = dscr("BT", [512, T], BF16)
    X.CT = dscr("CT", [512, T], BF16)
    X.y_tok = dscr("y_tok", [T, 2048], F32)
    X.x3T = dscr("x3T", [D, T], F32)
    X.hmT = dscr("hmT", [D, T], BF16)

    es = ExitStack()
    with es:
        P = Prog(nc, es)
        kb = KB(nc, P)
        X.P, X.kb = P, kb
        X.ident_f = kb.sb(es, "ident_f", [128, 128], F32)
        X.ident_b = kb.sb(es, "ident_b", [128, 128], BF16)
        X.ones_b = kb.sb(es, "ones_b", [128, 128], BF16)
        X.epsc = kb.sb(es, "epsc", [128, 1], F32)
        X.modt = kb.sb(es, "modt", [128, 4, 24], F32)
        X.modA = kb.sb(es, "modA", [128, 4, 8], F32)
        X.pball = nc.alloc_psum_tensor("pball", [128, 8, 512], F32)
        X.pb = [Tl(X.pball[:, i, :], "pb%d" % i) for i in range(8)]
        for p_ in X.pb:
            p_.b.excl = True
        kb.ld(X.ident_f[:], X.cd["ident_f"], X.ident_f)
        kb.ld(X.ident_b[:], X.cd["ident_b"], X.ident_b)
        kb.ld(X.ones_b[:], X.cd["ones_b"], X.ones_b)
        kb.memset("dve", X.epsc[:], EPS, w=[X.epsc])

        if "adaln" in phases:
            phase_adaln(X)
            P.barrier()
            if "modt" in dbg_out:
                dm = nc.dram_tensor("modt_d", [128, 96], F32, kind="ExternalOutput").ap()
                kb.st(dm, X.modt[:, :, :].rearrange("p a b -> p (a b)"), X.modt)
                dm2 = nc.dram_tensor("modA_d", [128, 32], F32, kind="ExternalOutput").ap()
                kb.st(dm2, X.modA[:, :, :].rearrange("p a b -> p (a b)"), X.modA)
        if "l0proj" in phases:
            phase_l0proj(X)
            P.barrier()
        if "gla" in phases:
            phase_gla(X)
            P.barrier()
        if "nsa" in phases:
            phase_nsa(X)
            P.barrier()
        if "l0out" in phases:
            phase_l0out(X)
            P.barrier()
        if "dense" in phases:
            phase_dense(X)
            P.barrier()
        if "ssm_in" in phases:
            phase_ssm_in(X)
        if "ssm_conv" in phases:
            phase_ssm_conv(X)
        if "ssm_scan" in phases:
            phase_ssm_scan(X)
        if "ssm_out" in phases:
            phase_ssm_out(X)
        if "moe" in phases:
            phase_moe(X)
        P.barrier()
        P.replay()
    return nc, ext_in


def load_cast(X, es, name, dram2d, K, M, W16, col0=0, chunk=512):
    kb = X.kb
    KC = K // 128
    stg = [kb.sb(es, "%s_stg%d" % (name, i), [128, KC, chunk], F32) for i in range(2)]
    src = dram2d.rearrange("(k p) m -> p k m", p=128)
    i = 0
    for m0 in range(0, M, chunk):
        mw = min(chunk, M - m0)
        s = stg[i % 2]
        kb.ld(s[:, :, 0:mw], src[:, :, m0:m0 + mw], s)
        eng = ("dve", "act")[i % 2]
        kb.cp(eng, W16[:, :, col0 + m0:col0 + m0 + mw], s[:, :, 0:mw], r=[s], w=[W16])
        i += 1


def phase_adaln(X):
    kb, P, nc = X.kb, X.P, X.nc
    with ExitStack() as es:
        cv = kb.sb(es, "cv", [128, 8], F32)
        sc = kb.sb(es, "sc", [128, 8], F32)
        kb.ld(cv[:], X.cvec, cv)
        kb.act(sc[:], cv[:], AF.Silu, r=[cv], w=[sc])
        wh = [kb.sb(es, "modw%d" % i, [128, 4, 3072], F32) for i in range(2)]
        bt = kb.sb(es, "modbt", [128, 4, 24], F32)
        nw = kb.sb(es, "modnw", [128, 4, 8], F32)
        tmp = kb.sb(es, "modtmp", [128, 24], F32)
        for j in range(4):
            kb.ld(bt[:, j, :], X.mod_b[j], bt)
            kb.ld(nw[:, j, :], X.norm_w[j], nw)
        for j in range(4):
            src = X.mod_w[j].rearrange("(k p) m -> p k m", p=128)
            pss = []
            for hf in range(2):
                w = wh[hf]
                for k in range(4):
                    kb.ld(w[:, k, :], src[:, hf * 4 + k, :], w)
                ps = X.pb[(2 * j + hf) % 4]
                pss.append(ps)
                for m in range(24):
                    for k in range(4):
                        kb.mm(ps[:, m:m + 1], w[:, k, m * 128:(m + 1) * 128], sc[:, hf * 4 + k:hf * 4 + k + 1],
                              start=(k == 0), stop=(k == 3), r=[w, sc], w=[ps])
            kb.tt("dve", tmp[:], pss[0][:, 0:24], bt[:, j, :], ALU.add, r=[pss[0], bt], w=[tmp])
            kb.tt("dve", X.modt[:, j, :], pss[1][:, 0:24], tmp[:], ALU.add, r=[pss[1], tmp], w=[X.modt])
            kb.stt("dve", X.modA[:, j, :], X.modt[:, j, 8:16], 1.0, nw[:, j, :], ALU.add, ALU.mult,
                   r=[X.modt, nw], w=[X.modA])


def norm_modulate(X, xt, hT, j, sq, ps_ss, rstd, hF, W=TT, hT32=None):
    kb = X.kb
    for k in range(8):
        kb.act(sq[:, k, 0:W], xt[:, k, 0:W], AF.Square, r=[xt], w=[sq])
    for k in range(8):
        kb.mm(ps_ss[:, 0:W], X.ones_b[:, :], sq[:, k, 0:W], start=(k == 0), stop=(k == 7), r=[X.ones_b, sq], w=[ps_ss])
    kb.act(rstd[:, 0:W], ps_ss[:, 0:W], AF.Ln, bias=X.epsc[:, 0:1], scale=1.0 / D, r=[ps_ss, X.epsc], w=[rstd])
    kb.act(rstd[:, 0:W], rstd[:, 0:W], AF.Exp, scale=-0.5, r=[rstd], w=[rstd])
    for k in range(8):
        kb.tt("dve", hF[:, k, 0:W], xt[:, k, 0:W], rstd[:, 0:W], ALU.mult, r=[xt, rstd], w=[hF])
        kb.act(hT[:, k, 0:W], hF[:, k, 0:W], AF.Identity, bias=X.modt[:, j, k:k + 1], scale=X.modA[:, j, k:k + 1],
               r=[hF, X.modA, X.modt], w=[hT])
        if hT32 is not None:
            kb.act(hT32[:, k, 0:W], hF[:, k, 0:W], AF.Identity, bias=X.modt[:, j, k:k + 1], scale=X.modA[:, j, k:k + 1],
                   r=[hF, X.modA, X.modt], w=[hT32])


def phase_l0proj(X):
    kb, P, nc = X.kb, X.P, X.nc
    with ExitStack() as es:
        W16 = kb.sb(es, "w_in16", [128, 8, 2856], BF16)
        with ExitStack() as es2:
            load_cast(X, es2, "win", X.hyb_w_in, D, 2856, W16, chunk=476)
            P.barrier()
        xt = [kb.sb(es, "xt%d" % i, [128, 8, TT], F32) for i in range(2)]
        sq = kb.sb(es, "sq", [128, 8, TT], BF16)
        hF = kb.sb(es, "hF", [128, 8, TT], F32)
        hTs = [kb.sb(es, "hT%d" % i, [128, 8, TT], BF16) for i in range(2)]
        rstd = kb.sb(es, "rstd", [128, TT], F32)
        ofm = [kb.sb(es, "ofm%d" % i, [128, TT], F32) for i in range(4)]
        ofb = [kb.sb(es, "ofb%d" % i, [128, TT], BF16) for i in range(4)]
        otk = [kb.sb(es, "otk%d" % i, [128, 768], F32) for i in range(2)]
        otv = [kb.sb(es, "otv%d" % i, [128, 512], BF16) for i in range(2)]
        otn = [kb.sb(es, "otn%d" % i, [128, 256], BF16) for i in range(2)]
        xsrc = X.xT.rearrange("(k p) t -> p k t", p=128)
        fm = []
        for pc in range(2):
            fm.append((0 + pc * 128, 128, X.gqT[pc * 128:(pc + 1) * 128, :], F32, None))
        for pc in range(2):
            fm.append((256 + pc * 128, 128, X.gkT[pc * 128:(pc + 1) * 128, :], F32, None))
        fm.append((1024, 16, X.glrT[:, :], F32, None))
        for pc in range(4):
            fm.append((1040 + pc * 128, 128, X.grT[pc * 128:(pc + 1) * 128, :], F32, None))
        for pc in range(4):
            fm.append((1552 + pc * 128, 128, X.nqT[pc * 128:(pc + 1) * 128, :], BF16, 0.125))
        for jj, col in enumerate((2064, 2192, 2320, 2576)):
            fm.append((col, 128, X.nkT[jj], BF16, None))
        fm.append((2832, 24, X.ngT[:, :], F32, None))
        nps = 0
        dbgs = ""
        def prep(tt_):
            x = xt[tt_ % 2]
            for k in range(8):
                kb.ld(x[:, k, :], xsrc[:, k, tt_ * TT:(tt_ + 1) * TT], x)
            norm_modulate(X, x, hTs[tt_ % 2], 0, sq, X.pb[7], rstd, hF)

        prep(0)
        for tt_ in range(1 if "one" in dbgs else NT):
            t0 = tt_ * TT
            hT = hTs[tt_ % 2]
            if tt_ + 1 < NT:
                prep(tt_ + 1)
            for idx, (c0, ncol, dst, dt, scl) in enumerate(fm):
                ps = X.pb[nps % 6]
                nps += 1
                for k in range(8):
                    kb.mm(ps[0:ncol, :], W16[:, k, c0:c0 + ncol], hT[:, k, :], start=(k == 0), stop=(k == 7),
                          r=[W16, hT], w=[ps])
                stg = (ofm if dt == F32 else ofb)[idx % 4]
                eng = "act" if idx % 2 else "dve"
                if scl is None:
                    kb.cp(eng, stg[0:ncol, :], ps[0:ncol, :], r=[ps], w=[stg])
                else:
                    kb.ts("dve", stg[0:ncol, :], ps[0:ncol, :], scl, None, ALU.mult, r=[ps], w=[stg])
                kb.st(dst[:, t0:t0 + TT], stg[0:ncol, :], stg)
            for sub in range(0 if 'notk' in dbgs else 4):
                s0 = t0 + sub * 128
                psA = X.pb[nps % 6]; nps += 1
                psB = X.pb[nps % 6]; nps += 1
                psC = X.pb[nps % 6]; nps += 1
                for k in range(8):
                    kb.mm(psA[:, 0:512], hT[:, k, sub * 128:(sub + 1) * 128], W16[:, k, 256:768], start=(k == 0), stop=(k == 7),
                          r=[W16, hT], w=[psA])
                for k in range(8):
                    kb.mm(psB[:, 0:256], hT[:, k, sub * 128:(sub + 1) * 128], W16[:, k, 768:1024], start=(k == 0), stop=(k == 7),
                          r=[W16, hT], w=[psB])
                for k in range(8):
                    kb.mm(psC[:, 0:128], hT[:, k, sub * 128:(sub + 1) * 128], W16[:, k, 2448:2576], start=(k == 0), stop=(k == 7),
                          r=[W16, hT], w=[psC])
                for k in range(8):
                    kb.mm(psC[:, 128:256], hT[:, k, sub * 128:(sub + 1) * 128], W16[:, k, 2704:2832], start=(k == 0), stop=(k == 7),
                          r=[W16, hT], w=[psC])
                ok = otk[sub % 2]; ov = otv[sub % 2]; on = otn[sub % 2]
                kb.cp("act", ok[:, 0:256], psA[:, 0:256], r=[psA], w=[ok])
                kb.st(X.gk_tok[s0:s0 + 128, :], ok[:, 0:256], ok)
                kb.cp("dve", ov[:, 0:256], psA[:, 256:512], r=[psA], w=[ov])
                kb.cp("act", ov[:, 256:512], psB[:, 0:256], r=[psB], w=[ov])
                kb.st(X.gv_tok[s0:s0 + 128, :], ov[:, :], ov)
                kb.cp("dve", on[:, :], psC[:, 0:256], r=[psC], w=[on])
                kb.st(X.nv_tok[0, s0:s0 + 128, :], on[:, 0:128], on)
                kb.st(X.nv_tok[1, s0:s0 + 128, :], on[:, 128:256], on)


def phase_gla(X):
    kb, P, nc = X.kb, X.P, X.nc
    cd = X.cd
    with ExitStack() as es:
        gk_up = kb.sb(es, "g_gkup", [16, 256], F32)
        nbias = kb.sb(es, "g_nbias", [128, 2], F32)
        brow = kb.sb(es, "g_brow", [64, 512], F32)
        resetm = kb.sb(es, "g_resetm", [128, TT], F32)
        u64 = kb.sb(es, "g_u64", [64, 64], F32)
        cmask = kb.sb(es, "g_cmask", [64, 512], F32)
        gnorm = kb.sb(es, "g_gnorm", [128, 1], F32)
        onec = kb.sb(es, "g_onec", [128, 1], F32)
        kb.ld(gk_up[:], X.gk_up, gk_up)
        kb.ld(nbias[:], X.gk_bias_col, nbias)
        kb.ts("dve", nbias[:], nbias[:], -1.0, None, ALU.mult, r=[nbias], w=[nbias])
        kb.ld(brow[:, 0:256], X.gk_bias_rep, brow)
        kb.ld(brow[:, 256:512], X.gk_bias_rep, brow)
        kb.ld(resetm[:], cd["resetm"], resetm)
        kb.ld(u64[:], cd["u64"], u64)
        kb.ld(cmask[:], cd["cmask64"], cmask)
        kb.ld(gnorm[:], X.gla_norm, gnorm)
        kb.memset("dve", onec[:], 1.0, w=[onec])
        S = [kb.sb(es, "g_S%d" % pc, [128, 256], F32) for pc in range(2)]
        for pc in range(2):
            kb.memset("dve", S[pc][:], 0.0, w=[S[pc]])
        Sprev = [kb.sb(es, "g_Sprev%d" % pc, [128, 8, 256], BF16) for pc in range(2)]
        KVs = [kb.sb(es, "g_KVs%d" % pc, [128, 8, 256], F32) for pc in range(2)]
        lrT = kb.sb(es, "g_lrT", [16, TT], F32)
        qT = [kb.sb(es, "g_qT%d" % pc, [128, TT], F32) for pc in range(2)]
        kT = [kb.sb(es, "g_kT%d" % pc, [128, TT], F32) for pc in range(2)]
        rT = kb.sb(es, "g_rT", [128, 4, TT], F32)
        k64 = kb.sb(es, "g_k64", [64, 8, 256], F32)
        v64 = kb.sb(es, "g_v64", [64, 8, 512], BF16)
        ef = kb.sb(es, "g_ef", [128, TT], F32)
        bT = kb.sb(es, "g_bT", [128, TT], F32)
        eb = kb.sb(es, "g_eb", [128, TT], F32)
        enb = kb.sb(es, "g_enb", [128, TT], F32)
        qd = [kb.sb(es, "g_qd%d" % pc, [128, TT], BF16) for pc in range(2)]
        kd = [kb.sb(es, "g_kd%d" % pc, [128, TT], BF16) for pc in range(2)]
        eend = [kb.sb(es, "g_eend%d" % pc, [128, 8], F32) for pc in range(2)]
        zt = kb.sb(es, "g_zt", [64, 512], F32)
        spt = kb.sb(es, "g_spt", [64, 512], F32)
        ext = kb.sb(es, "g_ext", [64, 512], F32)
        kte = kb.sb(es, "g_kte", [64, 8, 256], BF16)
        attb = [kb.sb(es, "g_attb%d" % i, [64, 512], BF16) for i in range(2)]
        osq = kb.sb(es, "g_osq", [128, TT], BF16)
        rs = kb.sb(es, "g_rs", [128, TT], F32)
        sr = kb.sb(es, "g_sr", [128, TT], F32)
        o1 = kb.sb(es, "g_o1", [128, TT], F32)
        ob = [kb.sb(es, "g_ob%d" % i, [128, TT], BF16) for i in range(2)]
        pbi = [0]

        def nextpb():
            pbi[0] += 1
            return X.pb[pbi[0] % 8]

        for tt_ in range(NT):
            t0 = tt_ * TT
            kb.ld(lrT[:], X.glrT[:, t0:t0 + TT], lrT)
            for pc in range(2):
                kb.ld(qT[pc][:], X.gqT[pc * 128:(pc + 1) * 128, t0:t0 + TT], qT[pc])
                kb.ld(kT[pc][:], X.gkT[pc * 128:(pc + 1) * 128, t0:t0 + TT], kT[pc])
            for h in range(4):
                kb.ld(rT[:, h, :], X.grT[h * 128:(h + 1) * 128, t0:t0 + TT], rT)
            kb.ld(k64[:], X.gk_tok[t0:t0 + TT, :].rearrange("(n s) d -> s n d", s=64), k64)
            kb.ld(v64[:], X.gv_tok[t0:t0 + TT, :].rearrange("(n s) d -> s n d", s=64), v64)
            for pc in range(2):
                pz = nextpb()
                kb.mm(pz[:, :], gk_up[0:16, pc * 128:(pc + 1) * 128], lrT[0:16, :], r=[gk_up, lrT], w=[pz])
                kb.act(ef[:], pz[:, :], AF.Exp, bias=nbias[:, pc:pc + 1], scale=-1.0, r=[pz, nbias], w=[ef])
                kb.act(ef[:], ef[:], AF.Ln, bias=onec[:, 0:1], r=[ef, onec], w=[ef])
                kb.ts("dve", ef[:], ef[:], -1.0 / 16.0, None, ALU.mult, r=[ef], w=[ef])
                P.op("dve", (lambda o, d0, d1: (lambda e: e.tensor_tensor_scan(o, d0, d1, 0.0, ALU.mult, ALU.add)))(bT[:], resetm[:], ef[:]),
                     [resetm, ef], [bT])
                kb.act(eb[:], bT[:], AF.Exp, r=[bT], w=[eb])
                kb.act(enb[:], bT[:], AF.Exp, scale=-1.0, r=[bT], w=[enb])
                kb.stt("dve", qd[pc][:], qT[pc][:], 0.125, eb[:], ALU.mult, ALU.mult, r=[qT[pc], eb], w=[qd[pc]])
                kb.tt("dve", kd[pc][:], kT[pc][:], enb[:], ALU.mult, r=[kT[pc], enb], w=[kd[pc]])
                kb.act(eend[pc][:], bT[:].rearrange("p (n c) -> p n c", c=64)[:, :, 63], AF.Exp, r=[bT], w=[eend[pc]])
            for gi in range(4):
                pz = nextpb()
                for cc in range(2):
                    j = 2 * gi + cc
                    kb.mm(pz[0:64, cc * 256:(cc + 1) * 256], lrT[0:16, j * 64:(j + 1) * 64], gk_up[0:16, :], r=[gk_up, lrT], w=[pz])
                kb.tt("dve", zt[:], pz[0:64, :], brow[:], ALU.add, r=[pz, brow], w=[zt])
                kb.act(spt[:], zt[:], AF.Exp, scale=-1.0, r=[zt], w=[spt])
                kb.act(spt[:], spt[:], AF.Ln, bias=onec[0:64, 0:1], r=[spt, onec], w=[spt])
                p2 = nextpb()
                kb.mm(p2[0:64, :], u64[:, :], spt[:], r=[u64, spt], w=[p2])
                kb.act(ext[:], p2[0:64, :], AF.Exp, scale=-1.0 / 16.0, r=[p2], w=[ext])
                kb.tt("dve", kte[:, 2 * gi:2 * gi + 2, :], k64[:, 2 * gi:2 * gi + 2, :],
                      ext[:].rearrange("p (a b) -> p a b", a=2), ALU.mult, r=[k64, ext], w=[kte])
            for pc in range(2):
                for jj in range(4):
                    pk = nextpb()
                    for cc in range(2):
                        j = 2 * jj + cc
                        kb.mm(pk[:, cc * 256:(cc + 1) * 256], kte[0:64, j, pc * 128:(pc + 1) * 128],
                              v64[0:64, j, pc * 256:(pc + 1) * 256], r=[kte, v64], w=[pk])
                    kb.cp("act", KVs[pc][:, 2 * jj:2 * jj + 2, :], pk[:, :].rearrange("p (a b) -> p a b", a=2), r=[pk], w=[KVs[pc]])
                for j in range(8):
                    kb.cp("act", Sprev[pc][:, j, :], S[pc][:], r=[S[pc]], w=[Sprev[pc]])
                    kb.stt("dve", S[pc][:], S[pc][:], eend[pc][:, j:j + 1], KVs[pc][:, j, :], ALU.mult, ALU.add,
                           r=[S[pc], eend[pc], KVs[pc]], w=[S[pc]])
                for hh in range(2):
                    pa = nextpb()
                    for j in range(8):
                        kb.mm(pa[0:64, j * 64:(j + 1) * 64], kd[pc][hh * 64:(hh + 1) * 64, j * 64:(j + 1) * 64],
                              qd[pc][hh * 64:(hh + 1) * 64, j * 64:(j + 1) * 64], r=[kd[pc], qd[pc]], w=[pa], rg=hh * 64)
                    kb.tt("dve", attb[hh][:], pa[0:64, :], cmask[:], ALU.mult, r=[pa, cmask], w=[attb[hh]])
                for hh in range(2):
                    h = pc * 2 + hh
                    po = nextpb()
                    for j in range(8):
                        kb.mm(po[:, j * 64:(j + 1) * 64], v64[0:64, j, h * 128:(h + 1) * 128], attb[hh][0:64, j * 64:(j + 1) * 64],
                              start=True, stop=False, r=[v64, attb[hh]], w=[po])
                        kb.mm(po[:, j * 64:(j + 1) * 64], Sprev[pc][hh * 64:(hh + 1) * 64, j, hh * 128:(hh + 1) * 128],
                              qd[pc][hh * 64:(hh + 1) * 64, j * 64:(j + 1) * 64], start=False, stop=True,
                              r=[Sprev[pc], qd[pc]], w=[po], rg=hh * 64)
                    kb.act(osq[:], po[:, :], AF.Square, r=[po], w=[osq])
                    pss = nextpb()
                    kb.mm(pss[:, :], X.ones_b[:, :], osq[:], r=[X.ones_b, osq], w=[pss])
                    kb.act(rs[:], pss[:, :], AF.Ln, bias=X.epsc[:, 0:1], scale=1.0 / 128.0, r=[pss, X.epsc], w=[rs])
                    kb.act(rs[:], rs[:], AF.Exp, scale=-0.5, r=[rs], w=[rs])
                    kb.act(sr[:], rT[:, h, :], AF.Silu, r=[rT], w=[sr])
                    kb.tt("dve", o1[:], po[:, :], rs[:], ALU.mult, r=[po, rs], w=[o1])
                    o_ = ob[h % 2]
                    kb.stt("dve", o_[:], o1[:], gnorm[:, 0:1], sr[:], ALU.mult, ALU.mult, r=[o1, gnorm, sr], w=[o_])
                    kb.st(X.mixT[h * 128:(h + 1) * 128, t0:t0 + TT], o_[:], o_)
        P.barrier()


def phase_nsa(X):
    kb, P, nc = X.kb, X.P, X.nc
    cd = X.cd
    GEL = 1.5957691216057308
    with ExitStack() as es:
        kslc = [kb.sb(es, "n_kslc%d" % g, [128, T], BF16) for g in range(2)]
        kwin = [kb.sb(es, "n_kwin%d" % g, [128, T], BF16) for g in range(2)]
        vslc = [kb.sb(es, "n_vslc%d" % g, [128, 32, 128], BF16) for g in range(2)]
        vwin = [kb.sb(es, "n_vwin%d" % g, [128, 32, 128], BF16) for g in range(2)]
        kcT = [kb.sb(es, "n_kcT%d" % g, [128, 256], BF16) for g in range(2)]
        vc = [kb.sb(es, "n_vc%d" % g, [128, 2, 128], BF16) for g in range(2)]
        esel = kb.sb(es, "n_esel", [64, T], BF16)
        winm = kb.sb(es, "n_winm", [128, 8, 512], BF16)
        ovl = kb.sb(es, "n_ovl", [128, 2, 65], BF16)
        M1 = kb.sb(es, "n_M1", [128, 32, 64], F32)
        M2 = kb.sb(es, "n_M2", [128, 32, 64], F32)
        gsel = kb.sb(es, "n_gsel", [24, 24 * 64], F32)
        tiny = kb.sb(es, "n_tiny", [128, 1], F32)
        kb.ld(esel[:], cd["esel"], esel)
        kb.ld(winm[:], cd["winm"], winm)
        kb.ld(ovl[:], cd["ovl"], ovl)
        kb.ld(M1[:], cd["selm1"], M1)
        kb.ld(M2[:], cd["selm2"], M2)
        kb.ld(gsel[:], cd["gsel"], gsel)
        for g in range(2):
            kb.ld(kslc[g][0:64, :], X.nkT[2][g * 64:(g + 1) * 64, :], kslc[g])
            kb.ld(kslc[g][64:124, :], cd["esel"][0:60, :], kslc[g])
            kb.ld(kslc[g][124:128, :], cd["kaug"], kslc[g])
            kb.memset("dve", kwin[g][64:128, :], 0.0, w=[kwin[g]])
            kb.ld(kwin[g][0:64, :], X.nkT[3][g * 64:(g + 1) * 64, :], kwin[g])
            kb.ld(kwin[g][124:128, :], cd["kaug"], kwin[g])
            kb.memset("dve", vslc[g][:, :, 64:128], 1.0, w=[vslc[g]])
            kb.memset("dve", vwin[g][:, :, 64:128], 1.0, w=[vwin[g]])
            kb.ld(vslc[g][:, :, 0:64], X.nv_tok[0][:, g * 64:(g + 1) * 64].rearrange("(n p) d -> p n d", p=128), vslc[g])
            kb.ld(vwin[g][:, :, 0:64], X.nv_tok[1][:, g * 64:(g + 1) * 64].rearrange("(n p) d -> p n d", p=128), vwin[g])
            kb.memset("dve", kcT[g][:], 0.0, w=[kcT[g]])
            kb.memset("dve", vc[g][:], 0.0, w=[vc[g]])
        with ExitStack() as es2:
            w1s = kb.sb(es2, "n_w1s", [64, 32, 256], F32)
            w1b = kb.sb(es2, "n_w1b", [64, 32, 256], BF16)
            w2s = kb.sb(es2, "n_w2s", [128, 2, 64], F32)
            w2b = kb.sb(es2, "n_w2b", [128, 2, 64], BF16)
            pes = kb.sb(es2, "n_pes", [64, 32], F32)
            peb = kb.sb(es2, "n_peb", [64, 32], BF16)
            bh = kb.sb(es2, "n_bh", [128, 2], F32)
            cin = kb.sb(es2, "n_cin", [64, T], BF16)
            xs = kb.sb(es2, "n_xs", [128, 256], F32)
            x2 = kb.sb(es2, "n_x2", [128, 256], F32)
            hT = [kb.sb(es2, "n_hT%d" % i, [128, 256], BF16) for i in range(2)]
            for jkv in range(2):
                kb.ld(w1s[:], X.cmp_w1[jkv].rearrange("(l d) m -> d l m", d=64), w1s)
                kb.cp("dve", w1b[:, 0:16, :], w1s[:, 0:16, :], r=[w1s], w=[w1b])
                kb.cp("act", w1b[:, 16:32, :], w1s[:, 16:32, :], r=[w1s], w=[w1b])
                kb.ld(w2s[:], X.cmp_w2[jkv].rearrange("(c p) d -> p c d", p=128), w2s)
                kb.cp("dve", w2b[:], w2s[:], r=[w2s], w=[w2b])
                kb.ld(pes[:], X.cmp_peT[jkv], pes)
                kb.cp("dve", peb[:], pes[:], r=[pes], w=[peb])
                pbh = X.pb[0]
                for mc in range(2):
                    for l in range(32):
                        kb.mm(pbh[:, mc:mc + 1], w1b[0:64, l, mc * 128:(mc + 1) * 128], peb[0:64, l:l + 1],
                              start=(l == 0), stop=(l == 31), r=[w1b, peb], w=[pbh])
                kb.cp("dve", bh[:], pbh[:, 0:2], r=[pbh], w=[bh])
                for g in range(2):
                    kb.ld(cin[:], X.nkT[jkv][g * 64:(g + 1) * 64, :], cin)
                    cv = cin[:].rearrange("p (n s) -> p n s", s=16)
                    for mc in range(2):
                        ph = X.pb[1 + mc]
                        for l in range(32):
                            rhs = cv[:, 0:255, l] if l < 16 else cv[:, 1:256, l - 16]
                            kb.mm(ph[:, 0:255], w1b[0:64, l, mc * 128:(mc + 1) * 128], rhs,
                                  start=(l == 0), stop=(l == 31), r=[w1b, cin], w=[ph])
                        kb.act(xs[:, 0:255], ph[:, 0:255], AF.Identity, bias=bh[:, mc:mc + 1], r=[ph, bh], w=[xs])
                        kb.act(x2[:, 0:255], xs[:, 0:255], AF.Square, r=[xs], w=[x2])
                        kb.ts("dve", x2[:, 0:255], x2[:, 0:255], 0.044715, 1.0, ALU.mult, ALU.add, r=[x2], w=[x2])
                        kb.tt("dve", x2[:, 0:255], x2[:, 0:255], xs[:, 0:255], ALU.mult, r=[x2, xs], w=[x2])
                        kb.act(x2[:, 0:255], x2[:, 0:255], AF.Sigmoid, scale=GEL, r=[x2], w=[x2])
                        kb.memset("dve", hT[mc][:, 255:256], 0.0, w=[hT[mc]])
                        kb.tt("dve", hT[mc][:, 0:255], xs[:, 0:255], x2[:, 0:255], ALU.mult, r=[xs, x2], w=[hT[mc]])
                    if jkv == 0:
                        pk = X.pb[3]
                        for mc in range(2):
                            kb.mm(pk[0:64, 0:255], w2b[:, mc, :], hT[mc][:, 0:255], start=(mc == 0), stop=(mc == 1),
                                  r=[w2b, hT[mc]], w=[pk])
                        kb.cp("dve", kcT[g][0:64, 0:255], pk[0:64, 0:255], r=[pk], w=[kcT[g]])
                        kb.ld(kcT[g][124:128, :], cd["kaugc"], kcT[g])
                    else:
                        for ck in range(2):
                            pv = X.pb[3 + ck]
                            for mc in range(2):
                                kb.mm(pv[:, 0:64], hT[mc][:, ck * 128:(ck + 1) * 128], w2b[:, mc, :], start=(mc == 0), stop=(mc == 1),
                                      r=[w2b, hT[mc]], w=[pv])
                            kb.cp("act", vc[g][:, ck, 0:64], pv[:, 0:64], r=[pv], w=[vc[g]])
                        kb.memset("dve", vc[g][:, :, 64:128], 1.0, w=[vc[g]])
            P.barrier()
        if X.dbg_stop == "cmp":
            for g in range(2):
                d1 = nc.dram_tensor("kcT_d%d" % g, [68, 256], BF16, kind="ExternalOutput").ap()
                kb.st(d1, kcT[g][:], kcT[g])
                d2 = nc.dram_tensor("vc_d%d" % g, [128, 256], BF16, kind="ExternalOutput").ap()
                kb.st(d2, vc[g][:].rearrange("p a b -> p (a b)"), vc[g])
            P.barrier()
            return
        kb.memset("dve", tiny[:], 1e-20, w=[tiny])
        q8 = [kb.sb(es, "n_q8_%d" % i, [128, 8, TT], BF16) for i in range(2)]
        for i in range(2):
            kb.memset("dve", q8[i][64:128, :, :], 0.0, w=[q8[i]])
        cmpm = kb.sb(es, "n_cmpm", [128, 2, TT], BF16)
        gl = kb.sb(es, "n_gl", [24, TT], F32)
        pe16 = [kb.sb(es, "n_pe16_%d" % i, [128, TT], BF16) for i in range(6)]
        pcmp = [[kb.sb(es, "n_pcmp%d_%d" % (r, ck), [128, TT], BF16) for ck in range(2)] for r in range(4)]
        acc = kb.sb(es, "n_acc", [64, 4, TT], F32)
        rls = [kb.sb(es, "n_rl%d" % i, [64, TT], F32) for i in range(2)]
        ftmps = [kb.sb(es, "n_ftmp%d" % i, [64, TT], F32) for i in range(2)]
        otmps = [kb.sb(es, "n_otmp%d" % i, [64, TT], F32) for i in range(2)]
        cmbi = [0]
        ob = [kb.sb(es, "n_ob%d" % i, [64, TT], BF16) for i in range(2)]
        selbT = kb.sb(es, "n_selbT", [64, TT], BF16)
        rli = kb.sb(es, "n_rli", [128, 4, 4], F32)
        imp4 = kb.sb(es, "n_imp4", [128, 4, 64], F32)
        itmps = [kb.sb(es, "n_itmp%d" % i, [128, 4, 64], F32) for i in range(3)]
        top84 = kb.sb(es, "n_top84", [128, 4, 8], F32)
        pei = [0]

        def next_pe16():
            pei[0] += 1
            return pe16[pei[0] % 6]

        sc32 = [kb.sb(es, "n_sc32_%d" % i, [128, TT], F32) for i in range(3)]
        sci = [0]

        def next_sc():
            sci[0] += 1
            return sc32[sci[0] % 3]

        def combine(po, h, r, br, first):
            cmbi[0] += 1
            rl, ftmp, otmp = rls[cmbi[0] % 2], ftmps[cmbi[0] % 2], otmps[cmbi[0] % 2]
            pg = X.pb[3]
            kb.mm(pg[0:64, :], gsel[0:24, (3 * h + br) * 64:(3 * h + br + 1) * 64], gl[0:24, :], r=[gsel, gl], w=[pg])
            kb.ts("dve", rl[:], po[64:128, :], 1e-20, None, ALU.max, r=[po], w=[rl])
            kb.act(rl[:], rl[:], AF.Ln, r=[rl], w=[rl])
            kb.act(rl[:], rl[:], AF.Exp, scale=-1.0, r=[rl], w=[rl])
            kb.tt("dve", ftmp[:], pg[0:64, :], rl[:], ALU.mult, r=[pg, rl], w=[ftmp])
            if first:
                kb.tt("dve", acc[:, r, :], po[0:64, :], ftmp[:], ALU.mult, r=[po, ftmp], w=[acc])
            else:
                kb.tt("dve", otmp[:], po[0:64, :], ftmp[:], ALU.mult, r=[po, ftmp], w=[otmp])
                kb.tt("dve", acc[:, r, :], acc[:, r, :], otmp[:], ALU.add, r=[acc, otmp], w=[acc])

        LOOK = 3
        poi = [0]

        def next_po():
            poi[0] += 1
            return X.pb[poi[0] % 3]

        for qt in range(NT):
            t0 = qt * TT
            q = q8[qt % 2]
            kb.ld(q[0:64, :, :], X.nqT[:, t0:t0 + TT].rearrange("(h d) t -> d h t", d=64), q)
            kb.ld(q[124:128, :, :], cd["qaug"][:, :, t0:t0 + TT].rearrange("h a t -> a h t"), q)
            kb.ld(cmpm[:], cd["cmpm"][qt], cmpm)
            kb.ld(gl[:], X.ngT[:, t0:t0 + TT], gl)
            kb.act(gl[:], gl[:], AF.Sigmoid, r=[gl], w=[gl])
            ncks = 2 if qt >= 4 else 1
            for g in range(2):
                pimp = [X.pb[4 + sub] for sub in range(4)]
                nsc = 0
                for r in range(4):
                    h = g * 4 + r
                    for ck in range(ncks):
                        ps = X.pb[1 + nsc % 2]
                        nsc += 1
                        kb.mm(ps[:, :], kcT[g][:, ck * 128:(ck + 1) * 128], q[:, h, :], r=[kcT[g], q], w=[ps])
                        sc_ = next_sc()
                        kb.tt("dve", sc_[:], ps[:, :], cmpm[:, ck, :], ALU.add, r=[ps, cmpm], w=[sc_])
                        kb.act(pcmp[r][ck][:], sc_[:], AF.Exp, r=[sc_], w=[pcmp[r][ck]])
                for r in range(4):
                    h = g * 4 + r
                    po = X.pb[0]
                    for ck in range(ncks):
                        kb.mm(po[:, :], vc[g][:, ck, :], pcmp[r][ck][:], start=(ck == 0), stop=(ck == ncks - 1),
                              r=[vc[g], pcmp[r][ck]], w=[po])
                    for sub in range(4):
                        for ck in range(ncks):
                            kb.mm(pimp[sub][:, r * 65:(r + 1) * 65], pcmp[r][ck][:, sub * 128:(sub + 1) * 128], ovl[:, ck, :],
                                  start=(ck == 0), stop=(ck == ncks - 1), r=[pcmp[r][ck], ovl], w=[pimp[sub]])
                    combine(po, h, r, 0, True)
                pv4 = X.pball[:, 4:8, 0:260].rearrange("p s (r c) -> p s r c", c=65)
                kb.ts("dve", rli[:], pv4[:, :, :, 64], 1e-20, None, ALU.max, r=pimp, w=[rli])
                kb.recip(rli[:], rli[:], r=[rli], w=[rli])
                for r in range(4):
                    dst = imp4 if r == 0 else itmps[r - 1]
                    itmp = dst
                    kb.tt("dve", dst[:], pv4[:, :, r, 0:64], rli[:, :, r:r + 1].to_broadcast([128, 4, 64]), ALU.mult,
                          r=pimp + [rli], w=[dst])
                    if r > 0:
                        kb.tt("dve", imp4[:], imp4[:], itmp[:], ALU.add, r=[imp4, itmp], w=[imp4])
                kb.tt("dve", imp4[:], imp4[:], M1[:, qt * 4:qt * 4 + 4, :], ALU.mult, r=[imp4, M1], w=[imp4])
                kb.tt("dve", imp4[:], imp4[:], M2[:, qt * 4:qt * 4 + 4, :], ALU.add, r=[imp4, M2], w=[imp4])
                for sub in range(4):
                    P.op("dve", (lambda o, i: (lambda e: e.max(out=o, in_=i)))(top84[:, sub, :], imp4[:, sub, :]), [imp4], [top84])
                kb.tt("dve", imp4[:], imp4[:], top84[:, :, 7:8].to_broadcast([128, 4, 64]), ALU.is_ge, r=[imp4, top84], w=[imp4])
                kb.ts("dve", imp4[:], imp4[:], 1.0, 30000.0, ALU.subtract, ALU.mult, r=[imp4], w=[imp4])
                pt = X.pb[3]
                for sub in range(4):
                    kb.tr(pt[0:64, sub * 128:(sub + 1) * 128], imp4[:, sub, :], X.ident_f[:, :], r=[imp4, X.ident_f], w=[pt])
                if qt == NT - 1:
                    kb.cp("act", selbT[:, :], pt[0:64, :], r=[pt], w=[selbT])
                kb.cp("act", q[64:124, 4 * g:4 * g + 4, :], pt[0:60, :].unsqueeze(1).to_broadcast([60, 4, TT]), r=[pt], w=[q])
                jobs = []
                nst = 4 * qt + 4
                st0 = max(0, 4 * qt - 4)
                for r in range(4):
                    for st in range(nst):
                        jobs.append((r, 1, st, st == 0, st == nst - 1))
                    for st in range(st0, nst):
                        jobs.append((r, 2, st, st == st0, st == nst - 1))
                pes = {}
                pos = {}
                nsc = 0

                def front(i):
                    r, br, st, isfirst, islast = jobs[i]
                    h = g * 4 + r
                    ps = X.pb[4 + i % 4]
                    pe_ = next_pe16()
                    if br == 1:
                        hi = st >= 30
                        kb.mm(ps[:, :], kslc[g][:, st * 128:(st + 1) * 128], q[:, h, :], start=True, stop=not hi,
                              r=[kslc[g], q], w=[ps])
                        if hi:
                            kb.mm(ps[:, :], esel[0:64, st * 128:(st + 1) * 128], selbT[0:64, :], start=False, stop=True,
                                  r=[esel, selbT], w=[ps])
                        masked = st >= 4 * qt
                    else:
                        kb.mm(ps[:, :], kwin[g][:, st * 128:(st + 1) * 128], q[:, h, :], r=[kwin[g], q], w=[ps])
                        masked = True
                    if masked:
                        sc_ = next_sc()
                        kb.tt("dve", sc_[:], ps[:, :], winm[:, 4 + st - 4 * qt, :], ALU.add, r=[ps, winm], w=[sc_])
                        kb.act(pe_[:], sc_[:], AF.Exp, r=[sc_], w=[pe_])
                    else:
                        kb.act(pe_[:], ps[:, :], AF.Exp, r=[ps], w=[pe_])
                    pes[i] = pe_

                def back(i):
                    r, br, st, isfirst, islast = jobs[i]
                    h = g * 4 + r
                    if isfirst:
                        pos[(r, br)] = next_po()
                    po = pos[(r, br)]
                    vv = vslc if br == 1 else vwin
                    kb.mm(po[:, :], vv[g][:, st, :], pes[i][:], start=isfirst, stop=islast, r=[vv[g], pes[i]], w=[po])
                    if islast:
                        combine(po, h, r, br, False)
                        if br == 2:
                            o_ = ob[h % 2]
                            kb.cp("act", o_[:], acc[:, r, :], r=[acc], w=[o_])
                            kb.st(X.mixT[512 + h * 64:512 + (h + 1) * 64, t0:t0 + TT], o_[:], o_)

                LK = 4
                for i in range(min(LK, len(jobs))):
                    front(i)
                for i in range(0, len(jobs), 2):
                    for j in (i + LK, i + LK + 1):
                        if j < len(jobs):
                            front(j)
                    for j in (i, i + 1):
                        if j < len(jobs):
                            back(j)
        P.barrier()


def phase_l0out(X):
    kb, P, nc = X.kb, X.P, X.nc
    with ExitStack() as es:
        W16 = kb.sb(es, "wo16", [128, 8, 1024], BF16)
        with ExitStack() as es2:
            load_cast(X, es2, "wo", X.hyb_w_out, D, D, W16, chunk=512)
            P.barrier()
        mx = [kb.sb(es, "o_mx%d" % i, [128, 8, TT], BF16) for i in range(2)]
        xt = [kb.sb(es, "o_xt%d" % i, [128, 8, TT], F32) for i in range(2)]
        xo = [kb.sb(es, "o_xo%d" % i, [128, TT], F32) for i in range(3)]
        msrc = X.mixT.rearrange("(k p) t -> p k t", p=128)
        xsrc = X.xT.rearrange("(k p) t -> p k t", p=128)
        n = 0
        for tt_ in range(NT):
            t0 = tt_ * TT
            m = mx[tt_ % 2]
            x = xt[tt_ % 2]
            for k in range(8):
                kb.ld(m[:, k, :], msrc[:, k, t0:t0 + TT], m)
                kb.ld(x[:, k, :], xsrc[:, k, t0:t0 + TT], x)
            for dc in range(8):
                ps = X.pb[n % 4]
                o = xo[n % 3]
                n += 1
                for k in range(8):
                    kb.mm(ps[:, :], W16[:, k, dc * 128:(dc + 1) * 128], m[:, k, :], start=(k == 0), stop=(k == 7), r=[W16, m], w=[ps])
                kb.stt("dve", o[:], ps[:, :], X.modt[:, 0, 16 + dc:17 + dc], x[:, dc, :], ALU.mult, ALU.add,
                       r=[ps, X.modt, x], w=[o])
                kb.st(X.x1T[dc * 128:(dc + 1) * 128, t0:t0 + TT], o[:], o)
        P.barrier()


def phase_dense(X):
    kb, P, nc = X.kb, X.P, X.nc
    W = 256
    NF = 22
    with ExitStack() as es:
        Wgu = kb.sb(es, "d_wgu", [128, 8, 5632], BF16)
        Wd = kb.sb(es, "d_wd", [128, NF, 1024], BF16)
        with ExitStack() as es2:
            load_cast(X, es2, "dgu", X.dense_w_gu, D, 5632, Wgu, chunk=512)
            P.barrier()
        with ExitStack() as es2:
            load_cast(X, es2, "ddn", X.dense_w_down, 2816, 1024, Wd, chunk=256)
            P.barrier()
        xt = [kb.sb(es, "d_xt%d" % i, [128, 8, W], F32) for i in range(3)]
        sq = kb.sb(es, "d_sq", [128, 8, W], BF16)
        hF = kb.sb(es, "d_hF", [128, 8, W], F32)
        hTs = [kb.sb(es, "d_hT%d" % i, [128, 8, W], BF16) for i in range(2)]
        rstd = kb.sb(es, "d_rstd", [128, W], F32)
        sg = [kb.sb(es, "d_sg%d" % i, [128, W], F32) for i in range(2)]
        a16s = [kb.sb(es, "d_a16_%d" % i, [128, NF, W], BF16) for i in range(2)]
        xo = [kb.sb(es, "d_xo%d" % i, [128, W], F32) for i in range(3)]
        xsrc = X.x1T.rearrange("(k p) t -> p k t", p=128)
        n = 0
        def prep(tt_):
            x = xt[tt_ % 3]
            for k in range(8):
                kb.ld(x[:, k, :], xsrc[:, k, tt_ * W:(tt_ + 1) * W], x)
            norm_modulate(X, x, hTs[tt_ % 2], 1, sq, X.pb[7], rstd, hF, W=W)

        prep(0)
        for tt_ in range(T // W):
            t0 = tt_ * W
            x = xt[tt_ % 3]
            hT = hTs[tt_ % 2]
            a16 = a16s[tt_ % 2]
            if tt_ + 1 < T // W:
                prep(tt_ + 1)
            for fc in range(NF):
                ps = X.pb[n % 6]
                n += 1
                for k in range(8):
                    kb.mm(ps[:, 0:W], Wgu[:, k, fc * 128:(fc + 1) * 128], hT[:, k, :], start=(k == 0), stop=(k == 7), r=[Wgu, hT], w=[ps])
                for k in range(8):
                    kb.mm(ps[:, W:2 * W], Wgu[:, k, 2816 + fc * 128:2816 + (fc + 1) * 128], hT[:, k, :], start=(k == 0), stop=(k == 7),
                          r=[Wgu, hT], w=[ps])
                s_ = sg[fc % 2]
                kb.act(s_[:], ps[:, 0:W], AF.Silu, r=[ps], w=[s_])
                kb.tt("dve", a16[:, fc, :], s_[:], ps[:, W:2 * W], ALU.mult, r=[s_, ps], w=[a16])
            for dc in range(8):
                ps = X.pb[n % 6]
                o = xo[n % 3]
                n += 1
                for fc in range(NF):
                    kb.mm(ps[:, 0:W], Wd[:, fc, dc * 128:(dc + 1) * 128], a16[:, fc, :], start=(fc == 0), stop=(fc == NF - 1), r=[Wd, a16], w=[ps])
                kb.stt("dve", o[:], ps[:, 0:W], X.modt[:, 1, 16 + dc:17 + dc], x[:, dc, :], ALU.mult, ALU.add,
                       r=[ps, X.modt, x], w=[o])
                kb.st(X.x2T[dc * 128:(dc + 1) * 128, t0:t0 + W], o[:], o)
        P.barrier()


def phase_ssm_in(X):
    kb, P, nc = X.kb, X.P, X.nc
    with ExitStack() as es:
        W16 = kb.sb(es, "s_win16", [128, 8, 5152], BF16)
        with ExitStack() as es2:
            load_cast(X, es2, "swin", X.ssm_w_in, D, 5152, W16, chunk=368)
            P.barrier()
        xt = [kb.sb(es, "s_xt%d" % i, [128, 8, TT], F32) for i in range(2)]
        sq = kb.sb(es, "s_sq", [128, 8, TT], BF16)
        hF = kb.sb(es, "s_hF", [128, 8, TT], F32)
        hTs = [kb.sb(es, "s_hT%d" % i, [128, 8, TT], BF16) for i in range(2)]
        rstd = kb.sb(es, "s_rstd", [128, TT], F32)
        ofm = [kb.sb(es, "s_ofm%d" % i, [128, TT], F32) for i in range(4)]
        zst = [kb.sb(es, "s_zst%d" % i, [128, 2048], F32) for i in range(2)]
        dtb = kb.sb(es, "s_dtb", [128, 32], F32)
        d1 = kb.sb(es, "s_d1", [128, 32], F32)
        d2 = kb.sb(es, "s_d2", [128, 32], F32)
        d3 = [kb.sb(es, "s_d3_%d" % i, [128, 32], F32) for i in range(2)]
        onec = kb.sb(es, "s_onec", [128, 1], F32)
        kb.memset("dve", onec[:], 1.0, w=[onec])
        kb.ld(dtb[:], X.dt_bias_rep, dtb)
        xsrc = X.x2T.rearrange("(k p) t -> p k t", p=128)
        n = 0
        def prep(tt_):
            x = xt[tt_ % 2]
            for k in range(8):
                kb.ld(x[:, k, :], xsrc[:, k, tt_ * TT:(tt_ + 1) * TT], x)
            norm_modulate(X, x, hTs[tt_ % 2], 2, sq, X.pb[7], rstd, hF)

        prep(0)
        for tt_ in range(NT):
            t0 = tt_ * TT
            hT = hTs[tt_ % 2]
            if tt_ + 1 < NT:
                prep(tt_ + 1)
            for fc in range(24):
                ps = X.pb[n % 6]
                o = ofm[n % 4]
                n += 1
                c0 = 2048 + fc * 128
                for k in range(8):
                    kb.mm(ps[:, :], W16[:, k, c0:c0 + 128], hT[:, k, :], start=(k == 0), stop=(k == 7), r=[W16, hT], w=[ps])
                kb.cp("act" if fc % 2 else "dve", o[:], ps[:, :], r=[ps], w=[o])
                kb.st(X.xbcT[fc * 128:(fc + 1) * 128, t0:t0 + TT], o[:], o)
            for sub in range(4):
                s0 = t0 + sub * 128
                zs = zst[sub % 2]
                for q4 in range(4):
                    ps = X.pb[n % 6]
                    n += 1
                    for k in range(8):
                        kb.mm(ps[:, :], hT[:, k, sub * 128:(sub + 1) * 128], W16[:, k, q4 * 512:(q4 + 1) * 512], start=(k == 0), stop=(k == 7),
                              r=[W16, hT], w=[ps])
                    kb.cp("act" if q4 % 2 else "dve", zs[:, q4 * 512:(q4 + 1) * 512], ps[:, :], r=[ps], w=[zs])
                kb.st(X.z_tok[s0:s0 + 128, :], zs[:], zs)
                ps = X.pb[n % 6]
                n += 1
                for k in range(8):
                    kb.mm(ps[:, 0:32], hT[:, k, sub * 128:(sub + 1) * 128], W16[:, k, 5120:5152], start=(k == 0), stop=(k == 7),
                          r=[W16, hT], w=[ps])
                kb.tt("dve", d1[:], ps[:, 0:32], dtb[:], ALU.add, r=[ps, dtb], w=[d1])
                kb.act(d2[:], d1[:], AF.Abs, r=[d1], w=[d2])
                kb.act(d2[:], d2[:], AF.Exp, scale=-1.0, r=[d2], w=[d2])
                kb.act(d2[:], d2[:], AF.Ln, bias=onec[:, 0:1], r=[d2, onec], w=[d2])
                dd = d3[sub % 2]
                kb.stt("dve", dd[:], d1[:], 0.0, d2[:], ALU.max, ALU.add, r=[d1, d2], w=[dd])
                kb.st(X.dt_tok[s0:s0 + 128, :], dd[:], dd)
        P.barrier()


def phase_ssm_conv(X):
    kb, P, nc = X.kb, X.P, X.nc
    NA = 8
    with ExitStack() as es:
        cw = kb.sb(es, "c_cw", [128, 24, 4], F32)
        cb = kb.sb(es, "c_cb", [128, 24], F32)
        kb.ld(cw[:], X.conv_w_l, cw)
        kb.ld(cb[:], X.conv_b_l, cb)
        xp = [kb.sb(es, "c_xp%d" % i, [128, T + 3], F32) for i in range(2)]
        accs = [kb.sb(es, "c_acc%d" % i, [128, T], F32) for i in range(NA)]
        ob = kb.sb(es, "c_ob", [128, T], BF16)
        tst = [kb.sb(es, "c_tst%d" % i, [128, 512], F32) for i in range(4)]
        tsb = [kb.sb(es, "c_tsb%d" % i, [128, 512], BF16) for i in range(3)]
        for i in range(2):
            kb.memset("dve", xp[i][:, 0:3], 0.0, w=[xp[i]])
        nn = [0]

        def conv1(fc):
            x = xp[fc % 2]
            acc = accs[fc % NA]
            kb.ld(x[:, 3:T + 3], X.xbcT[fc * 128:(fc + 1) * 128, :], x)
            kb.act(acc[:], x[:, 0:T], AF.Identity, bias=cb[:, fc:fc + 1], scale=cw[:, fc, 0:1], r=[x, cb, cw], w=[acc])

        def conv2(fc):
            x = xp[fc % 2]
            acc = accs[fc % NA]
            for k in range(1, 4):
                kb.stt("dve", acc[:], x[:, k:k + T], cw[:, fc, k:k + 1], acc[:], ALU.mult, ALU.add, r=[x, cw, acc], w=[acc])
            kb.act(acc[:], acc[:], AF.Silu, r=[acc], w=[acc])
            if fc >= 16:
                kb.cp("pool", ob[:], acc[:], r=[acc], w=[ob])
                if fc < 20:
                    kb.st(X.BT[(fc - 16) * 128:(fc - 15) * 128, :], ob[:], ob)
                else:
                    kb.st(X.CT[(fc - 20) * 128:(fc - 19) * 128, :], ob[:], ob)

        def conv_range(f0, f1):
            conv1(f0)
            for fc in range(f0, f1):
                if fc + 1 < f1:
                    conv1(fc + 1)
                conv2(fc)

        def trans(grp):
            if grp >= 5:
                return
            ga = [accs[(4 * grp + j) % NA] for j in range(4)]
            for blk in range(T // 128):
                n = nn[0]
                nn[0] += 1
                ps = X.pb[n % 8]
                for j in range(4):
                    kb.tr(ps[:, j * 128:(j + 1) * 128], ga[j][:, blk * 128:(blk + 1) * 128], X.ident_f[:, :], r=[ga[j], X.ident_f], w=[ps])
                if grp < 4:
                    st_ = tst[n % 4]
                    kb.cp("act" if n % 2 else "dve", st_[:], ps[:, :], r=[ps], w=[st_])
                    kb.st(X.x_tok[blk * 128:(blk + 1) * 128, grp * 512:(grp + 1) * 512], st_[:], st_)
                else:
                    st_ = tsb[n % 3]
                    kb.cp("act" if n % 2 else "dve", st_[:], ps[:, :], r=[ps], w=[st_])
                    kb.st(X.B_tok[blk * 128:(blk + 1) * 128, :], st_[:], st_)

        conv_range(0, 4)
        for grp in range(6):
            if grp + 1 < 6:
                conv_range(4 * (grp + 1), 4 * (grp + 2))
            trans(grp)
        P.barrier()


def phase_ssm_scan(X):
    kb, P, nc = X.kb, X.P, X.nc
    cd = X.cd
    NCH = 2
    NCK = T // 64
    with ExitStack() as es:
        u64 = kb.sb(es, "m_u64", [64, 64], F32)
        tri = kb.sb(es, "m_tri", [64, 64], F32)
        cmask = kb.sb(es, "m_cmask", [64, 64], F32)
        ones_f = kb.sb(es, "m_onesf", [64, 128], F32)
        Arow = kb.sb(es, "m_Arow", [64, 32], F32)
        Dbc = kb.sb(es, "m_Dbc", [64, 2048], F32)
        kb.ld(u64[:], cd["u64"], u64)
        kb.ld(tri[:], cd["tri64"], tri)
        kb.ld(cmask[:], cd["cmask64"][:, 0:64], cmask)
        kb.memset("dve", ones_f[:], 1.0, w=[ones_f])
        kb.ld(Arow[:], X.a_log_rep, Arow)
        kb.act(Arow[:], Arow[:], AF.Exp, r=[Arow], w=[Arow])
        kb.ts("dve", Arow[:], Arow[:], -1.0, None, ALU.mult, r=[Arow], w=[Arow])
        kb.ld(Dbc[:], X.d_rep, Dbc)
        S32 = [kb.sb(es, "m_S32_%d" % g, [128, 512], F32) for g in range(4)]
        S16 = [kb.sb(es, "m_S16_%d" % g, [128, 512], BF16) for g in range(4)]
        for g in range(4):
            kb.memset("dve", S32[g][:], 0.0, w=[S32[g]])
            kb.memset("dve", S16[g][:], 0.0, w=[S16[g]])
        NB = 3
        x64 = [kb.sb(es, "m_x64_%d" % i, [64, NCH, 2048], F32) for i in range(NB)]
        B64 = [kb.sb(es, "m_B64_%d" % i, [64, NCH, 512], BF16) for i in range(NB)]
        BTt = [kb.sb(es, "m_BT_%d" % i, [128, 4, NCH * 64], BF16) for i in range(NB)]
        CTt = [kb.sb(es, "m_CT_%d" % i, [128, 4, NCH * 64], BF16) for i in range(NB)]
        dt64 = [kb.sb(es, "m_dt64_%d" % i, [64, NCH, 32], F32) for i in range(NB)]
        a_tok = [kb.sb(es, "m_atok%d" % i, [64, 32], F32) for i in range(2)]
        dte = [kb.sb(es, "m_dte%d" % i, [64, 32], F32) for i in range(2)]
        dfs = [kb.sb(es, "m_dfs%d" % i, [64, 32], F32) for i in range(2)]
        cdec = [kb.sb(es, "m_cdec%d" % i, [128, 32], F32) for i in range(2)]
        xdt = [kb.sb(es, "m_xdt%d" % i, [64, 2048], BF16) for i in range(2)]
        xdte = [kb.sb(es, "m_xdte%d" % i, [64, 2048], BF16) for i in range(2)]
        xdf = kb.sb(es, "m_xdf", [64, 2048], F32)
        xD = [kb.sb(es, "m_xD%d" % i, [64, 2048], F32) for i in range(2)]
        R = [kb.sb(es, "m_R%d" % i, [64, 512], F32) for i in range(4)]
        LT = [kb.sb(es, "m_LT%d" % i, [64, 512], F32) for i in range(4)]
        cbm = [kb.sb(es, "m_cbm%d" % i, [64, 64], F32) for i in range(8)]
        WT = [kb.sb(es, "m_WT%d" % i, [64, 512], BF16) for i in range(8)]
        yo = [kb.sb(es, "m_yo%d" % i, [64, 512], F32) for i in range(2)]
        yst = [kb.sb(es, "m_yst%d" % i, [64, 2048], F32) for i in range(2)]
        stmp = [kb.sb(es, "m_stmp%d" % i, [128, 512], F32) for i in range(2)]
        nb = [0]

        def npb():
            nb[0] += 1
            return X.pb[nb[0] % 8]

        def loads(tg):
            t0 = tg * 64 * NCH
            i = tg % NB
            kb.ld(x64[i][:], X.x_tok[t0:t0 + 64 * NCH, :].rearrange("(n s) c -> s n c", s=64), x64[i])
            kb.ld(B64[i][:], X.B_tok[t0:t0 + 64 * NCH, :].rearrange("(n s) c -> s n c", s=64), B64[i])
            kb.ld(BTt[i][:], X.BT[:, t0:t0 + 64 * NCH].rearrange("(g p) t -> p g t", p=128), BTt[i])
            kb.ld(CTt[i][:], X.CT[:, t0:t0 + 64 * NCH].rearrange("(g p) t -> p g t", p=128), CTt[i])
            kb.ld(dt64[i][:], X.dt_tok[t0:t0 + 64 * NCH, :].rearrange("(n s) c -> s n c", s=64), dt64[i])

        def stage_a(c):
            tg, ci = divmod(c, NCH)
            if ci == 0:
                loads(tg)
            i = tg % NB
            p = c % 2
            xx, dd, bt, ct = x64[i], dt64[i], BTt[i], CTt[i]
            at = a_tok[p]
            kb.tt("dve", at[:], dd[:, ci, :], Arow[:], ALU.mult, r=[dd, Arow], w=[at])
            pm = npb()
            kb.mm(pm[0:64, 0:32], u64[:, :], at[:, :], r=[u64, at], w=[pm])
            kb.mm(pm[0:64, 32:64], tri[:, :], at[:, :], r=[tri, at], w=[pm])
            kb.mm(pm[:, 64:96], ones_f[:, :], at[:, :], r=[ones_f, at], w=[pm])
            pcb = npb()
            for g in range(4):
                kb.mm(pcb[0:64, g * 64:(g + 1) * 64], bt[:, g, ci * 64:(ci + 1) * 64], ct[:, g, ci * 64:(ci + 1) * 64],
                      r=[bt, ct], w=[pcb])
            kb.act(dte[p][:], pm[0:64, 0:32], AF.Exp, r=[pm], w=[dte[p]])
            kb.act(dfs[p][:], pm[0:64, 32:64], AF.Exp, r=[pm], w=[dfs[p]])
            kb.act(cdec[p][:], pm[:, 64:96], AF.Exp, r=[pm], w=[cdec[p]])
            for g in range(4):
                kb.tt("dve", R[g][:].rearrange("k (h l) -> k h l", l=64), tri[:, :].unsqueeze(1).to_broadcast([64, 8, 64]),
                      at[:, 8 * g:8 * g + 8].unsqueeze(2).to_broadcast([64, 8, 64]), ALU.mult, r=[tri, at], w=[R[g]])
                pseg = npb()
                kb.mm(pseg[0:64, :], u64[:, :], R[g][:], r=[u64, R[g]], w=[pseg])
                kb.act(LT[g][:], pseg[0:64, :], AF.Exp, r=[pseg], w=[LT[g]])
            xv = xx[:, ci, :].rearrange("s (h p) -> s h p", p=64)
            kb.tt("dve", xdf[:].rearrange("s (h p) -> s h p", p=64), xv, dd[:, ci, :].unsqueeze(2).to_broadcast([64, 32, 64]),
                  ALU.mult, r=[xx, dd], w=[xdf])
            kb.cp("act", xdt[p][:], xdf[:], r=[xdf], w=[xdt[p]])
            kb.tt("dve", xdte[p][:].rearrange("s (h p) -> s h p", p=64), xdf[:].rearrange("s (h p) -> s h p", p=64),
                  dte[p][:, :].unsqueeze(2).to_broadcast([64, 32, 64]), ALU.mult, r=[xdf, dte[p]], w=[xdte[p]])
            kb.tt("pool", xD[p][:], xx[:, ci, :], Dbc[:], ALU.mult, r=[xx, Dbc], w=[xD[p]])
            for g in range(4):
                cb_ = cbm[p * 4 + g]
                wt_ = WT[p * 4 + g]
                kb.tt("dve", cb_[:], pcb[0:64, g * 64:(g + 1) * 64], cmask[:], ALU.mult, r=[pcb, cmask], w=[cb_])
                kb.tt("dve", wt_[:].rearrange("s (h l) -> s h l", l=64), LT[g][:].rearrange("s (h l) -> s h l", l=64),
                      cb_[:, :].unsqueeze(1).to_broadcast([64, 8, 64]), ALU.mult, r=[LT[g], cb_], w=[wt_])

        def stage_b(c):
            tg, ci = divmod(c, NCH)
            i = tg % NB
            p = c % 2
            bb, ct = B64[i], CTt[i]
            ys = yst[c % 2]
            pys, pyos, pSs = [], [], []
            for g in range(4):
                wt_ = WT[p * 4 + g]
                py = npb()
                for h in range(8):
                    hg = 8 * g + h
                    kb.mm(py[0:64, h * 64:(h + 1) * 64], wt_[0:64, h * 64:(h + 1) * 64], xdt[p][0:64, hg * 64:(hg + 1) * 64],
                          r=[wt_, xdt[p]], w=[py])
                pyo = npb()
                kb.mm(pyo[0:64, :], ct[:, g, ci * 64:(ci + 1) * 64], S16[g][:, :], r=[ct, S16[g]], w=[pyo])
                pS = npb()
                kb.mm(pS[:, :], bb[0:64, ci, g * 128:(g + 1) * 128], xdte[p][0:64, g * 512:(g + 1) * 512], r=[bb, xdte[p]], w=[pS])
                st_ = stmp[g % 2]
                kb.tt("dve", st_[:].rearrange("n (h p) -> n h p", p=64), S32[g][:].rearrange("n (h p) -> n h p", p=64),
                      cdec[p][:, 8 * g:8 * g + 8].unsqueeze(2).to_broadcast([128, 8, 64]), ALU.mult, r=[S32[g], cdec[p]], w=[st_])
                kb.tt("dve", S32[g][:], st_[:], pS[:, :], ALU.add, r=[st_, pS], w=[S32[g]])
                kb.cp("act", S16[g][:], S32[g][:], r=[S32[g]], w=[S16[g]])
                yo_ = yo[g % 2]
                kb.tt("dve", yo_[:].rearrange("l (h p) -> l h p", p=64), pyo[0:64, :].rearrange("l (h p) -> l h p", p=64),
                      dfs[p][:, 8 * g:8 * g + 8].unsqueeze(2).to_broadcast([64, 8, 64]), ALU.mult, r=[pyo, dfs[p]], w=[yo_])
                kb.tt("dve", yo_[:], yo_[:], py[0:64, :], ALU.add, r=[yo_, py], w=[yo_])
                kb.tt("pool", ys[:, g * 512:(g + 1) * 512], yo_[:], xD[p][:, g * 512:(g + 1) * 512], ALU.add, r=[yo_, xD[p]], w=[ys])
            kb.st(X.y_tok[c * 64:(c + 1) * 64, :], ys[:], ys)

        stage_a(0)
        for c in range(NCK):
            if c + 1 < NCK:
                stage_a(c + 1)
            stage_b(c)
        P.barrier()


def phase_ssm_out(X):
    kb, P, nc = X.kb, X.P, X.nc
    with ExitStack() as es:
        Wo = kb.sb(es, "so_wo", [128, 16, 1024], BF16)
        with ExitStack() as es2:
            load_cast(X, es2, "sowo", X.ssm_w_out, 2048, 1024, Wo, chunk=256)
            P.barrier()
        nwb = kb.sb(es, "so_nwb", [128, 2048], F32)
        kb.ld(nwb[:], X.gate_norm_rep, nwb)
        yt = [kb.sb(es, "so_y%d" % i, [128, 2048], F32) for i in range(3)]
        zt = [kb.sb(es, "so_z%d" % i, [128, 2048], F32) for i in range(3)]
        junk = kb.sb(es, "so_junk", [128, 4, 512], BF16)
        sss = [kb.sb(es, "so_ss%d" % i, [128, 4], F32) for i in range(2)]
        yns = [kb.sb(es, "so_yn%d" % i, [128, 2048], F32) for i in range(2)]
        yTs = [kb.sb(es, "so_yT%d" % i, [128, 16, TT], BF16) for i in range(2)]
        xt = [kb.sb(es, "so_xt%d" % i, [128, 8, TT], F32) for i in range(2)]
        xo = [kb.sb(es, "so_xo%d" % i, [128, TT], F32) for i in range(3)]
        xsrc = X.x2T.rearrange("(k p) t -> p k t", p=128)
        nn = [0]

        def prep_sub(si):
            tt_, sub = divmod(si, 4)
            if sub == 0:
                x = xt[tt_ % 2]
                for k in range(8):
                    kb.ld(x[:, k, :], xsrc[:, k, tt_ * TT:(tt_ + 1) * TT], x)
            s0 = si * 128
            y = yt[si % 3]; z = zt[si % 3]; ss = sss[si % 2]; yn = yns[si % 2]
            kb.ld(y[:], X.y_tok[s0:s0 + 128, :], y)
            kb.ld(z[:], X.z_tok[s0:s0 + 128, :], z)
            kb.act(z[:], z[:], AF.Silu, r=[z], w=[z])
            kb.tt("dve", y[:], y[:], z[:], ALU.mult, r=[y, z], w=[y])
            for g in range(4):
                kb.act(junk[:, g, :], y[:, g * 512:(g + 1) * 512], AF.Square, accum=ss[:, g:g + 1], r=[y], w=[ss])
            kb.act(ss[:], ss[:], AF.Ln, bias=X.epsc[:, 0:1], scale=1.0 / 512.0, r=[ss, X.epsc], w=[ss])
            kb.act(ss[:], ss[:], AF.Exp, scale=-0.5, r=[ss], w=[ss])
            for g in range(4):
                kb.stt("dve", yn[:, g * 512:(g + 1) * 512], y[:, g * 512:(g + 1) * 512], ss[:, g:g + 1],
                       nwb[:, g * 512:(g + 1) * 512], ALU.mult, ALU.mult, r=[y, ss, nwb], w=[yn])

        def tr_sub(si):
            tt_, sub = divmod(si, 4)
            yn = yns[si % 2]
            yT = yTs[tt_ % 2]
            for q4 in range(4):
                n = nn[0]; nn[0] += 1
                ps = X.pb[n % 8]
                for i in range(4):
                    kc = q4 * 4 + i
                    kb.tr(ps[:, i * 128:(i + 1) * 128], yn[:, kc * 128:(kc + 1) * 128], X.ident_f[:, :], r=[yn, X.ident_f], w=[ps])
                kb.cp("act" if q4 % 2 else "dve", yT[:, q4 * 4:(q4 + 1) * 4, sub * 128:(sub + 1) * 128],
                      ps[:, :].rearrange("p (a b) -> p a b", a=4), r=[ps], w=[yT])

        def outproj(tt_):
            t0 = tt_ * TT
            x = xt[tt_ % 2]
            yT = yTs[tt_ % 2]
            for dc in range(8):
                n = nn[0]; nn[0] += 1
                ps = X.pb[n % 8]
                o = xo[n % 3]
                for kc in range(16):
                    kb.mm(ps[:, :], Wo[:, kc, dc * 128:(dc + 1) * 128], yT[:, kc, :], start=(kc == 0), stop=(kc == 15), r=[Wo, yT], w=[ps])
                kb.stt("dve", o[:], ps[:, :], X.modt[:, 2, 16 + dc:17 + dc], x[:, dc, :], ALU.mult, ALU.add, r=[ps, X.modt, x], w=[o])
                kb.st(X.x3T[dc * 128:(dc + 1) * 128, t0:t0 + TT], o[:], o)

        NS = T // 128
        prep_sub(0)
        for si in range(NS):
            if si + 1 < NS:
                prep_sub(si + 1)
            tr_sub(si)
            if si % 4 == 3:
                outproj(si // 4)
        P.barrier()


def phase_moe(X):
    kb, P, nc = X.kb, X.P, X.nc
    NE, FE = 8, 3584
    FS = 256
    NFS = FE // FS
    HALF = 2048
    with ExitStack() as es:
        gate_all = kb.sb(es, "e_gate", [128, 32, 8], F32)
        gm_bc = kb.sb(es, "e_gmbc", [128, 1024], F32)
        fw_bc = kb.sb(es, "e_fwbc", [128, 1024], F32)
        kb.ld(fw_bc[:], X.final_w_rep, fw_bc)
        with ExitStack() as es2:
            dg = kb.sb(es2, "e_dg", [128, 128], F32)
            onesf = kb.sb(es2, "e_onesf", [128, 128], F32)
            kb.memset("dve", onesf[:], 1.0, w=[onesf])
            for c in range(8):
                ps = X.pb[c // 4]
                kb.ts("dve", dg[:], X.ident_f[:, :], X.modt[:, 3, 16 + c:17 + c], None, ALU.mult, r=[X.ident_f, X.modt], w=[dg])
                kb.mm(ps[:, (c % 4) * 128:(c % 4 + 1) * 128], onesf[:, :], dg[:, :], r=[onesf, dg], w=[ps])
                if c % 4 == 3:
                    kb.cp("dve", gm_bc[:, (c // 4) * 512:(c // 4 + 1) * 512], ps[:, :], r=[ps], w=[gm_bc])
            P.barrier()
        with ExitStack() as es2:
            r32 = kb.sb(es2, "e_r32", [128, 8, 8], F32)
            kb.ld(r32[:], X.router.rearrange("(k p) e -> p k e", p=128), r32)
            xt = [kb.sb(es2, "e_xt%d" % i, [128, 8, TT], F32) for i in range(2)]
            sq = kb.sb(es2, "e_sq", [128, 8, TT], BF16)
            hF = kb.sb(es2, "e_hF", [128, 8, TT], F32)
            hT = [kb.sb(es2, "e_hT%d" % i, [128, 8, TT], BF16) for i in range(2)]
            h32 = kb.sb(es2, "e_h32", [128, 8, TT], F32)
            rstd = kb.sb(es2, "e_rstd", [128, TT], F32)
            lg_all = kb.sb(es2, "e_lgall", [128, 32, 8], F32)
            v1 = kb.sb(es2, "e_v1", [128, 32], F32)
            v2 = kb.sb(es2, "e_v2", [128, 32], F32)
            eqm = kb.sb(es2, "e_eqm", [128, 32, 8], F32)
            exa = kb.sb(es2, "e_exa", [128, 32, 8], F32)
            t8 = kb.sb(es2, "e_t8", [128, 8], F32)
            nv1 = kb.sb(es2, "e_nv1", [128, 1], F32)
            msk = kb.sb(es2, "e_msk", [128, 8], F32)
            ex = kb.sb(es2, "e_ex", [128, 8], F32)
            den = kb.sb(es2, "e_den", [128, 1], F32)
            xsrc = X.x3T.rearrange("(k p) t -> p k t", p=128)
            hdst = X.hmT.rearrange("(k p) t -> p k t", p=128)
            for tt_ in range(NT):
                t0 = tt_ * TT
                x = xt[tt_ % 2]
                h_ = hT[tt_ % 2]
                for k in range(8):
                    kb.ld(x[:, k, :], xsrc[:, k, t0:t0 + TT], x)
                norm_modulate(X, x, h_, 3, sq, X.pb[7], rstd, hF, hT32=h32)
                for k in range(8):
                    kb.st(hdst[:, k, t0:t0 + TT], h_[:, k, :], h_)
                for sub in range(4):
                    si = tt_ * 4 + sub
                    ps = X.pb[sub % 4]
                    for k in range(8):
                        kb.mm(ps[:, 0:8], h32[:, k, sub * 128:(sub + 1) * 128], r32[:, k, :], start=(k == 0), stop=(k == 7),
                              r=[h32, r32], w=[ps])
                    kb.cp("dve", lg_all[:, si, :], ps[:, 0:8], r=[ps], w=[lg_all])
            def b8(t):
                return t[:, :].unsqueeze(2).to_broadcast([128, 32, 8])
            P.op("dve", (lambda o, i: (lambda e: e.tensor_reduce(o, i, AX.X, ALU.max)))(v1[:], lg_all[:]), [lg_all], [v1])
            kb.tt("dve", eqm[:], lg_all[:], b8(v1), ALU.is_equal, r=[lg_all, v1], w=[eqm])
            kb.stt("dve", eqm[:], eqm[:], -1e30, lg_all[:], ALU.mult, ALU.add, r=[eqm, lg_all], w=[eqm])
            P.op("dve", (lambda o, i: (lambda e: e.tensor_reduce(o, i, AX.X, ALU.max)))(v2[:], eqm[:]), [eqm], [v2])
            kb.tt("dve", eqm[:], lg_all[:], b8(v2), ALU.is_ge, r=[lg_all, v2], w=[eqm])
            kb.tt("dve", exa[:], lg_all[:], b8(v1), ALU.subtract, r=[lg_all, v1], w=[exa])
            kb.act(exa[:], exa[:], AF.Exp, r=[exa], w=[exa])
            kb.tt("dve", exa[:], exa[:], eqm[:], ALU.mult, r=[exa, eqm], w=[exa])
            P.op("dve", (lambda o, i: (lambda e: e.tensor_reduce(o, i, AX.X, ALU.add)))(v2[:], exa[:]), [exa], [v2])
            kb.recip(v2[:], v2[:], r=[v2], w=[v2])
            kb.tt("dve", gate_all[:], exa[:], b8(v2), ALU.mult, r=[exa, v2], w=[gate_all])
            P.barrier()
        if X.dbg_stop == "gate":
            dgt = nc.dram_tensor("gate_d", [128, 256], F32, kind="ExternalOutput").ap()
            kb.st(dgt, gate_all[:].rearrange("p a b -> p (a b)"), gate_all)
            P.barrier()
            return
        acc = kb.sb(es, "e_acc", [128, 16, 1024], F32)
        hh = kb.sb(es, "e_hh", [128, 8, HALF], BF16)
        sgu = [kb.sb(es, "e_sgu%d" % i, [128, 8, FS], F32) for i in range(2)]
        swd = [kb.sb(es, "e_swd%d" % i, [128, 2, 1024], F32) for i in range(2)]
        wgu = [kb.sb(es, "e_wgu%d" % i, [128, 8, 2 * FS], BF16) for i in range(2)]
        wd = [kb.sb(es, "e_wd%d" % i, [128, 2, 1024], BF16) for i in range(2)]
        sg = [kb.sb(es, "e_sg%d" % i, [128, TT], F32) for i in range(2)]
        a16 = [kb.sb(es, "e_a16_%d" % i, [128, TT], BF16) for i in range(4)]
        x3s = kb.sb(es, "e_x3s", [128, 8, 128], F32)
        ytmp = kb.sb(es, "e_ytmp", [128, 1024], F32)
        junk = kb.sb(es, "e_junk", [128, 1024], BF16)
        ssq = kb.sb(es, "e_ssq", [128, 1], F32)
        ost = [kb.sb(es, "e_ost%d" % i, [128, 1024], F32) for i in range(2)]
        hsrc = X.hmT.rearrange("(k p) t -> p k t", p=128)
        x3src = X.x3T.rearrange("(k p) t -> p k t", p=128)
        n = 0
        wi = 0
        for half in range(2):
            h0 = half * HALF
            for k in range(8):
                kb.ld(hh[:, k, :], hsrc[:, k, h0:h0 + HALF], hh)
            for e in range(NE):
                for fs in range(NFS):
                    wg = wgu[wi % 2]; wdn = wd[wi % 2]
                    for part in range(2):
                        sst = sgu[part]
                        c0 = part * FE + fs * FS
                        kb.ld(sst[:], X.moe_w_gu[e].rearrange("(k p) m -> p k m", p=128)[:, :, c0:c0 + FS], sst)
                        kb.cp("pool", wg[:, :, part * FS:(part + 1) * FS], sst[:], r=[sst], w=[wg])
                    sd = swd[wi % 2]
                    kb.ld(sd[:], X.moe_w_down[e][fs * FS:(fs + 1) * FS, :].rearrange("(c p) d -> p c d", p=128), sd)
                    kb.cp("pool", wdn[:], sd[:], r=[sd], w=[wdn])
                    wi += 1
                    first = (e == 0 and fs == 0)
                    for tile in range(HALF // TT):
                        tl0 = tile * TT
                        aa = []
                        for c in range(2):
                            pg = X.pb[n % 8]; n += 1
                            pu = X.pb[n % 8]; n += 1
                            for k in range(8):
                                kb.mm(pg[:, :], wg[:, k, c * 128:(c + 1) * 128], hh[:, k, tl0:tl0 + TT], start=(k == 0), stop=(k == 7),
                                      r=[wg, hh], w=[pg])
                            for k in range(8):
                                kb.mm(pu[:, :], wg[:, k, FS + c * 128:FS + (c + 1) * 128], hh[:, k, tl0:tl0 + TT], start=(k == 0), stop=(k == 7),
                                      r=[wg, hh], w=[pu])
                            s_ = sg[c]
                            a_ = a16[(tile % 2) * 2 + c]
                            kb.act(s_[:], pg[:, :], AF.Silu, r=[pg], w=[s_])
                            kb.tt("dve", a_[:], s_[:], pu[:, :], ALU.mult, r=[s_, pu], w=[a_])
                            aa.append(a_)
                        for sub in range(4):
                            sl = tile * 4 + sub
                            sgl = half * 16 + sl
                            for dh in range(2):
                                po = X.pb[n % 8]; n += 1
                                for c in range(2):
                                    kb.mm(po[:, :], aa[c][:, sub * 128:(sub + 1) * 128], wdn[:, c, dh * 512:(dh + 1) * 512],
                                          start=(c == 0), stop=(c == 1), r=[aa[c], wdn], w=[po])
                                if first:
                                    kb.ts("dve", acc[:, sl, dh * 512:(dh + 1) * 512], po[:, :], gate_all[:, sgl, e:e + 1], None, ALU.mult,
                                          r=[po, gate_all], w=[acc])
                                else:
                                    kb.stt("dve", acc[:, sl, dh * 512:(dh + 1) * 512], po[:, :], gate_all[:, sgl, e:e + 1],
                                           acc[:, sl, dh * 512:(dh + 1) * 512], ALU.mult, ALU.add, r=[po, gate_all, acc], w=[acc])
            for sl in range(16):
                s0 = h0 + sl * 128
                kb.ld(x3s[:], x3src[:, :, s0:s0 + 128], x3s)
                px = [X.pb[n % 8], X.pb[(n + 1) % 8]]; n += 2
                for c in range(8):
                    kb.tr(px[c // 4][:, (c % 4) * 128:(c % 4 + 1) * 128], x3s[:, c, :], X.ident_f[:, :], r=[x3s, X.ident_f], w=[px[c // 4]])
                kb.tt("dve", ytmp[:], acc[:, sl, :], gm_bc[:], ALU.mult, r=[acc, gm_bc], w=[ytmp])
                for dh in range(2):
                    kb.tt("dve", ytmp[:, dh * 512:(dh + 1) * 512], ytmp[:, dh * 512:(dh + 1) * 512], px[dh][:, :], ALU.add,
                          r=[ytmp, px[dh]], w=[ytmp])
                kb.act(junk[:], ytmp[:], AF.Square, accum=ssq[:, 0:1], r=[ytmp], w=[junk, ssq])
                kb.act(ssq[:], ssq[:], AF.Ln, bias=X.epsc[:, 0:1], scale=1.0 / D, r=[ssq, X.epsc], w=[ssq])
                kb.act(ssq[:], ssq[:], AF.Exp, scale=-0.5, r=[ssq], w=[ssq])
                o = ost[sl % 2]
                kb.stt("dve", o[:], ytmp[:], ssq[:, 0:1], fw_bc[:], ALU.mult, ALU.mult, r=[ytmp, ssq, fw_bc], w=[o])
                kb.st(X.out[s0:s0 + 128, :], o[:], o)
        P.barrier()


def col_layout(v, nchunk):
    return np.ascontiguousarray(np.asarray(v, np.float32).reshape(nchunk, 128).T)


def make_in_maps(inputs, cores):
    consts = make_consts()
    mods = [("hyb_mod_w", "hyb_mod_b", "hyb_norm"), ("dense_mod_w", "dense_mod_b", "dense_norm"),
            ("ssm_mod_w", "ssm_mod_b", "ssm_norm"), ("moe_mod_w", "moe_mod_b", "moe_norm")]
    shared = {}
    for k, v in consts.items():
        shared["c_" + k] = v
    for j, (w, b, n) in enumerate(mods):
        shared["mod_w%d" % j] = np.ascontiguousarray(inputs[w][0])
        shared["mod_b%d" % j] = col_layout(inputs[b][0], 24)
        shared["norm_w%d" % j] = col_layout(inputs[n][0], 8)
    shared["hyb_w_in"] = np.ascontiguousarray(inputs["hyb_w_in"][0])
    shared["gk_up"] = np.ascontiguousarray(inputs["gla_gk_up"][0])
    shared["gk_bias_col"] = col_layout(inputs["gla_gk_bias"][0], 2)
    shared["gk_bias_rep"] = np.ascontiguousarray(np.broadcast_to(inputs["gla_gk_bias"][0][None, :], (64, 256)))
    shared["gla_norm"] = np.ascontiguousarray(inputs["gla_out_norm"][0][:, None])
    shared["hyb_w_out"] = np.ascontiguousarray(inputs["hyb_w_out"][0])
    shared["dense_w_gu"] = np.ascontiguousarray(inputs["dense_w_gu"][0])
    shared["dense_w_down"] = np.ascontiguousarray(inputs["dense_w_down"][0])
    shared["ssm_w_in"] = np.ascontiguousarray(inputs["ssm_w_in"][0])
    shared["ssm_w_out"] = np.ascontiguousarray(inputs["ssm_w_out"][0])
    cw = inputs["ssm_conv_w"][0]
    shared["conv_w_l"] = np.ascontiguousarray(cw.reshape(4, 24, 128).transpose(2, 1, 0))
    shared["conv_b_l"] = col_layout(inputs["ssm_conv_b"][0], 24)
    shared["dt_bias_rep"] = np.ascontiguousarray(np.broadcast_to(inputs["ssm_dt_bias"][0][None, :], (128, 32)))
    shared["a_log_rep"] = np.ascontiguousarray(np.broadcast_to(inputs["ssm_a_log"][0][None, :], (64, 32)))
    shared["d_rep"] = np.ascontiguousarray(np.broadcast_to(np.repeat(inputs["ssm_d"][0], 64)[None, :], (64, 2048)))
    shared["gate_norm_rep"] = np.ascontiguousarray(np.broadcast_to(inputs["ssm_gate_norm"][0][None, :], (128, 2048)))
    shared["router"] = np.ascontiguousarray(inputs["moe_router"][0])
    shared["moe_w_gu"] = np.ascontiguousarray(inputs["moe_w_gu"][0])
    shared["moe_w_down"] = np.ascontiguousarray(inputs["moe_w_down"][0])
    shared["final_w_rep"] = np.ascontiguousarray(np.broadcast_to(inputs["final_norm"][None, :], (128, D)))
    shared["cmp_peT"] = np.ascontiguousarray(inputs["nsa_cmp_pe"][0].transpose(0, 2, 1))
    shared["cmp_w1"] = np.ascontiguousarray(inputs["nsa_cmp_w1"][0])
    shared["cmp_w2"] = np.ascontiguousarray(inputs["nsa_cmp_w2"][0])
    maps = []
    for b in cores:
        m = dict(shared)
        m["xT"] = np.ascontiguousarray(inputs["x"][b].T)
        m["cvec"] = col_layout(inputs["c"][b], 8)
        maps.append(m)
    return maps


ALL_PHASES = ("adaln", "l0proj", "gla", "nsa", "l0out", "dense", "ssm_in", "ssm_conv", "ssm_scan", "ssm_out", "moe")
_CACHE = {}


def kernel(**inputs):
    inputs = {k: np.asarray(v) for k, v in inputs.items()}
    n = 8
    if "prog" not in _CACHE:
        _CACHE["prog"] = build_program(set(ALL_PHASES))
    nc, ext_in = _CACHE["prog"]
    maps = make_in_maps(inputs, list(range(n)))
    in_maps = [{k: v for k, v in m.items() if k in ext_in} for m in maps]
    res = run_bass_kernel_spmd(nc, in_maps, core_ids=list(range(n)))
    out = np.stack([np.asarray(res.results[i]["out"], dtype=np.float32) for i in range(n)], axis=0)
    return out
```

```python
import numpy as np
import ml_dtypes
import concourse.bass as bass
import concourse.mybir as mybir
from concourse.bass_utils import run_bass_kernel_spmd
from contextlib import ExitStack

F32 = mybir.dt.float32
BF16 = mybir.dt.bfloat16
ALU = mybir.AluOpType
AF = mybir.ActivationFunctionType
AX = mybir.AxisListType

T = 4096
D = 1024
NT = 8
TT = 512
EPS = 1e-6


class Buf:
    __slots__ = ("name", "w", "r", "dsem", "dcnt", "excl", "rg")

    def __init__(self, name):
        self.name = name
        self.rg = None
        self.excl = False
        self.w = None
        self.r = {}
        self.dsem = None
        self.dcnt = 0


class Tl:
    __slots__ = ("t", "b")

    def __init__(self, t, name):
        self.t = t
        self.b = Buf(name)

    def __getitem__(self, k):
        return self.t[k]


class Prog:
    CENG = ["pe", "act", "dve", "pool"]
    ALLQ = ["pe", "act", "dve", "pool", "sp"]

    def __init__(self, nc, es):
        self.nc = nc
        self.es = es
        self.q = {e: [] for e in self.ALLQ}
        self.cnt = {e: 0 for e in self.CENG}
        self.sems = {}
        for e in self.CENG:
            self.sems["E" + e] = es.enter_context(nc.semaphore("sem_" + e))
        self.known = {e: {} for e in self.ALLQ}
        self.ndsem = 0
        self.totals = {}
        self.free_dsems = []
        self.dma_bufs = []

    def _collect(self, eng, reads, writes, rg=None):
        deps = {}
        force = set()
        if eng == "pe":
            for b in writes:
                if b.w is not None and b.w[2] == "pe" and b.rg != rg:
                    force.add(b.w[0])

        def add(key, val, src):
            if deps.get(key, (0,))[0] < val:
                deps[key] = (val, src)
        for b in reads:
            if b.w is not None:
                add(*b.w)
            if b.excl:
                for key, (val, src) in b.r.items():
                    if src != eng:
                        add(key, val, src)
        for b in writes:
            if b.w is not None:
                add(*b.w)
            for key, (val, src) in b.r.items():
                if src == eng and eng != "pool":
                    continue
                add(key, val, src)
        waits = []
        kn = self.known[eng]
        for key, (val, src) in deps.items():
            if src == eng and eng == "pe" and key not in force:
                continue
            if kn.get(key, 0) >= val:
                continue
            kn[key] = val
            waits.append((key, val))
        return waits

    def op(self, eng, fn, reads=(), writes=(), rg=None):
        reads = [x.b if isinstance(x, Tl) else x for x in reads]
        writes = [x.b if isinstance(x, Tl) else x for x in writes]
        waits = self._collect(eng, reads, writes, rg)
        if eng == "pe":
            for b in writes:
                b.rg = rg
        self.cnt[eng] += 1
        key = "E" + eng
        n = self.cnt[eng]
        self.totals[key] = n
        self.q[eng].append((waits, fn, (key, 1)))
        for b in reads:
            b.r[key] = (n, eng)
        for b in writes:
            b.w = (key, n, eng)
            b.r = {}

    def _dsem(self, b):
        if b.dsem is None:
            if self.free_dsems:
                b.dsem = self.free_dsems.pop()
                b.dcnt = self.totals.get(b.dsem, 0)
            else:
                b.dsem = "D%d" % self.ndsem
                self.sems[b.dsem] = self.es.enter_context(self.nc.semaphore("dsem%d" % self.ndsem))
                self.ndsem += 1
                b.dcnt = 0
            self.dma_bufs.append(b)
        return b.dsem

    def load(self, fn, dst, q="sp"):
        dst = dst.b if isinstance(dst, Tl) else dst
        waits = self._collect(q, (), (dst,))
        key = self._dsem(dst)
        dst.dcnt += 16
        self.totals[key] = dst.dcnt
        self.q[q].append((waits, fn, (key, 16)))
        dst.w = (key, dst.dcnt, "dma")
        dst.r = {}

    def store(self, fn, src, q="sp"):
        src = src.b if isinstance(src, Tl) else src
        waits = self._collect(q, (src,), ())
        key = self._dsem(src)
        src.dcnt += 16
        self.totals[key] = src.dcnt
        self.q[q].append((waits, fn, (key, 16)))
        src.r[key] = (src.dcnt, "dma")

    def barrier(self):
        for e in self.ALLQ:
            waits = []
            kn = self.known[e]
            for key, tot in self.totals.items():
                if kn.get(key, 0) >= tot:
                    continue
                kn[key] = tot
                waits.append((key, tot))
            if waits:
                self.q[e].append((waits, None, None))
        for b in self.dma_bufs:
            self.free_dsems.append(b.dsem)
            b.dsem = None
        self.dma_bufs = []

    def replay(self):
        nc = self.nc
        sems = self.sems
        qs = self.q

        def run(name, e):
            for waits, fn, inc in qs[name]:
                for key, val in waits:
                    e.wait_ge(sems[key], val)
                if fn is not None:
                    ins = fn(e)
                    ins.then_inc(sems[inc[0]], inc[1])

        with nc.Block() as block:
            @block.tensor
            def _(e):
                run("pe", e)

            @block.scalar
            def _(e):
                run("act", e)

            @block.vector
            def _(e):
                run("dve", e)

            @block.gpsimd
            def _(e):
                run("pool", e)

            @block.sync
            def _(e):
                run("sp", e)


class KB:
    def __init__(self, nc, P):
        self.nc = nc
        self.P = P
        self.rr = 0

    def sb(self, es, name, shape, dt):
        t = es.enter_context(self.nc.sbuf_tensor(name, list(shape), dt))
        return Tl(t, name)

    def mm(self, out, lhsT, rhs, start=True, stop=True, r=(), w=(), rg=0):
        self.P.op("pe", lambda e: e.matmul(out, lhsT, rhs, start=start, stop=stop), r, w, rg=rg)

    def tr(self, out, in_, ident, r=(), w=()):
        self.P.op("pe", lambda e: e.transpose(out, in_, ident), r, w)

    def act(self, out, in_, func, bias=None, scale=None, accum=None, r=(), w=()):
        kw = {}
        if bias is not None:
            kw["bias"] = bias
        if scale is not None:
            kw["scale"] = scale
        if accum is not None:
            kw["accum_out"] = accum
        self.P.op("act", lambda e: e.activation(out, in_, func, **kw), r, w)

    def cp(self, eng, out, in_, r=(), w=()):
        if eng == "act":
            self.P.op("act", lambda e: e.copy(out, in_), r, w)
        else:
            self.P.op(eng, lambda e: e.tensor_copy(out, in_), r, w)

    def tt(self, eng, out, a, b, op, r=(), w=()):
        self.P.op(eng, lambda e: e.tensor_tensor(out, a, b, op), r, w)

    def ts(self, eng, out, a, s1, s2, op0, op1=None, r=(), w=()):
        if op1 is None:
            self.P.op(eng, lambda e: e.tensor_scalar(out, a, s1, None, op0), r, w)
        else:
            self.P.op(eng, lambda e: e.tensor_scalar(out, a, s1, s2, op0, op1), r, w)

    def stt(self, eng, out, a, s, b, op0, op1, r=(), w=()):
        self.P.op(eng, lambda e: e.scalar_tensor_tensor(out, a, s, b, op0, op1), r, w)

    def memset(self, eng, out, v, w=()):
        self.P.op(eng, lambda e: e.memset(out, v), (), w)

    def recip(self, out, in_, r=(), w=()):
        self.P.op("dve", lambda e: e.reciprocal(out, in_), r, w)

    def ld(self, out, in_, dst, q="sp"):
        self.P.load(lambda e: e.dma_start(out=out, in_=in_), dst, q)

    def st(self, out, in_, src, q="sp"):
        self.P.store(lambda e: e.dma_start(out=out, in_=in_), src, q)

    def alt(self):
        self.rr += 1
        return ("dve", "act")[self.rr % 2]


def make_consts():
    c = {}
    c["ident_f"] = np.eye(128, dtype=np.float32)
    c["ident_b"] = np.eye(128, dtype=np.float32).astype(ml_dtypes.bfloat16)
    c["ones_b"] = np.ones((128, 128), np.float32).astype(ml_dtypes.bfloat16)
    rm = np.ones((128, TT), np.float32)
    rm[:, ::64] = 0.0
    c["resetm"] = rm
    sp_, s_ = np.meshgrid(np.arange(64), np.arange(64), indexing="ij")
    c["u64"] = (sp_ > s_).astype(np.float32)
    c["tri64"] = (sp_ <= s_).astype(np.float32)
    m = (sp_ <= s_).astype(np.float32)
    c["cmask64"] = np.tile(m, (1, 8)).astype(np.float32)
    bf = ml_dtypes.bfloat16
    t = np.arange(T)
    slopes = 2.0 ** (-8.0 * np.arange(1, 9) / 8)
    qaug = np.zeros((8, 4, T), np.float32)
    for h in range(8):
        qaug[h, 0] = -slopes[h] * 64.0 * (t // 64)
        qaug[h, 1] = -slopes[h] * (t % 64)
        qaug[h, 2] = slopes[h]
        qaug[h, 3] = slopes[h]
    c["qaug"] = qaug.astype(bf)
    kaug = np.zeros((4, T), np.float32)
    kaug[0] = 1.0
    kaug[1] = 1.0
    kaug[2] = 64.0 * (t // 64)
    kaug[3] = t % 64
    c["kaug"] = kaug.astype(bf)
    n = np.arange(256)
    kaugc = np.zeros((4, 256), np.float32)
    kaugc[0] = 1.0
    kaugc[1] = 1.0
    kaugc[2] = 16.0 * n
    kaugc[3] = 15.5
    kaugc[:, 255] = 0.0
    c["kaugc"] = kaugc.astype(bf)
    c["esel"] = (t[None, :] // 64 == np.arange(64)[:, None]).astype(np.float32).astype(bf)
    p = np.arange(128)[:, None]
    f = np.arange(512)[None, :]
    winm = np.zeros((8, 128, 512), np.float32)
    for i in range(8):
        off = -512 + 128 * i
        dd = f - p - off
        winm[i] = (((dd >= 0) & (dd < 512)).astype(np.float32) - 1.0) * 30000.0
    c["winm"] = np.ascontiguousarray(winm.transpose(1, 0, 2)).astype(bf)
    cm = np.zeros((8, 128, 2, 512), np.float32)
    for qt in range(8):
        for ck in range(2):
            nn = 128 * ck + p
            cm[qt, :, ck, :] = (((16 * nn + 31 <= 512 * qt + f) & (nn < 255)).astype(np.float32) - 1.0) * 30000.0
    c["cmpm"] = cm.astype(bf)
    ovl = np.zeros((128, 2, 65), np.float32)
    for ck in range(2):
        nn = 128 * ck + np.arange(128)
        j = np.arange(64)
        o = ((16 * nn[:, None] < 64 * j[None, :] + 64) & (16 * nn[:, None] + 31 >= 64 * j[None, :])).astype(np.float32)
        o[nn >= 255] = 0.0
        ovl[:, ck, 0:64] = o
        ovl[:, ck, 64] = (nn < 255).astype(np.float32)
    c["ovl"] = ovl.astype(bf)
    M1 = np.zeros((128, 32, 64), np.float32)
    M2 = np.zeros((128, 32, 64), np.float32)
    for tile in range(32):
        tt_ = 128 * tile + np.arange(128)
        blk = tt_ // 64
        j = np.arange(64)[None, :]
        forced = (j == 0) | (j == blk[:, None]) | (j == blk[:, None] - 1)
        future = j > blk[:, None]
        M1[:, tile, :] = (~(forced | future)).astype(np.float32)
        M2[:, tile, :] = np.where(forced, 1e30 * (1 + j / 100.0), np.where(future, -1e30 * (1 + j / 100.0), 0.0))
    c["selm1"] = M1
    c["selm2"] = M2
    gs = np.zeros((24, 24 * 64), np.float32)
    for k in range(24):
        gs[k, k * 64:(k + 1) * 64] = 1.0
    c["gsel"] = gs
    return c


CONST_SPECS = None


class Ctx:
    pass


def build_program(phases, dbg_out=()):
    nc = bass.Bass("TRN2", target_bir_lowering=False)
    ext_in = {}
    scratch = {}

    def din(name, shape, dt=F32):
        ext_in[name] = (list(shape), dt)
        return nc.dram_tensor(name, list(shape), dt, kind="ExternalInput").ap()

    def dscr(name, shape, dt):
        kind = "ExternalOutput" if name in dbg_out else "Internal"
        if ("in:" + name) in dbg_out:
            kind = "ExternalInput"
            ext_in[name] = (list(shape), dt)
        t = nc.dram_tensor(name, list(shape), dt, kind=kind).ap()
        scratch[name] = t
        return t

    consts = make_consts()
    X = Ctx()
    X.dbg_stop = [d[5:] for d in dbg_out if d.startswith("stop:")]
    X.dbg_stop = X.dbg_stop[0] if X.dbg_stop else None
    X.nc = nc
    X.cd = {k: din("c_" + k, v.shape, BF16 if v.dtype == ml_dtypes.bfloat16 else F32) for k, v in consts.items()}

    X.xT = din("xT", [D, T])
    X.cvec = din("cvec", [128, 8])
    X.mod_w = [din("mod_w%d" % j, [D, 3 * D]) for j in range(4)]
    X.mod_b = [din("mod_b%d" % j, [128, 24]) for j in range(4)]
    X.norm_w = [din("norm_w%d" % j, [128, 8]) for j in range(4)]
    X.hyb_w_in = din("hyb_w_in", [D, 2856])
    X.gk_up = din("gk_up", [16, 256])
    X.gk_bias_col = din("gk_bias_col", [128, 2])
    X.gk_bias_rep = din("gk_bias_rep", [64, 256])
    X.gla_norm = din("gla_norm", [128, 1])
    X.hyb_w_out = din("hyb_w_out", [D, D])
    X.dense_w_gu = din("dense_w_gu", [D, 5632])
    X.dense_w_down = din("dense_w_down", [2816, D])
    X.ssm_w_in = din("ssm_w_in", [D, 5152])
    X.ssm_w_out = din("ssm_w_out", [2048, D])
    X.conv_w_l = din("conv_w_l", [128, 24, 4])
    X.conv_b_l = din("conv_b_l", [128, 24])
    X.dt_bias_rep = din("dt_bias_rep", [128, 32])
    X.a_log_rep = din("a_log_rep", [64, 32])
    X.d_rep = din("d_rep", [64, 2048])
    X.gate_norm_rep = din("gate_norm_rep", [128, 2048])
    X.router = din("router", [D, 8])
    X.moe_w_gu = din("moe_w_gu", [8, D, 7168])
    X.moe_w_down = din("moe_w_down", [8, 3584, D])
    X.final_w_rep = din("final_w_rep", [128, D])
    X.out = nc.dram_tensor("out", [T, D], F32, kind="ExternalOutput").ap()
    X.cmp_peT = din("cmp_peT", [2, 64, 32])
    X.cmp_w1 = din("cmp_w1", [2, 2048, 256])
    X.cmp_w2 = din("cmp_w2", [2, 256, 64])

    X.gqT = dscr("gqT", [256, T], F32)
    X.gkT = dscr("gkT", [256, T], F32)
    X.grT = dscr("grT", [512, T], F32)
    X.glrT = dscr("glrT", [16, T], F32)
    X.gk_tok = dscr("gk_tok", [T, 256], F32)
    X.gv_tok = dscr("gv_tok", [T, 512], BF16)
    X.nqT = dscr("nqT", [512, T], BF16)
    X.nkT = dscr("nkT", [4, 128, T], BF16)
    X.nv_tok = dscr("nv_tok", [2, T, 128], BF16)
    X.ngT = dscr("ngT", [24, T], F32)
    X.mixT = dscr("mixT", [D, T], BF16)
    X.x1T = dscr("x1T", [D, T], F32)
    X.x2T = dscr("x2T", [D, T], F32)
    X.xbcT = dscr("xbcT", [3072, T], F32)
    X.z_tok = dscr("z_tok", [T, 2048], F32)
    X.dt_tok = dscr("dt_tok", [T, 32], F32)
    X.x_tok = dscr("x_tok", [T, 2048], F32)
    X.B_tok = dscr("B_tok", [T, 512], BF16)
    X.BT = dscr("BT", [512, T], BF16)
    X.CT = dscr("CT", [512, T], BF16)
    X.y_tok = dscr("y_tok", [T, 2048], F32)
    X.x3T = dscr("x3T", [D, T], F32)
    X.hmT = dscr("hmT", [D, T], BF16)

    es = ExitStack()
    with es:
        P = Prog(nc, es)
        kb = KB(nc, P)
        X.P, X.kb = P, kb
        X.ident_f = kb.sb(es, "ident_f", [128, 128], F32)
        X.ident_b = kb.sb(es, "ident_b", [128, 128], BF16)
        X.ones_b = kb.sb(es, "ones_b", [128, 128], BF16)
        X.epsc = kb.sb(es, "epsc", [128, 1], F32)
        X.modt = kb.sb(es, "modt", [128, 4, 24], F32)
        X.modA = kb.sb(es, "modA", [128, 4, 8], F32)
        X.pball = nc.alloc_psum_tensor("pball", [128, 8, 512], F32)
        X.pb = [Tl(X.pball[:, i, :], "pb%d" % i) for i in range(8)]
        for p_ in X.pb:
            p_.b.excl = True
        kb.ld(X.ident_f[:], X.cd["ident_f"], X.ident_f)
        kb.ld(X.ident_b[:], X.cd["ident_b"], X.ident_b)
        kb.ld(X.ones_b[:], X.cd["ones_b"], X.ones_b)
        kb.memset("dve", X.epsc[:], EPS, w=[X.epsc])

        if "adaln" in phases:
            phase_adaln(X)
            P.barrier()
            if "modt" in dbg_out:
                dm = nc.dram_tensor("modt_d", [128, 96], F32, kind="ExternalOutput").ap()
                kb.st(dm, X.modt[:, :, :].rearrange("p a b -> p (a b)"), X.modt)
                dm2 = nc.dram_tensor("modA_d", [128, 32], F32, kind="ExternalOutput").ap()
                kb.st(dm2, X.modA[:, :, :].rearrange("p a b -> p (a b)"), X.modA)
        if "l0proj" in phases:
            phase_l0proj(X)
            P.barrier()
        if "gla" in phases:
            phase_gla(X)
            P.barrier()
        if "nsa" in phases:
            phase_nsa(X)
            P.barrier()
        if "l0out" in phases:
            phase_l0out(X)
            P.barrier()
        if "dense" in phases:
            phase_dense(X)
            P.barrier()
        if "ssm_in" in phases:
            phase_ssm_in(X)
        if "ssm_conv" in phases:
            phase_ssm_conv(X)
        if "ssm_scan" in phases:
            phase_ssm_scan(X)
        if "ssm_out" in phases:
            phase_ssm_out(X)
        if "moe" in phases:
            phase_moe(X)
        P.barrier()
        P.replay()
    return nc, ext_in


def load_cast(X, es, name, dram2d, K, M, W16, col0=0, chunk=512):
    kb = X.kb
    KC = K // 128
    stg = [kb.sb(es, "%s_stg%d" % (name, i), [128, KC, chunk], F32) for i in range(2)]
    src = dram2d.rearrange("(k p) m -> p k m", p=128)
    i = 0
    for m0 in range(0, M, chunk):
        mw = min(chunk, M - m0)
        s = stg[i % 2]
        kb.ld(s[:, :, 0:mw], src[:, :, m0:m0 + mw], s)
        eng = ("dve", "act")[i % 2]
        kb.cp(eng, W16[:, :, col0 + m0:col0 + m0 + mw], s[:, :, 0:mw], r=[s], w=[W16])
        i += 1


def phase_adaln(X):
    kb, P, nc = X.kb, X.P, X.nc
    with ExitStack() as es:
        cv = kb.sb(es, "cv", [128, 8], F32)
        sc = kb.sb(es, "sc", [128, 8], F32)
        kb.ld(cv[:], X.cvec, cv)
        kb.act(sc[:], cv[:], AF.Silu, r=[cv], w=[sc])
        wh = [kb.sb(es, "modw%d" % i, [128, 4, 3072], F32) for i in range(2)]
        bt = kb.sb(es, "modbt", [128, 4, 24], F32)
        nw = kb.sb(es, "modnw", [128, 4, 8], F32)
        tmp = kb.sb(es, "modtmp", [128, 24], F32)
        for j in range(4):
            kb.ld(bt[:, j, :], X.mod_b[j], bt)
            kb.ld(nw[:, j, :], X.norm_w[j], nw)
        for j in range(4):
            src = X.mod_w[j].rearrange("(k p) m -> p k m", p=128)
            pss = []
            for hf in range(2):
                w = wh[hf]
                for k in range(4):
                    kb.ld(w[:, k, :], src[:, hf * 4 + k, :], w)
                ps = X.pb[(2 * j + hf) % 4]
                pss.append(ps)
                for m in range(24):
                    for k in range(4):
                        kb.mm(ps[:, m:m + 1], w[:, k, m * 128:(m + 1) * 128], sc[:, hf * 4 + k:hf * 4 + k + 1],
                              start=(k == 0), stop=(k == 3), r=[w, sc], w=[ps])
            kb.tt("dve", tmp[:], pss[0][:, 0:24], bt[:, j, :], ALU.add, r=[pss[0], bt], w=[tmp])
            kb.tt("dve", X.modt[:, j, :], pss[1][:, 0:24], tmp[:], ALU.add, r=[pss[1], tmp], w=[X.modt])
            kb.stt("dve", X.modA[:, j, :], X.modt[:, j, 8:16], 1.0, nw[:, j, :], ALU.add, ALU.mult,
                   r=[X.modt, nw], w=[X.modA])


def norm_modulate(X, xt, hT, j, sq, ps_ss, rstd, hF, W=TT, hT32=None):
    kb = X.kb
    for k in range(8):
        kb.act(sq[:, k, 0:W], xt[:, k, 0:W], AF.Square, r=[xt], w=[sq])
    for k in range(8):
        kb.mm(ps_ss[:, 0:W], X.ones_b[:, :], sq[:, k, 0:W], start=(k == 0), stop=(k == 7), r=[X.ones_b, sq], w=[ps_ss])
    kb.act(rstd[:, 0:W], ps_ss[:, 0:W], AF.Ln, bias=X.epsc[:, 0:1], scale=1.0 / D, r=[ps_ss, X.epsc], w=[rstd])
    kb.act(rstd[:, 0:W], rstd[:, 0:W], AF.Exp, scale=-0.5, r=[rstd], w=[rstd])
    for k in range(8):
        kb.tt("dve", hF[:, k, 0:W], xt[:, k, 0:W], rstd[:, 0:W], ALU.mult, r=[xt, rstd], w=[hF])
        kb.act(hT[:, k, 0:W], hF[:, k, 0:W], AF.Identity, bias=X.modt[:, j, k:k + 1], scale=X.modA[:, j, k:k + 1],
               r=[hF, X.modA, X.modt], w=[hT])
        if hT32 is not None:
            kb.act(hT32[:, k, 0:W], hF[:, k, 0:W], AF.Identity, bias=X.modt[:, j, k:k + 1], scale=X.modA[:, j, k:k + 1],
                   r=[hF, X.modA, X.modt], w=[hT32])


def phase_l0proj(X):
    kb, P, nc = X.kb, X.P, X.nc
    with ExitStack() as es:
        W16 = kb.sb(es, "w_in16", [128, 8, 2856], BF16)
        with ExitStack() as es2:
            load_cast(X, es2, "win", X.hyb_w_in, D, 2856, W16, chunk=476)
            P.barrier()
        xt = [kb.sb(es, "xt%d" % i, [128, 8, TT], F32) for i in range(2)]
        sq = kb.sb(es, "sq", [128, 8, TT], BF16)
        hF = kb.sb(es, "hF", [128, 8, TT], F32)
        hTs = [kb.sb(es, "hT%d" % i, [128, 8, TT], BF16) for i in range(2)]
        rstd = kb.sb(es, "rstd", [128, TT], F32)
        ofm = [kb.sb(es, "ofm%d" % i, [128, TT], F32) for i in range(4)]
        ofb = [kb.sb(es, "ofb%d" % i, [128, TT], BF16) for i in range(4)]
        otk = [kb.sb(es, "otk%d" % i, [128, 768], F32) for i in range(2)]
        otv = [kb.sb(es, "otv%d" % i, [128, 512], BF16) for i in range(2)]
        otn = [kb.sb(es, "otn%d" % i, [128, 256], BF16) for i in range(2)]
        xsrc = X.xT.rearrange("(k p) t -> p k t", p=128)
        fm = []
        for pc in range(2):
            fm.append((0 + pc * 128, 128, X.gqT[pc * 128:(pc + 1) * 128, :], F32, None))
        for pc in range(2):
            fm.append((256 + pc * 128, 128, X.gkT[pc * 128:(pc + 1) * 128, :], F32, None))
        fm.append((1024, 16, X.glrT[:, :], F32, None))
        for pc in range(4):
            fm.append((1040 + pc * 128, 128, X.grT[pc * 128:(pc + 1) * 128, :], F32, None))
        for pc in range(4):
            fm.append((1552 + pc * 128, 128, X.nqT[pc * 128:(pc + 1) * 128, :], BF16, 0.125))
        for jj, col in enumerate((2064, 2192, 2320, 2576)):
            fm.append((col, 128, X.nkT[jj], BF16, None))
        fm.append((2832, 24, X.ngT[:, :], F32, None))
        nps = 0
        dbgs = ""
        def prep(tt_):
            x = xt[tt_ % 2]
            for k in range(8):
                kb.ld(x[:, k, :], xsrc[:, k, tt_ * TT:(tt_ + 1) * TT], x)
            norm_modulate(X, x, hTs[tt_ % 2], 0, sq, X.pb[7], rstd, hF)

        prep(0)
        for tt_ in range(1 if "one" in dbgs else NT):
            t0 = tt_ * TT
            hT = hTs[tt_ % 2]
            if tt_ + 1 < NT:
                prep(tt_ + 1)
            for idx, (c0, ncol, dst, dt, scl) in enumerate(fm):
                ps = X.pb[nps % 6]
                nps += 1
                for k in range(8):
                    kb.mm(ps[0:ncol, :], W16[:, k, c0:c0 + ncol], hT[:, k, :], start=(k == 0), stop=(k == 7),
                          r=[W16, hT], w=[ps])
                stg = (ofm if dt == F32 else ofb)[idx % 4]
                eng = "act" if idx % 2 else "dve"
                if scl is None:
                    kb.cp(eng, stg[0:ncol, :], ps[0:ncol, :], r=[ps], w=[stg])
                else:
                    kb.ts("dve", stg[0:ncol, :], ps[0:ncol, :], scl, None, ALU.mult, r=[ps], w=[stg])
                kb.st(dst[:, t0:t0 + TT], stg[0:ncol, :], stg)
            for sub in range(0 if 'notk' in dbgs else 4):
                s0 = t0 + sub * 128
                psA = X.pb[nps % 6]; nps += 1
                psB = X.pb[nps % 6]; nps += 1
                psC = X.pb[nps % 6]; nps += 1
                for k in range(8):
                    kb.mm(psA[:, 0:512], hT[:, k, sub * 128:(sub + 1) * 128], W16[:, k, 256:768], start=(k == 0), stop=(k == 7),
                          r=[W16, hT], w=[psA])
                for k in range(8):
                    kb.mm(psB[:, 0:256], hT[:, k, sub * 128:(sub + 1) * 128], W16[:, k, 768:1024], start=(k == 0), stop=(k == 7),
                          r=[W16, hT], w=[psB])
                for k in range(8):
                    kb.mm(psC[:, 0:128], hT[:, k, sub * 128:(sub + 1) * 128], W16[:, k, 2448:2576], start=(k == 0), stop=(k == 7),
                          r=[W16, hT], w=[psC])
                for k in range(8):
                    kb.mm(psC[:, 128:256], hT[:, k, sub * 128:(sub + 1) * 128], W16[:, k, 2704:2832], start=(k == 0), stop=(k == 7),
                          r=[W16, hT], w=[psC])
                ok = otk[sub % 2]; ov = otv[sub % 2]; on = otn[sub % 2]
                kb.cp("act", ok[:, 0:256], psA[:, 0:256], r=[psA], w=[ok])
                kb.st(X.gk_tok[s0:s0 + 128, :], ok[:, 0:256], ok)
                kb.cp("dve", ov[:, 0:256], psA[:, 256:512], r=[psA], w=[ov])
                kb.cp("act", ov[:, 256:512], psB[:, 0:256], r=[psB], w=[ov])
                kb.st(X.gv_tok[s0:s0 + 128, :], ov[:, :], ov)
                kb.cp("dve", on[:, :], psC[:, 0:256], r=[psC], w=[on])
                kb.st(X.nv_tok[0, s0:s0 + 128, :], on[:, 0:128], on)
                kb.st(X.nv_tok[1, s0:s0 + 128, :], on[:, 128:256], on)


def phase_gla(X):
    kb, P, nc = X.kb, X.P, X.nc
    cd = X.cd
    with ExitStack() as es:
        gk_up = kb.sb(es, "g_gkup", [16, 256], F32)
        nbias = kb.sb(es, "g_nbias", [128, 2], F32)
        brow = kb.sb(es, "g_brow", [64, 512], F32)
        resetm = kb.sb(es, "g_resetm", [128, TT], F32)
        u64 = kb.sb(es, "g_u64", [64, 64], F32)
        cmask = kb.sb(es, "g_cmask", [64, 512], F32)
        gnorm = kb.sb(es, "g_gnorm", [128, 1], F32)
        onec = kb.sb(es, "g_onec", [128, 1], F32)
        kb.ld(gk_up[:], X.gk_up, gk_up)
        kb.ld(nbias[:], X.gk_bias_col, nbias)
        kb.ts("dve", nbias[:], nbias[:], -1.0, None, ALU.mult, r=[nbias], w=[nbias])
        kb.ld(brow[:, 0:256], X.gk_bias_rep, brow)
        kb.ld(brow[:, 256:512], X.gk_bias_rep, brow)
        kb.ld(resetm[:], cd["resetm"], resetm)
        kb.ld(u64[:], cd["u64"], u64)
        kb.ld(cmask[:], cd["cmask64"], cmask)
        kb.ld(gnorm[:], X.gla_norm, gnorm)
        kb.memset("dve", onec[:], 1.0, w=[onec])
        S = [kb.sb(es, "g_S%d" % pc, [128, 256], F32) for pc in range(2)]
        for pc in range(2):
            kb.memset("dve", S[pc][:], 0.0, w=[S[pc]])
        Sprev = [kb.sb(es, "g_Sprev%d" % pc, [128, 8, 256], BF16) for pc in range(2)]
        KVs = [kb.sb(es, "g_KVs%d" % pc, [128, 8, 256], F32) for pc in range(2)]
        lrT = kb.sb(es, "g_lrT", [16, TT], F32)
        qT = [kb.sb(es, "g_qT%d" % pc, [128, TT], F32) for pc in range(2)]
        kT = [kb.sb(es, "g_kT%d" % pc, [128, TT], F32) for pc in range(2)]
        rT = kb.sb(es, "g_rT", [128, 4, TT], F32)
        k64 = kb.sb(es, "g_k64", [64, 8, 256], F32)
        v64 = kb.sb(es, "g_v64", [64, 8, 512], BF16)
        ef = kb.sb(es, "g_ef", [128, TT], F32)
        bT = kb.sb(es, "g_bT", [128, TT], F32)
        eb = kb.sb(es, "g_eb", [128, TT], F32)
        enb = kb.sb(es, "g_enb", [128, TT], F32)
        qd = [kb.sb(es, "g_qd%d" % pc, [128, TT], BF16) for pc in range(2)]
        kd = [kb.sb(es, "g_kd%d" % pc, [128, TT], BF16) for pc in range(2)]
        eend = [kb.sb(es, "g_eend%d" % pc, [128, 8], F32) for pc in range(2)]
        zt = kb.sb(es, "g_zt", [64, 512], F32)
        spt = kb.sb(es, "g_spt", [64, 512], F32)
        ext = kb.sb(es, "g_ext", [64, 512], F32)
        kte = kb.sb(es, "g_kte", [64, 8, 256], BF16)
        attb = [kb.sb(es, "g_attb%d" % i, [64, 512], BF16) for i in range(2)]
        osq = kb.sb(es, "g_osq", [128, TT], BF16)
        rs = kb.sb(es, "g_rs", [128, TT], F32)
        sr = kb.sb(es, "g_sr", [128, TT], F32)
        o1 = kb.sb(es, "g_o1", [128, TT], F32)
        ob = [kb.sb(es, "g_ob%d" % i, [128, TT], BF16) for i in range(2)]
        pbi = [0]

        def nextpb():
            pbi[0] += 1
            return X.pb[pbi[0] % 8]

        for tt_ in range(NT):
            t0 = tt_ * TT
            kb.ld(lrT[:], X.glrT[:, t0:t0 + TT], lrT)
            for pc in range(2):
                kb.ld(qT[pc][:], X.gqT[pc * 128:(pc + 1) * 128, t0:t0 + TT], qT[pc])
                kb.ld(kT[pc][:], X.gkT[pc * 128:(pc + 1) * 128, t0:t0 + TT], kT[pc])
            for h in range(4):
                kb.ld(rT[:, h, :], X.grT[h * 128:(h + 1) * 128, t0:t0 + TT], rT)
            kb.ld(k64[:], X.gk_tok[t0:t0 + TT, :].rearrange("(n s) d -> s n d", s=64), k64)
            kb.ld(v64[:], X.gv_tok[t0:t0 + TT, :].rearrange("(n s) d -> s n d", s=64), v64)
            for pc in range(2):
                pz = nextpb()
                kb.mm(pz[:, :], gk_up[0:16, pc * 128:(pc + 1) * 128], lrT[0:16, :], r=[gk_up, lrT], w=[pz])
                kb.act(ef[:], pz[:, :], AF.Exp, bias=nbias[:, pc:pc + 1], scale=-1.0, r=[pz, nbias], w=[ef])
                kb.act(ef[:], ef[:], AF.Ln, bias=onec[:, 0:1], r=[ef, onec], w=[ef])
                kb.ts("dve", ef[:], ef[:], -1.0 / 16.0, None, ALU.mult, r=[ef], w=[ef])
                P.op("dve", (lambda o, d0, d1: (lambda e: e.tensor_tensor_scan(o, d0, d1, 0.0, ALU.mult, ALU.add)))(bT[:], resetm[:], ef[:]),
                     [resetm, ef], [bT])
                kb.act(eb[:], bT[:], AF.Exp, r=[bT], w=[eb])
                kb.act(enb[:], bT[:], AF.Exp, scale=-1.0, r=[bT], w=[enb])
                kb.stt("dve", qd[pc][:], qT[pc][:], 0.125, eb[:], ALU.mult, ALU.mult, r=[qT[pc], eb], w=[qd[pc]])
                kb.tt("dve", kd[pc][:], kT[pc][:], enb[:], ALU.mult, r=[kT[pc], enb], w=[kd[pc]])
                kb.act(eend[pc][:], bT[:].rearrange("p (n c) -> p n c", c=64)[:, :, 63], AF.Exp, r=[bT], w=[eend[pc]])
            for gi in range(4):
                pz = nextpb()
                for cc in range(2):
                    j = 2 * gi + cc
                    kb.mm(pz[0:64, cc * 256:(cc + 1) * 256], lrT[0:16, j * 64:(j + 1) * 64], gk_up[0:16, :], r=[gk_up, lrT], w=[pz])
                kb.tt("dve", zt[:], pz[0:64, :], brow[:], ALU.add, r=[pz, brow], w=[zt])
                kb.act(spt[:], zt[:], AF.Exp, scale=-1.0, r=[zt], w=[spt])
                kb.act(spt[:], spt[:], AF.Ln, bias=onec[0:64, 0:1], r=[spt, onec], w=[spt])
                p2 = nextpb()
                kb.mm(p2[0:64, :], u64[:, :], spt[:], r=[u64, spt], w=[p2])
                kb.act(ext[:], p2[0:64, :], AF.Exp, scale=-1.0 / 16.0, r=[p2], w=[ext])
                kb.tt("dve", kte[:, 2 * gi:2 * gi + 2, :], k64[:, 2 * gi:2 * gi + 2, :],
                      ext[:].rearrange("p (a b) -> p a b", a=2), ALU.mult, r=[k64, ext], w=[kte])
            for pc in range(2):
                for jj in range(4):
                    pk = nextpb()
                    for cc in range(2):
                        j = 2 * jj + cc
                        kb.mm(pk[:, cc * 256:(cc + 1) * 256], kte[0:64, j, pc * 128:(pc + 1) * 128],
                              v64[0:64, j, pc * 256:(pc + 1) * 256], r=[kte, v64], w=[pk])
                    kb.cp("act", KVs[pc][:, 2 * jj:2 * jj + 2, :], pk[:, :].rearrange("p (a b) -> p a b", a=2), r=[pk], w=[KVs[pc]])
                for j in range(8):
                    kb.cp("act", Sprev[pc][:, j, :], S[pc][:], r=[S[pc]], w=[Sprev[pc]])
                    kb.stt("dve", S[pc][:], S[pc][:], eend[pc][:, j:j + 1], KVs[pc][:, j, :], ALU.mult, ALU.add,
                           r=[S[pc], eend[pc], KVs[pc]], w=[S[pc]])
                for hh in range(2):
                    pa = nextpb()
                    for j in range(8):
                        kb.mm(pa[0:64, j * 64:(j + 1) * 64], kd[pc][hh * 64:(hh + 1) * 64, j * 64:(j + 1) * 64],
                              qd[pc][hh * 64:(hh + 1) * 64, j * 64:(j + 1) * 64], r=[kd[pc], qd[pc]], w=[pa], rg=hh * 64)
                    kb.tt("dve", attb[hh][:], pa[0:64, :], cmask[:], ALU.mult, r=[pa, cmask], w=[attb[hh]])
                for hh in range(2):
                    h = pc * 2 + hh
                    po = nextpb()
                    for j in range(8):
                        kb.mm(po[:, j * 64:(j + 1) * 64], v64[0:64, j, h * 128:(h + 1) * 128], attb[hh][0:64, j * 64:(j + 1) * 64],
                              start=True, stop=False, r=[v64, attb[hh]], w=[po])
                        kb.mm(po[:, j * 64:(j + 1) * 64], Sprev[pc][hh * 64:(hh + 1) * 64, j, hh * 128:(hh + 1) * 128],
                              qd[pc][hh * 64:(hh + 1) * 64, j * 64:(j + 1) * 64], start=False, stop=True,
                              r=[Sprev[pc], qd[pc]], w=[po], rg=hh * 64)
                    kb.act(osq[:], po[:, :], AF.Square, r=[po], w=[osq])
                    pss = nextpb()
                    kb.mm(pss[:, :], X.ones_b[:, :], osq[:], r=[X.ones_b, osq], w=[pss])
                    kb.act(rs[:], pss[:, :], AF.Ln, bias=X.epsc[:, 0:1], scale=1.0 / 128.0, r=[pss, X.epsc], w=[rs])
                    kb.act(rs[:], rs[:], AF.Exp, scale=-0.5, r=[rs], w=[rs])
                    kb.act(sr[:], rT[:, h, :], AF.Silu, r=[rT], w=[sr])
                    kb.tt("dve", o1[:], po[:, :], rs[:], ALU.mult, r=[po, rs], w=[o1])
                    o_ = ob[h % 2]
                    kb.stt("dve", o_[:], o1[:], gnorm[:, 0:1], sr[:], ALU.mult, ALU.mult, r=[o1, gnorm, sr], w=[o_])
                    kb.st(X.mixT[h * 128:(h + 1) * 128, t0:t0 + TT], o_[:], o_)
        P.barrier()


def phase_nsa(X):
    kb, P, nc = X.kb, X.P, X.nc
    cd = X.cd
    GEL = 1.5957691216057308
    with ExitStack() as es:
        kslc = [kb.sb(es, "n_kslc%d" % g, [128, T], BF16) for g in range(2)]
        kwin = [kb.sb(es, "n_kwin%d" % g, [128, T], BF16) for g in range(2)]
        vslc = [kb.sb(es, "n_vslc%d" % g, [128, 32, 128], BF16) for g in range(2)]
        vwin = [kb.sb(es, "n_vwin%d" % g, [128, 32, 128], BF16) for g in range(2)]
        kcT = [kb.sb(es, "n_kcT%d" % g, [128, 256], BF16) for g in range(2)]
        vc = [kb.sb(es, "n_vc%d" % g, [128, 2, 128], BF16) for g in range(2)]
        esel = kb.sb(es, "n_esel", [64, T], BF16)
        winm = kb.sb(es, "n_winm", [128, 8, 512], BF16)
        ovl = kb.sb(es, "n_ovl", [128, 2, 65], BF16)
        M1 = kb.sb(es, "n_M1", [128, 32, 64], F32)
        M2 = kb.sb(es, "n_M2", [128, 32, 64], F32)
        gsel = kb.sb(es, "n_gsel", [24, 24 * 64], F32)
        tiny = kb.sb(es, "n_tiny", [128, 1], F32)
        kb.ld(esel[:], cd["esel"], esel)
        kb.ld(winm[:], cd["winm"], winm)
        kb.ld(ovl[:], cd["ovl"], ovl)
        kb.ld(M1[:], cd["selm1"], M1)
        kb.ld(M2[:], cd["selm2"], M2)
        kb.ld(gsel[:], cd["gsel"], gsel)
        for g in range(2):
            kb.ld(kslc[g][0:64, :], X.nkT[2][g * 64:(g + 1) * 64, :], kslc[g])
            kb.ld(kslc[g][64:124, :], cd["esel"][0:60, :], kslc[g])
            kb.ld(kslc[g][124:128, :], cd["kaug"], kslc[g])
            kb.memset("dve", kwin[g][64:128, :], 0.0, w=[kwin[g]])
            kb.ld(kwin[g][0:64, :], X.nkT[3][g * 64:(g + 1) * 64, :], kwin[g])
            kb.ld(kwin[g][124:128, :], cd["kaug"], kwin[g])
            kb.memset("dve", vslc[g][:, :, 64:128], 1.0, w=[vslc[g]])
            kb.memset("dve", vwin[g][:, :, 64:128], 1.0, w=[vwin[g]])
            kb.ld(vslc[g][:, :, 0:64], X.nv_tok[0][:, g * 64:(g + 1) * 64].rearrange("(n p) d -> p n d", p=128), vslc[g])
            kb.ld(vwin[g][:, :, 0:64], X.nv_tok[1][:, g * 64:(g + 1) * 64].rearrange("(n p) d -> p n d", p=128), vwin[g])
            kb.memset("dve", kcT[g][:], 0.0, w=[kcT[g]])
            kb.memset("dve", vc[g][:], 0.0, w=[vc[g]])
        with ExitStack() as es2:
            w1s = kb.sb(es2, "n_w1s", [64, 32, 256], F32)
            w1b = kb.sb(es2, "n_w1b", [64, 32, 256], BF16)
            w2s = kb.sb(es2, "n_w2s", [128, 2, 64], F32)
            w2b = kb.sb(es2, "n_w2b", [128, 2, 64], BF16)
            pes = kb.sb(es2, "n_pes", [64, 32], F32)
            peb = kb.sb(es2, "n_peb", [64, 32], BF16)
            bh = kb.sb(es2, "n_bh", [128, 2], F32)
            cin = kb.sb(es2, "n_cin", [64, T], BF16)
            xs = kb.sb(es2, "n_xs", [128, 256], F32)
            x2 = kb.sb(es2, "n_x2", [128, 256], F32)
            hT = [kb.sb(es2, "n_hT%d" % i, [128, 256], BF16) for i in range(2)]
            for jkv in range(2):
                kb.ld(w1s[:], X.cmp_w1[jkv].rearrange("(l d) m -> d l m", d=64), w1s)
                kb.cp("dve", w1b[:, 0:16, :], w1s[:, 0:16, :], r=[w1s], w=[w1b])
                kb.cp("act", w1b[:, 16:32, :], w1s[:, 16:32, :], r=[w1s], w=[w1b])
                kb.ld(w2s[:], X.cmp_w2[jkv].rearrange("(c p) d -> p c d", p=128), w2s)
                kb.cp("dve", w2b[:], w2s[:], r=[w2s], w=[w2b])
                kb.ld(pes[:], X.cmp_peT[jkv], pes)
                kb.cp("dve", peb[:], pes[:], r=[pes], w=[peb])
                pbh = X.pb[0]
                for mc in range(2):
                    for l in range(32):
                        kb.mm(pbh[:, mc:mc + 1], w1b[0:64, l, mc * 128:(mc + 1) * 128], peb[0:64, l:l + 1],
                              start=(l == 0), stop=(l == 31), r=[w1b, peb], w=[pbh])
                kb.cp("dve", bh[:], pbh[:, 0:2], r=[pbh], w=[bh])
                for g in range(2):
                    kb.ld(cin[:], X.nkT[jkv][g * 64:(g + 1) * 64, :], cin)
                    cv = cin[:].rearrange("p (n s) -> p n s", s=16)
                    for mc in range(2):
                        ph = X.pb[1 + mc]
                        for l in range(32):
                            rhs = cv[:, 0:255, l] if l < 16 else cv[:, 1:256, l - 16]
                            kb.mm(ph[:, 0:255], w1b[0:64, l, mc * 128:(mc + 1) * 128], rhs,
                                  start=(l == 0), stop=(l == 31), r=[w1b, cin], w=[ph])
                        kb.act(xs[:, 0:255], ph[:, 0:255], AF.Identity, bias=bh[:, mc:mc + 1], r=[ph, bh], w=[xs])
                        kb.act(x2[:, 0:255], xs[:, 0:255], AF.Square, r=[xs], w=[x2])
                        kb.ts("dve", x2[:, 0:255], x2[:, 0:255], 0.044715, 1.0, ALU.mult, ALU.add, r=[x2], w=[x2])
                        kb.tt("dve", x2[:, 0:255], x2[:, 0:255], xs[:, 0:255], ALU.mult, r=[x2, xs], w=[x2])
                        kb.act(x2[:, 0:255], x2[:, 0:255], AF.Sigmoid, scale=GEL, r=[x2], w=[x2])
                        kb.memset("dve", hT[mc][:, 255:256], 0.0, w=[hT[mc]])
                        kb.tt("dve", hT[mc][:, 0:255], xs[:, 0:255], x2[:, 0:255], ALU.mult, r=[xs, x2], w=[hT[mc]])
                    if jkv == 0:
                        pk = X.pb[3]
                        for mc in range(2):
                            kb.mm(pk[0:64, 0:255], w2b[:, mc, :], hT[mc][:, 0:255], start=(mc == 0), stop=(mc == 1),
                                  r=[w2b, hT[mc]], w=[pk])
                        kb.cp("dve", kcT[g][0:64, 0:255], pk[0:64, 0:255], r=[pk], w=[kcT[g]])
                        kb.ld(kcT[g][124:128, :], cd["kaugc"], kcT[g])
                    else:
                        for ck in range(2):
                            pv = X.pb[3 + ck]
                            for mc in range(2):
                                kb.mm(pv[:, 0:64], hT[mc][:, ck * 128:(ck + 1) * 128], w2b[:, mc, :], start=(mc == 0), stop=(mc == 1),
                                      r=[w2b, hT[mc]], w=[pv])
                            kb.cp("act", vc[g][:, ck, 0:64], pv[:, 0:64], r=[pv], w=[vc[g]])
                        kb.memset("dve", vc[g][:, :, 64:128], 1.0, w=[vc[g]])
            P.barrier()
        if X.dbg_stop == "cmp":
            for g in range(2):
                d1 = nc.dram_tensor("kcT_d%d" % g, [68, 256], BF16, kind="ExternalOutput").ap()
                kb.st(d1, kcT[g][:], kcT[g])
                d2 = nc.dram_tensor("vc_d%d" % g, [128, 256], BF16, kind="ExternalOutput").ap()
                kb.st(d2, vc[g][:].rearrange("p a b -> p (a b)"), vc[g])
            P.barrier()
            return
        kb.memset("dve", tiny[:], 1e-20, w=[tiny])
        q8 = [kb.sb(es, "n_q8_%d" % i, [128, 8, TT], BF16) for i in range(2)]
        for i in range(2):
            kb.memset("dve", q8[i][64:128, :, :], 0.0, w=[q8[i]])
        cmpm = kb.sb(es, "n_cmpm", [128, 2, TT], BF16)
        gl = kb.sb(es, "n_gl", [24, TT], F32)
        pe16 = [kb.sb(es, "n_pe16_%d" % i, [128, TT], BF16) for i in range(6)]
        pcmp = [[kb.sb(es, "n_pcmp%d_%d" % (r, ck), [128, TT], BF16) for ck in range(2)] for r in range(4)]
        acc = kb.sb(es, "n_acc", [64, 4, TT], F32)
        rls = [kb.sb(es, "n_rl%d" % i, [64, TT], F32) for i in range(2)]
        ftmps = [kb.sb(es, "n_ftmp%d" % i, [64, TT], F32) for i in range(2)]
        otmps = [kb.sb(es, "n_otmp%d" % i, [64, TT], F32) for i in range(2)]
        cmbi = [0]
        ob = [kb.sb(es, "n_ob%d" % i, [64, TT], BF16) for i in range(2)]
        selbT = kb.sb(es, "n_selbT", [64, TT], BF16)
        rli = kb.sb(es, "n_rli", [128, 4, 4], F32)
        imp4 = kb.sb(es, "n_imp4", [128, 4, 64], F32)
        itmps = [kb.sb(es, "n_itmp%d" % i, [128, 4, 64], F32) for i in range(3)]
        top84 = kb.sb(es, "n_top84", [128, 4, 8], F32)
        pei = [0]

        def next_pe16():
            pei[0] += 1
            return pe16[pei[0] % 6]

        sc32 = [kb.sb(es, "n_sc32_%d" % i, [128, TT], F32) for i in range(3)]
        sci = [0]

        def next_sc():
            sci[0] += 1
            return sc32[sci[0] % 3]

        def combine(po, h, r, br, first):
            cmbi[0] += 1
            rl, ftmp, otmp = rls[cmbi[0] % 2], ftmps[cmbi[0] % 2], otmps[cmbi[0] % 2]
            pg = X.pb[3]
            kb.mm(pg[0:64, :], gsel[0:24, (3 * h + br) * 64:(3 * h + br + 1) * 64], gl[0:24, :], r=[gsel, gl], w=[pg])
            kb.ts("dve", rl[:], po[64:128, :], 1e-20, None, ALU.max, r=[po], w=[rl])
            kb.act(rl[:], rl[:], AF.Ln, r=[rl], w=[rl])
            kb.act(rl[:], rl[:], AF.Exp, scale=-1.0, r=[rl], w=[rl])
            kb.tt("dve", ftmp[:], pg[0:64, :], rl[:], ALU.mult, r=[pg, rl], w=[ftmp])
            if first:
                kb.tt("dve", acc[:, r, :], po[0:64, :], ftmp[:], ALU.mult, r=[po, ftmp], w=[acc])
            else:
                kb.tt("dve", otmp[:], po[0:64, :], ftmp[:], ALU.mult, r=[po, ftmp], w=[otmp])
                kb.tt("dve", acc[:, r, :], acc[:, r, :], otmp[:], ALU.add, r=[acc, otmp], w=[acc])

        LOOK = 3
        poi = [0]

        def next_po():
            poi[0] += 1
            return X.pb[poi[0] % 3]

        for qt in range(NT):
            t0 = qt * TT
            q = q8[qt % 2]
            kb.ld(q[0:64, :, :], X.nqT[:, t0:t0 + TT].rearrange("(h d) t -> d h t", d=64), q)
            kb.ld(q[124:128, :, :], cd["qaug"][:, :, t0:t0 + TT].rearrange("h a t -> a h t"), q)
            kb.ld(cmpm[:], cd["cmpm"][qt], cmpm)
            kb.ld(gl[:], X.ngT[:, t0:t0 + TT], gl)
            kb.act(gl[:], gl[:], AF.Sigmoid, r=[gl], w=[gl])
            ncks = 2 if qt >= 4 else 1
            for g in range(2):
                pimp = [X.pb[4 + sub] for sub in range(4)]
                nsc = 0
                for r in range(4):
                    h = g * 4 + r
                    for ck in range(ncks):
                        ps = X.pb[1 + nsc % 2]
                        nsc += 1
                        kb.mm(ps[:, :], kcT[g][:, ck * 128:(ck + 1) * 128], q[:, h, :], r=[kcT[g], q], w=[ps])
                        sc_ = next_sc()
                        kb.tt("dve", sc_[:], ps[:, :], cmpm[:, ck, :], ALU.add, r=[ps, cmpm], w=[sc_])
                        kb.act(pcmp[r][ck][:], sc_[:], AF.Exp, r=[sc_], w=[pcmp[r][ck]])
                for r in range(4):
                    h = g * 4 + r
                    po = X.pb[0]
                    for ck in range(ncks):
                        kb.mm(po[:, :], vc[g][:, ck, :], pcmp[r][ck][:], start=(ck == 0), stop=(ck == ncks - 1),
                              r=[vc[g], pcmp[r][ck]], w=[po])
                    for sub in range(4):
                        for ck in range(ncks):
                            kb.mm(pimp[sub][:, r * 65:(r + 1) * 65], pcmp[r][ck][:, sub * 128:(sub + 1) * 128], ovl[:, ck, :],
                                  start=(ck == 0), stop=(ck == ncks - 1), r=[pcmp[r][ck], ovl], w=[pimp[sub]])
                    combine(po, h, r, 0, True)
                pv4 = X.pball[:, 4:8, 0:260].rearrange("p s (r c) -> p s r c", c=65)
                kb.ts("dve", rli[:], pv4[:, :, :, 64], 1e-20, None, ALU.max, r=pimp, w=[rli])
                kb.recip(rli[:], rli[:], r=[rli], w=[rli])
                for r in range(4):
                    dst = imp4 if r == 0 else itmps[r - 1]
                    itmp = dst
                    kb.tt("dve", dst[:], pv4[:, :, r, 0:64], rli[:, :, r:r + 1].to_broadcast([128, 4, 64]), ALU.mult,
                          r=pimp + [rli], w=[dst])
                    if r > 0:
                        kb.tt("dve", imp4[:], imp4[:], itmp[:], ALU.add, r=[imp4, itmp], w=[imp4])
                kb.tt("dve", imp4[:], imp4[:], M1[:, qt * 4:qt * 4 + 4, :], ALU.mult, r=[imp4, M1], w=[imp4])
                kb.tt("dve", imp4[:], imp4[:], M2[:, qt * 4:qt * 4 + 4, :], ALU.add, r=[imp4, M2], w=[imp4])
                for sub in range(4):
                    P.op("dve", (lambda o, i: (lambda e: e.max(out=o, in_=i)))(top84[:, sub, :], imp4[:, sub, :]), [imp4], [top84])
                kb.tt("dve", imp4[:], imp4[:], top84[:, :, 7:8].to_broadcast([128, 4, 64]), ALU.is_ge, r=[imp4, top84], w=[imp4])
                kb.ts("dve", imp4[:], imp4[:], 1.0, 30000.0, ALU.subtract, ALU.mult, r=[imp4], w=[imp4])
                pt = X.pb[3]
                for sub in range(4):
                    kb.tr(pt[0:64, sub * 128:(sub + 1) * 128], imp4[:, sub, :], X.ident_f[:, :], r=[imp4, X.ident_f], w=[pt])
                if qt == NT - 1:
                    kb.cp("act", selbT[:, :], pt[0:64, :], r=[pt], w=[selbT])
                kb.cp("act", q[64:124, 4 * g:4 * g + 4, :], pt[0:60, :].unsqueeze(1).to_broadcast([60, 4, TT]), r=[pt], w=[q])
                jobs = []
                nst = 4 * qt + 4
                st0 = max(0, 4 * qt - 4)
                for r in range(4):
                    for st in range(nst):
                        jobs.append((r, 1, st, st == 0, st == nst - 1))
                    for st in range(st0, nst):
                        jobs.append((r, 2, st, st == st0, st == nst - 1))
                pes = {}
                pos = {}
                nsc = 0

                def front(i):
                    r, br, st, isfirst, islast = jobs[i]
                    h = g * 4 + r
                    ps = X.pb[4 + i % 4]
                    pe_ = next_pe16()
                    if br == 1:
                        hi = st >= 30
                        kb.mm(ps[:, :], kslc[g][:, st * 128:(st + 1) * 128], q[:, h, :], start=True, stop=not hi,
                              r=[kslc[g], q], w=[ps])
                        if hi:
                            kb.mm(ps[:, :], esel[0:64, st * 128:(st + 1) * 128], selbT[0:64, :], start=False, stop=True,
                                  r=[esel, selbT], w=[ps])
                        masked = st >= 4 * qt
                    else:
                        kb.mm(ps[:, :], kwin[g][:, st * 128:(st + 1) * 128], q[:, h, :], r=[kwin[g], q], w=[ps])
                        masked = True
                    if masked:
                        sc_ = next_sc()
                        kb.tt("dve", sc_[:], ps[:, :], winm[:, 4 + st - 4 * qt, :], ALU.add, r=[ps, winm], w=[sc_])
                        kb.act(pe_[:], sc_[:], AF.Exp, r=[sc_], w=[pe_])
                    else:
                        kb.act(pe_[:], ps[:, :], AF.Exp, r=[ps], w=[pe_])
                    pes[i] = pe_

                def back(i):
                    r, br, st, isfirst, islast = jobs[i]
                    h = g * 4 + r
                    if isfirst:
                        pos[(r, br)] = next_po()
                    po = pos[(r, br)]
                    vv = vslc if br == 1 else vwin
                    kb.mm(po[:, :], vv[g][:, st, :], pes[i][:], start=isfirst, stop=islast, r=[vv[g], pes[i]], w=[po])
                    if islast:
                        combine(po, h, r, br, False)
                        if br == 2:
                            o_ = ob[h % 2]
                            kb.cp("act", o_[:], acc[:, r, :], r=[acc], w=[o_])
                            kb.st(X.mixT[512 + h * 64:512 + (h + 1) * 64, t0:t0 + TT], o_[:], o_)

                LK = 4
                for i in range(min(LK, len(jobs))):
                    front(i)
                for i in range(0, len(jobs), 2):
                    for j in (i + LK, i + LK + 1):
                        if j < len(jobs):
                            front(j)
                    for j in (i, i + 1):
                        if j < len(jobs):
                            back(j)
        P.barrier()


def phase_l0out(X):
    kb, P, nc = X.kb, X.P, X.nc
    with ExitStack() as es:
        W16 = kb.sb(es, "wo16", [128, 8, 1024], BF16)
        with ExitStack() as es2:
            load_cast(X, es2, "wo", X.hyb_w_out, D, D, W16, chunk=512)
            P.barrier()
        mx = [kb.sb(es, "o_mx%d" % i, [128, 8, TT], BF16) for i in range(2)]
        xt = [kb.sb(es, "o_xt%d" % i, [128, 8, TT], F32) for i in range(2)]
        xo = [kb.sb(es, "o_xo%d" % i, [128, TT], F32) for i in range(3)]
        msrc = X.mixT.rearrange("(k p) t -> p k t", p=128)
        xsrc = X.xT.rearrange("(k p) t -> p k t", p=128)
        n = 0
        for tt_ in range(NT):
            t0 = tt_ * TT
            m = mx[tt_ % 2]
            x = xt[tt_ % 2]
            for k in range(8):
                kb.ld(m[:, k, :], msrc[:, k, t0:t0 + TT], m)
                kb.ld(x[:, k, :], xsrc[:, k, t0:t0 + TT], x)
            for dc in range(8):
                ps = X.pb[n % 4]
                o = xo[n % 3]
                n += 1
                for k in range(8):
                    kb.mm(ps[:, :], W16[:, k, dc * 128:(dc + 1) * 128], m[:, k, :], start=(k == 0), stop=(k == 7), r=[W16, m], w=[ps])
                kb.stt("dve", o[:], ps[:, :], X.modt[:, 0, 16 + dc:17 + dc], x[:, dc, :], ALU.mult, ALU.add,
                       r=[ps, X.modt, x], w=[o])
                kb.st(X.x1T[dc * 128:(dc + 1) * 128, t0:t0 + TT], o[:], o)
        P.barrier()


def phase_dense(X):
    kb, P, nc = X.kb, X.P, X.nc
    W = 256
    NF = 22
    with ExitStack() as es:
        Wgu = kb.sb(es, "d_wgu", [128, 8, 5632], BF16)
        Wd = kb.sb(es, "d_wd", [128, NF, 1024], BF16)
        with ExitStack() as es2:
            load_cast(X, es2, "dgu", X.dense_w_gu, D, 5632, Wgu, chunk=512)
            P.barrier()
        with ExitStack() as es2:
            load_cast(X, es2, "ddn", X.dense_w_down, 2816, 1024, Wd, chunk=256)
            P.barrier()
        xt = [kb.sb(es, "d_xt%d" % i, [128, 8, W], F32) for i in range(3)]
        sq = kb.sb(es, "d_sq", [128, 8, W], BF16)
        hF = kb.sb(es, "d_hF", [128, 8, W], F32)
        hTs = [kb.sb(es, "d_hT%d" % i, [128, 8, W], BF16) for i in range(2)]
        rstd = kb.sb(es, "d_rstd", [128, W], F32)
        sg = [kb.sb(es, "d_sg%d" % i, [128, W], F32) for i in range(2)]
        a16s = [kb.sb(es, "d_a16_%d" % i, [128, NF, W], BF16) for i in range(2)]
        xo = [kb.sb(es, "d_xo%d" % i, [128, W], F32) for i in range(3)]
        xsrc = X.x1T.rearrange("(k p) t -> p k t", p=128)
        n = 0
        def prep(tt_):
            x = xt[tt_ % 3]
            for k in range(8):
                kb.ld(x[:, k, :], xsrc[:, k, tt_ * W:(tt_ + 1) * W], x)
            norm_modulate(X, x, hTs[tt_ % 2], 1, sq, X.pb[7], rstd, hF, W=W)

        prep(0)
        for tt_ in range(T // W):
            t0 = tt_ * W
            x = xt[tt_ % 3]
            hT = hTs[tt_ % 2]
            a16 = a16s[tt_ % 2]
            if tt_ + 1 < T // W:
                prep(tt_ + 1)
            for fc in range(NF):
                ps = X.pb[n % 6]
                n += 1
                for k in range(8):
                    kb.mm(ps[:, 0:W], Wgu[:, k, fc * 128:(fc + 1) * 128], hT[:, k, :], start=(k == 0), stop=(k == 7), r=[Wgu, hT], w=[ps])
                for k in range(8):
                    kb.mm(ps[:, W:2 * W], Wgu[:, k, 2816 + fc * 128:2816 + (fc + 1) * 128], hT[:, k, :], start=(k == 0), stop=(k == 7),
                          r=[Wgu, hT], w=[ps])
                s_ = sg[fc % 2]
                kb.act(s_[:], ps[:, 0:W], AF.Silu, r=[ps], w=[s_])
                kb.tt("dve", a16[:, fc, :], s_[:], ps[:, W:2 * W], ALU.mult, r=[s_, ps], w=[a16])
            for dc in range(8):
                ps = X.pb[n % 6]
                o = xo[n % 3]
                n += 1
                for fc in range(NF):
                    kb.mm(ps[:, 0:W], Wd[:, fc, dc * 128:(dc + 1) * 128], a16[:, fc, :], start=(fc == 0), stop=(fc == NF - 1), r=[Wd, a16], w=[ps])
                kb.stt("dve", o[:], ps[:, 0:W], X.modt[:, 1, 16 + dc:17 + dc], x[:, dc, :], ALU.mult, ALU.add,
                       r=[ps, X.modt, x], w=[o])
                kb.st(X.x2T[dc * 128:(dc + 1) * 128, t0:t0 + W], o[:], o)
        P.barrier()


def phase_ssm_in(X):
    kb, P, nc = X.kb, X.P, X.nc
    with ExitStack() as es:
        W16 = kb.sb(es, "s_win16", [128, 8, 5152], BF16)
        with ExitStack() as es2:
            load_cast(X, es2, "swin", X.ssm_w_in, D, 5152, W16, chunk=368)
            P.barrier()
        xt = [kb.sb(es, "s_xt%d" % i, [128, 8, TT], F32) for i in range(2)]
        sq = kb.sb(es, "s_sq", [128, 8, TT], BF16)
        hF = kb.sb(es, "s_hF", [128, 8, TT], F32)
        hTs = [kb.sb(es, "s_hT%d" % i, [128, 8, TT], BF16) for i in range(2)]
        rstd = kb.sb(es, "s_rstd", [128, TT], F32)
        ofm = [kb.sb(es, "s_ofm%d" % i, [128, TT], F32) for i in range(4)]
        zst = [kb.sb(es, "s_zst%d" % i, [128, 2048], F32) for i in range(2)]
        dtb = kb.sb(es, "s_dtb", [128, 32], F32)
        d1 = kb.sb(es, "s_d1", [128, 32], F32)
        d2 = kb.sb(es, "s_d2", [128, 32], F32)
        d3 = [kb.sb(es, "s_d3_%d" % i, [128, 32], F32) for i in range(2)]
        onec = kb.sb(es, "s_onec", [128, 1], F32)
        kb.memset("dve", onec[:], 1.0, w=[onec])
        kb.ld(dtb[:], X.dt_bias_rep, dtb)
        xsrc = X.x2T.rearrange("(k p) t -> p k t", p=128)
        n = 0
        def prep(tt_):
            x = xt[tt_ % 2]
            for k in range(8):
                kb.ld(x[:, k, :], xsrc[:, k, tt_ * TT:(tt_ + 1) * TT], x)
            norm_modulate(X, x, hTs[tt_ % 2], 2, sq, X.pb[7], rstd, hF)

        prep(0)
        for tt_ in range(NT):
            t0 = tt_ * TT
            hT = hTs[tt_ % 2]
            if tt_ + 1 < NT:
                prep(tt_ + 1)
            for fc in range(24):
                ps = X.pb[n % 6]
                o = ofm[n % 4]
                n += 1
                c0 = 2048 + fc * 128
                for k in range(8):
                    kb.mm(ps[:, :], W16[:, k, c0:c0 + 128], hT[:, k, :], start=(k == 0), stop=(k == 7), r=[W16, hT], w=[ps])
                kb.cp("act" if fc % 2 else "dve", o[:], ps[:, :], r=[ps], w=[o])
                kb.st(X.xbcT[fc * 128:(fc + 1) * 128, t0:t0 + TT], o[:], o)
            for sub in range(4):
                s0 = t0 + sub * 128
                zs = zst[sub % 2]
                for q4 in range(4):
                    ps = X.pb[n % 6]
                    n += 1
                    for k in range(8):
                        kb.mm(ps[:, :], hT[:, k, sub * 128:(sub + 1) * 128], W16[:, k, q4 * 512:(q4 + 1) * 512], start=(k == 0), stop=(k == 7),
                              r=[W16, hT], w=[ps])
                    kb.cp("act" if q4 % 2 else "dve", zs[:, q4 * 512:(q4 + 1) * 512], ps[:, :], r=[ps], w=[zs])
                kb.st(X.z_tok[s0:s0 + 128, :], zs[:], zs)
                ps = X.pb[n % 6]
                n += 1
                for k in range(8):
                    kb.mm(ps[:, 0:32], hT[:, k, sub * 128:(sub + 1) * 128], W16[:, k, 5120:5152], start=(k == 0), stop=(k == 7),
                          r=[W16, hT], w=[ps])
                kb.tt("dve", d1[:], ps[:, 0:32], dtb[:], ALU.add, r=[ps, dtb], w=[d1])
                kb.act(d2[:], d1[:], AF.Abs, r=[d1], w=[d2])
                kb.act(d2[:], d2[:], AF.Exp, scale=-1.0, r=[d2], w=[d2])
                kb.act(d2[:], d2[:], AF.Ln, bias=onec[:, 0:1], r=[d2, onec], w=[d2])
                dd = d3[sub % 2]
                kb.stt("dve", dd[:], d1[:], 0.0, d2[:], ALU.max, ALU.add, r=[d1, d2], w=[dd])
                kb.st(X.dt_tok[s0:s0 + 128, :], dd[:], dd)
        P.barrier()


def phase_ssm_conv(X):
    kb, P, nc = X.kb, X.P, X.nc
    NA = 8
    with ExitStack() as es:
        cw = kb.sb(es, "c_cw", [128, 24, 4], F32)
        cb = kb.sb(es, "c_cb", [128, 24], F32)
        kb.ld(cw[:], X.conv_w_l, cw)
        kb.ld(cb[:], X.conv_b_l, cb)
        xp = [kb.sb(es, "c_xp%d" % i, [128, T + 3], F32) for i in range(2)]
        accs = [kb.sb(es, "c_acc%d" % i, [128, T], F32) for i in range(NA)]
        ob = kb.sb(es, "c_ob", [128, T], BF16)
        tst = [kb.sb(es, "c_tst%d" % i, [128, 512], F32) for i in range(4)]
        tsb = [kb.sb(es, "c_tsb%d" % i, [128, 512], BF16) for i in range(3)]
        for i in range(2):
            kb.memset("dve", xp[i][:, 0:3], 0.0, w=[xp[i]])
        nn = [0]

        def conv1(fc):
            x = xp[fc % 2]
            acc = accs[fc % NA]
            kb.ld(x[:, 3:T + 3], X.xbcT[fc * 128:(fc + 1) * 128, :], x)
            kb.act(acc[:], x[:, 0:T], AF.Identity, bias=cb[:, fc:fc + 1], scale=cw[:, fc, 0:1], r=[x, cb, cw], w=[acc])

        def conv2(fc):
            x = xp[fc % 2]
            acc = accs[fc % NA]
            for k in range(1, 4):
                kb.stt("dve", acc[:], x[:, k:k + T], cw[:, fc, k:k + 1], acc[:], ALU.mult, ALU.add, r=[x, cw, acc], w=[acc])
            kb.act(acc[:], acc[:], AF.Silu, r=[acc], w=[acc])
            if fc >= 16:
                kb.cp("pool", ob[:], acc[:], r=[acc], w=[ob])
                if fc < 20:
                    kb.st(X.BT[(fc - 16) * 128:(fc - 15) * 128, :], ob[:], ob)
                else:
                    kb.st(X.CT[(fc - 20) * 128:(fc - 19) * 128, :], ob[:], ob)

        def conv_range(f0, f1):
            conv1(f0)
            for fc in range(f0, f1):
                if fc + 1 < f1:
                    conv1(fc + 1)
                conv2(fc)

        def trans(grp):
            if grp >= 5:
                return
            ga = [accs[(4 * grp + j) % NA] for j in range(4)]
            for blk in range(T // 128):
                n = nn[0]
                nn[0] += 1
                ps = X.pb[n % 8]
                for j in range(4):
                    kb.tr(ps[:, j * 128:(j + 1) * 128], ga[j][:, blk * 128:(blk + 1) * 128], X.ident_f[:, :], r=[ga[j], X.ident_f], w=[ps])
                if grp < 4:
                    st_ = tst[n % 4]
                    kb.cp("act" if n % 2 else "dve", st_[:], ps[:, :], r=[ps], w=[st_])
                    kb.st(X.x_tok[blk * 128:(blk + 1) * 128, grp * 512:(grp + 1) * 512], st_[:], st_)
                else:
                    st_ = tsb[n % 3]
                    kb.cp("act" if n % 2 else "dve", st_[:], ps[:, :], r=[ps], w=[st_])
                    kb.st(X.B_tok[blk * 128:(blk + 1) * 128, :], st_[:], st_)

        conv_range(0, 4)
        for grp in range(6):
            if grp + 1 < 6:
                conv_range(4 * (grp + 1), 4 * (grp + 2))
            trans(grp)
        P.barrier()


def phase_ssm_scan(X):
    kb, P, nc = X.kb, X.P, X.nc
    cd = X.cd
    NCH = 2
    NCK = T // 64
    with ExitStack() as es:
        u64 = kb.sb(es, "m_u64", [64, 64], F32)
        tri = kb.sb(es, "m_tri", [64, 64], F32)
        cmask = kb.sb(es, "m_cmask", [64, 64], F32)
        ones_f = kb.sb(es, "m_onesf", [64, 128], F32)
        Arow = kb.sb(es, "m_Arow", [64, 32], F32)
        Dbc = kb.sb(es, "m_Dbc", [64, 2048], F32)
        kb.ld(u64[:], cd["u64"], u64)
        kb.ld(tri[:], cd["tri64"], tri)
        kb.ld(cmask[:], cd["cmask64"][:, 0:64], cmask)
        kb.memset("dve", ones_f[:], 1.0, w=[ones_f])
        kb.ld(Arow[:], X.a_log_rep, Arow)
        kb.act(Arow[:], Arow[:], AF.Exp, r=[Arow], w=[Arow])
        kb.ts("dve", Arow[:], Arow[:], -1.0, None, ALU.mult, r=[Arow], w=[Arow])
        kb.ld(Dbc[:], X.d_rep, Dbc)
        S32 = [kb.sb(es, "m_S32_%d" % g, [128, 512], F32) for g in range(4)]
        S16 = [kb.sb(es, "m_S16_%d" % g, [128, 512], BF16) for g in range(4)]
        for g in range(4):
            kb.memset("dve", S32[g][:], 0.0, w=[S32[g]])
            kb.memset("dve", S16[g][:], 0.0, w=[S16[g]])
        NB = 3
        x64 = [kb.sb(es, "m_x64_%d" % i, [64, NCH, 2048], F32) for i in range(NB)]
        B64 = [kb.sb(es, "m_B64_%d" % i, [64, NCH, 512], BF16) for i in range(NB)]
        BTt = [kb.sb(es, "m_BT_%d" % i, [128, 4, NCH * 64], BF16) for i in range(NB)]
        CTt = [kb.sb(es, "m_CT_%d" % i, [128, 4, NCH * 64], BF16) for i in range(NB)]
        dt64 = [kb.sb(es, "m_dt64_%d" % i, [64, NCH, 32], F32) for i in range(NB)]
        a_tok = [kb.sb(es, "m_atok%d" % i, [64, 32], F32) for i in range(2)]
        dte = [kb.sb(es, "m_dte%d" % i, [64, 32], F32) for i in range(2)]
        dfs = [kb.sb(es, "m_dfs%d" % i, [64, 32], F32) for i in range(2)]
        cdec = [kb.sb(es, "m_cdec%d" % i, [128, 32], F32) for i in range(2)]
        xdt = [kb.sb(es, "m_xdt%d" % i, [64, 2048], BF16) for i in range(2)]
        xdte = [kb.sb(es, "m_xdte%d" % i, [64, 2048], BF16) for i in range(2)]
        xdf = kb.sb(es, "m_xdf", [64, 2048], F32)
        xD = [kb.sb(es, "m_xD%d" % i, [64, 2048], F32) for i in range(2)]
        R4 = kb.sb(es, "m_R4", [64, 2048], F32)
        LT = [kb.sb(es, "m_LT%d" % i, [64, 512], F32) for i in range(4)]
        cbm4 = [kb.sb(es, "m_cbm4_%d" % i, [64, 4, 64], F32) for i in range(2)]
        WT = [kb.sb(es, "m_WT%d" % i, [64, 512], BF16) for i in range(8)]
        yo = [kb.sb(es, "m_yo%d" % i, [64, 512], F32) for i in range(2)]
        yst = [kb.sb(es, "m_yst%d" % i, [64, 2048], F32) for i in range(2)]
        stmp = [kb.sb(es, "m_stmp%d" % i, [128, 512], F32) for i in range(2)]
        nb = [0]

        def npb():
            nb[0] += 1
            return X.pb[nb[0] % 8]

        def loads(tg):
            t0 = tg * 64 * NCH
            i = tg % NB
            kb.ld(x64[i][:], X.x_tok[t0:t0 + 64 * NCH, :].rearrange("(n s) c -> s n c", s=64), x64[i])
            kb.ld(B64[i][:], X.B_tok[t0:t0 + 64 * NCH, :].rearrange("(n s) c -> s n c", s=64), B64[i])
            kb.ld(BTt[i][:], X.BT[:, t0:t0 + 64 * NCH].rearrange("(g p) t -> p g t", p=128), BTt[i])
            kb.ld(CTt[i][:], X.CT[:, t0:t0 + 64 * NCH].rearrange("(g p) t -> p g t", p=128), CTt[i])
            kb.ld(dt64[i][:], X.dt_tok[t0:t0 + 64 * NCH, :].rearrange("(n s) c -> s n c", s=64), dt64[i])

        def stage_a(c):
            tg, ci = divmod(c, NCH)
            if ci == 0:
                loads(tg)
            i = tg % NB
            p = c % 2
            xx, dd, bt, ct = x64[i], dt64[i], BTt[i], CTt[i]
            at = a_tok[p]
            kb.tt("dve", at[:], dd[:, ci, :], Arow[:], ALU.mult, r=[dd, Arow], w=[at])
            pm = npb()
            kb.mm(pm[0:64, 0:32], u64[:, :], at[:, :], r=[u64, at], w=[pm])
            kb.mm(pm[0:64, 32:64], tri[:, :], at[:, :], r=[tri, at], w=[pm])
            kb.mm(pm[:, 64:96], ones_f[:, :], at[:, :], r=[ones_f, at], w=[pm])
            pcb = npb()
            for g in range(4):
                kb.mm(pcb[0:64, g * 64:(g + 1) * 64], bt[:, g, ci * 64:(ci + 1) * 64], ct[:, g, ci * 64:(ci + 1) * 64],
                      r=[bt, ct], w=[pcb])
            kb.act(dte[p][:], pm[0:64, 0:32], AF.Exp, r=[pm], w=[dte[p]])
            kb.act(dfs[p][:], pm[0:64, 32:64], AF.Exp, r=[pm], w=[dfs[p]])
            kb.act(cdec[p][:], pm[:, 64:96], AF.Exp, r=[pm], w=[cdec[p]])
            kb.tt("dve", R4[:].rearrange("k (h l) -> k h l", l=64), tri[:, :].unsqueeze(1).to_broadcast([64, 32, 64]),
                  at[:, :].unsqueeze(2).to_broadcast([64, 32, 64]), ALU.mult, r=[tri, at], w=[R4])
            for g in range(4):
                pseg = npb()
                kb.mm(pseg[0:64, :], u64[:, :], R4[:, g * 512:(g + 1) * 512], r=[u64, R4], w=[pseg])
                kb.act(LT[g][:], pseg[0:64, :], AF.Exp, r=[pseg], w=[LT[g]])
            xv = xx[:, ci, :].rearrange("s (h p) -> s h p", p=64)
            kb.tt("dve", xdf[:].rearrange("s (h p) -> s h p", p=64), xv, dd[:, ci, :].unsqueeze(2).to_broadcast([64, 32, 64]),
                  ALU.mult, r=[xx, dd], w=[xdf])
            kb.cp("act", xdt[p][:], xdf[:], r=[xdf], w=[xdt[p]])
            kb.tt("dve", xdte[p][:].rearrange("s (h p) -> s h p", p=64), xdf[:].rearrange("s (h p) -> s h p", p=64),
                  dte[p][:, :].unsqueeze(2).to_broadcast([64, 32, 64]), ALU.mult, r=[xdf, dte[p]], w=[xdte[p]])
            kb.tt("pool", xD[p][:], xx[:, ci, :], Dbc[:], ALU.mult, r=[xx, Dbc], w=[xD[p]])
            c4 = cbm4[p]
            kb.tt("dve", c4[:], pcb[0:64, 0:256].rearrange("s (g l) -> s g l", l=64), cmask[:, :].unsqueeze(1).to_broadcast([64, 4, 64]),
                  ALU.mult, r=[pcb, cmask], w=[c4])
            for g in range(4):
                wt_ = WT[p * 4 + g]
                kb.tt("dve", wt_[:].rearrange("s (h l) -> s h l", l=64), LT[g][:].rearrange("s (h l) -> s h l", l=64),
                      c4[:, g, :].unsqueeze(1).to_broadcast([64, 8, 64]), ALU.mult, r=[LT[g], c4], w=[wt_])

        def stage_b(c):
            tg, ci = divmod(c, NCH)
            i = tg % NB
            p = c % 2
            bb, ct = B64[i], CTt[i]
            ys = yst[c % 2]
            pys, pyos, pSs = [], [], []
            for g in range(4):
                wt_ = WT[p * 4 + g]
                py = npb()
                for h in range(8):
                    hg = 8 * g + h
                    kb.mm(py[0:64, h * 64:(h + 1) * 64], wt_[0:64, h * 64:(h + 1) * 64], xdt[p][0:64, hg * 64:(hg + 1) * 64],
                          r=[wt_, xdt[p]], w=[py])
                pyo = npb()
                kb.mm(pyo[0:64, :], ct[:, g, ci * 64:(ci + 1) * 64], S16[g][:, :], r=[ct, S16[g]], w=[pyo])
                pS = npb()
                kb.mm(pS[:, :], bb[0:64, ci, g * 128:(g + 1) * 128], xdte[p][0:64, g * 512:(g + 1) * 512], r=[bb, xdte[p]], w=[pS])
                st_ = stmp[g % 2]
                kb.tt("dve", st_[:].rearrange("n (h p) -> n h p", p=64), S32[g][:].rearrange("n (h p) -> n h p", p=64),
                      cdec[p][:, 8 * g:8 * g + 8].unsqueeze(2).to_broadcast([128, 8, 64]), ALU.mult, r=[S32[g], cdec[p]], w=[st_])
                kb.tt("dve", S32[g][:], st_[:], pS[:, :], ALU.add, r=[st_, pS], w=[S32[g]])
                kb.cp("act", S16[g][:], S32[g][:], r=[S32[g]], w=[S16[g]])
                yo_ = yo[g % 2]
                kb.tt("dve", yo_[:].rearrange("l (h p) -> l h p", p=64), pyo[0:64, :].rearrange("l (h p) -> l h p", p=64),
                      dfs[p][:, 8 * g:8 * g + 8].unsqueeze(2).to_broadcast([64, 8, 64]), ALU.mult, r=[pyo, dfs[p]], w=[yo_])
                kb.tt("dve", yo_[:], yo_[:], py[0:64, :], ALU.add, r=[yo_, py], w=[yo_])
                kb.tt("pool", ys[:, g * 512:(g + 1) * 512], yo_[:], xD[p][:, g * 512:(g + 1) * 512], ALU.add, r=[yo_, xD[p]], w=[ys])
            kb.st(X.y_tok[c * 64:(c + 1) * 64, :], ys[:], ys)

        stage_a(0)
        for c in range(NCK):
            if c + 1 < NCK:
                stage_a(c + 1)
            stage_b(c)
        P.barrier()


def phase_ssm_out(X):
    kb, P, nc = X.kb, X.P, X.nc
    with ExitStack() as es:
        Wo = kb.sb(es, "so_wo", [128, 16, 1024], BF16)
        with ExitStack() as es2:
            load_cast(X, es2, "sowo", X.ssm_w_out, 2048, 1024, Wo, chunk=256)
            P.barrier()
        nwb = kb.sb(es, "so_nwb", [128, 2048], F32)
        kb.ld(nwb[:], X.gate_norm_rep, nwb)
        yt = [kb.sb(es, "so_y%d" % i, [128, 2048], F32) for i in range(3)]
        zt = [kb.sb(es, "so_z%d" % i, [128, 2048], F32) for i in range(3)]
        junk = kb.sb(es, "so_junk", [128, 4, 512], BF16)
        sss = [kb.sb(es, "so_ss%d" % i, [128, 4], F32) for i in range(2)]
        yns = [kb.sb(es, "so_yn%d" % i, [128, 2048], F32) for i in range(2)]
        yTs = [kb.sb(es, "so_yT%d" % i, [128, 16, TT], BF16) for i in range(2)]
        xt = [kb.sb(es, "so_xt%d" % i, [128, 8, TT], F32) for i in range(2)]
        xo = [kb.sb(es, "so_xo%d" % i, [128, TT], F32) for i in range(3)]
        xsrc = X.x2T.rearrange("(k p) t -> p k t", p=128)
        nn = [0]

        def prep_sub(si):
            tt_, sub = divmod(si, 4)
            if sub == 0:
                x = xt[tt_ % 2]
                for k in range(8):
                    kb.ld(x[:, k, :], xsrc[:, k, tt_ * TT:(tt_ + 1) * TT], x)
            s0 = si * 128
            y = yt[si % 3]; z = zt[si % 3]; ss = sss[si % 2]; yn = yns[si % 2]
            kb.ld(y[:], X.y_tok[s0:s0 + 128, :], y)
            kb.ld(z[:], X.z_tok[s0:s0 + 128, :], z)
            kb.act(z[:], z[:], AF.Silu, r=[z], w=[z])
            kb.tt("dve", y[:], y[:], z[:], ALU.mult, r=[y, z], w=[y])
            for g in range(4):
                kb.act(junk[:, g, :], y[:, g * 512:(g + 1) * 512], AF.Square, accum=ss[:, g:g + 1], r=[y], w=[ss])
            kb.act(ss[:], ss[:], AF.Ln, bias=X.epsc[:, 0:1], scale=1.0 / 512.0, r=[ss, X.epsc], w=[ss])
            kb.act(ss[:], ss[:], AF.Exp, scale=-0.5, r=[ss], w=[ss])
            for g in range(4):
                kb.stt("dve", yn[:, g * 512:(g + 1) * 512], y[:, g * 512:(g + 1) * 512], ss[:, g:g + 1],
                       nwb[:, g * 512:(g + 1) * 512], ALU.mult, ALU.mult, r=[y, ss, nwb], w=[yn])

        def tr_sub(si):
            tt_, sub = divmod(si, 4)
            yn = yns[si % 2]
            yT = yTs[tt_ % 2]
            for q4 in range(4):
                n = nn[0]; nn[0] += 1
                ps = X.pb[n % 8]
                for i in range(4):
                    kc = q4 * 4 + i
                    kb.tr(ps[:, i * 128:(i + 1) * 128], yn[:, kc * 128:(kc + 1) * 128], X.ident_f[:, :], r=[yn, X.ident_f], w=[ps])
                kb.cp("act" if q4 % 2 else "dve", yT[:, q4 * 4:(q4 + 1) * 4, sub * 128:(sub + 1) * 128],
                      ps[:, :].rearrange("p (a b) -> p a b", a=4), r=[ps], w=[yT])

        def outproj(tt_):
            t0 = tt_ * TT
            x = xt[tt_ % 2]
            yT = yTs[tt_ % 2]
            for dc in range(8):
                n = nn[0]; nn[0] += 1
                ps = X.pb[n % 8]
                o = xo[n % 3]
                for kc in range(16):
                    kb.mm(ps[:, :], Wo[:, kc, dc * 128:(dc + 1) * 128], yT[:, kc, :], start=(kc == 0), stop=(kc == 15), r=[Wo, yT], w=[ps])
                kb.stt("dve", o[:], ps[:, :], X.modt[:, 2, 16 + dc:17 + dc], x[:, dc, :], ALU.mult, ALU.add, r=[ps, X.modt, x], w=[o])
                kb.st(X.x3T[dc * 128:(dc + 1) * 128, t0:t0 + TT], o[:], o)

        NS = T // 128
        prep_sub(0)
        for si in range(NS):
            if si + 1 < NS:
                prep_sub(si + 1)
            tr_sub(si)
            if si % 4 == 3:
                outproj(si // 4)
        P.barrier()


def phase_moe(X):
    kb, P, nc = X.kb, X.P, X.nc
    NE, FE = 8, 3584
    FS = 256
    NFS = FE // FS
    HALF = 2048
    with ExitStack() as es:
        gate_all = kb.sb(es, "e_gate", [128, 32, 8], F32)
        gm_bc = kb.sb(es, "e_gmbc", [128, 1024], F32)
        fw_bc = kb.sb(es, "e_fwbc", [128, 1024], F32)
        kb.ld(fw_bc[:], X.final_w_rep, fw_bc)
        with ExitStack() as es2:
            dg = kb.sb(es2, "e_dg", [128, 128], F32)
            onesf = kb.sb(es2, "e_onesf", [128, 128], F32)
            kb.memset("dve", onesf[:], 1.0, w=[onesf])
            for c in range(8):
                ps = X.pb[c // 4]
                kb.ts("dve", dg[:], X.ident_f[:, :], X.modt[:, 3, 16 + c:17 + c], None, ALU.mult, r=[X.ident_f, X.modt], w=[dg])
                kb.mm(ps[:, (c % 4) * 128:(c % 4 + 1) * 128], onesf[:, :], dg[:, :], r=[onesf, dg], w=[ps])
                if c % 4 == 3:
                    kb.cp("dve", gm_bc[:, (c // 4) * 512:(c // 4 + 1) * 512], ps[:, :], r=[ps], w=[gm_bc])
            P.barrier()
        with ExitStack() as es2:
            r32 = kb.sb(es2, "e_r32", [128, 8, 8], F32)
            kb.ld(r32[:], X.router.rearrange("(k p) e -> p k e", p=128), r32)
            xt = [kb.sb(es2, "e_xt%d" % i, [128, 8, TT], F32) for i in range(2)]
            sq = kb.sb(es2, "e_sq", [128, 8, TT], BF16)
            hF = kb.sb(es2, "e_hF", [128, 8, TT], F32)
            hT = [kb.sb(es2, "e_hT%d" % i, [128, 8, TT], BF16) for i in range(2)]
            h32 = kb.sb(es2, "e_h32", [128, 8, TT], F32)
            rstd = kb.sb(es2, "e_rstd", [128, TT], F32)
            lg_all = kb.sb(es2, "e_lgall", [128, 32, 8], F32)
            v1 = kb.sb(es2, "e_v1", [128, 32], F32)
            v2 = kb.sb(es2, "e_v2", [128, 32], F32)
            eqm = kb.sb(es2, "e_eqm", [128, 32, 8], F32)
            exa = kb.sb(es2, "e_exa", [128, 32, 8], F32)
            t8 = kb.sb(es2, "e_t8", [128, 8], F32)
            nv1 = kb.sb(es2, "e_nv1", [128, 1], F32)
            msk = kb.sb(es2, "e_msk", [128, 8], F32)
            ex = kb.sb(es2, "e_ex", [128, 8], F32)
            den = kb.sb(es2, "e_den", [128, 1], F32)
            xsrc = X.x3T.rearrange("(k p) t -> p k t", p=128)
            hdst = X.hmT.rearrange("(k p) t -> p k t", p=128)
            for tt_ in range(NT):
                t0 = tt_ * TT
                x = xt[tt_ % 2]
                h_ = hT[tt_ % 2]
                for k in range(8):
                    kb.ld(x[:, k, :], xsrc[:, k, t0:t0 + TT], x)
                norm_modulate(X, x, h_, 3, sq, X.pb[7], rstd, hF, hT32=h32)
                for k in range(8):
                    kb.st(hdst[:, k, t0:t0 + TT], h_[:, k, :], h_)
                for sub in range(4):
                    si = tt_ * 4 + sub
                    ps = X.pb[sub % 4]
                    for k in range(8):
                        kb.mm(ps[:, 0:8], h32[:, k, sub * 128:(sub + 1) * 128], r32[:, k, :], start=(k == 0), stop=(k == 7),
                              r=[h32, r32], w=[ps])
                    kb.cp("dve", lg_all[:, si, :], ps[:, 0:8], r=[ps], w=[lg_all])
            def b8(t):
                return t[:, :].unsqueeze(2).to_broadcast([128, 32, 8])
            P.op("dve", (lambda o, i: (lambda e: e.tensor_reduce(o, i, AX.X, ALU.max)))(v1[:], lg_all[:]), [lg_all], [v1])
            kb.tt("dve", eqm[:], lg_all[:], b8(v1), ALU.is_equal, r=[lg_all, v1], w=[eqm])
            kb.stt("dve", eqm[:], eqm[:], -1e30, lg_all[:], ALU.mult, ALU.add, r=[eqm, lg_all], w=[eqm])
            P.op("dve", (lambda o, i: (lambda e: e.tensor_reduce(o, i, AX.X, ALU.max)))(v2[:], eqm[:]), [eqm], [v2])
            kb.tt("dve", eqm[:], lg_all[:], b8(v2), ALU.is_ge, r=[lg_all, v2], w=[eqm])
            kb.tt("dve", exa[:], lg_all[:], b8(v1), ALU.subtract, r=[lg_all, v1], w=[exa])
            kb.act(exa[:], exa[:], AF.Exp, r=[exa], w=[exa])
            kb.tt("dve", exa[:], exa[:], eqm[:], ALU.mult, r=[exa, eqm], w=[exa])
            P.op("dve", (lambda o, i: (lambda e: e.tensor_reduce(o, i, AX.X, ALU.add)))(v2[:], exa[:]), [exa], [v2])
            kb.recip(v2[:], v2[:], r=[v2], w=[v2])
            kb.tt("dve", gate_all[:], exa[:], b8(v2), ALU.mult, r=[exa, v2], w=[gate_all])
            P.barrier()
        if X.dbg_stop == "gate":
            dgt = nc.dram_tensor("gate_d", [128, 256], F32, kind="ExternalOutput").ap()
            kb.st(dgt, gate_all[:].rearrange("p a b -> p (a b)"), gate_all)
            P.barrier()
            return
        acc = kb.sb(es, "e_acc", [128, 16, 1024], F32)
        hh = kb.sb(es, "e_hh", [128, 8, HALF], BF16)
        sgu = [kb.sb(es, "e_sgu%d" % i, [128, 8, FS], F32) for i in range(2)]
        swd = [kb.sb(es, "e_swd%d" % i, [128, 2, 1024], F32) for i in range(2)]
        wgu = [kb.sb(es, "e_wgu%d" % i, [128, 8, 2 * FS], BF16) for i in range(2)]
        wd = [kb.sb(es, "e_wd%d" % i, [128, 2, 1024], BF16) for i in range(2)]
        sg = [kb.sb(es, "e_sg%d" % i, [128, TT], F32) for i in range(2)]
        a16 = [kb.sb(es, "e_a16_%d" % i, [128, TT], BF16) for i in range(4)]
        x3s = kb.sb(es, "e_x3s", [128, 8, 128], F32)
        ytmp = kb.sb(es, "e_ytmp", [128, 1024], F32)
        junk = kb.sb(es, "e_junk", [128, 1024], BF16)
        ssq = kb.sb(es, "e_ssq", [128, 1], F32)
        ost = [kb.sb(es, "e_ost%d" % i, [128, 1024], F32) for i in range(2)]
        hsrc = X.hmT.rearrange("(k p) t -> p k t", p=128)
        x3src = X.x3T.rearrange("(k p) t -> p k t", p=128)
        n = 0
        wi = 0
        for half in range(2):
            h0 = half * HALF
            for k in range(8):
                kb.ld(hh[:, k, :], hsrc[:, k, h0:h0 + HALF], hh)
            for e in range(NE):
                for fs in range(NFS):
                    wg = wgu[wi % 2]; wdn = wd[wi % 2]
                    for part in range(2):
                        sst = sgu[part]
                        c0 = part * FE + fs * FS
                        kb.ld(sst[:], X.moe_w_gu[e].rearrange("(k p) m -> p k m", p=128)[:, :, c0:c0 + FS], sst)
                        kb.cp("pool", wg[:, :, part * FS:(part + 1) * FS], sst[:], r=[sst], w=[wg])
                    sd = swd[wi % 2]
                    kb.ld(sd[:], X.moe_w_down[e][fs * FS:(fs + 1) * FS, :].rearrange("(c p) d -> p c d", p=128), sd)
                    kb.cp("pool", wdn[:], sd[:], r=[sd], w=[wdn])
                    wi += 1
                    first = (e == 0 and fs == 0)
                    for tile in range(HALF // TT):
                        tl0 = tile * TT
                        aa = []
                        for c in range(2):
                            pg = X.pb[n % 8]; n += 1
                            pu = X.pb[n % 8]; n += 1
                            for k in range(8):
                                kb.mm(pg[:, :], wg[:, k, c * 128:(c + 1) * 128], hh[:, k, tl0:tl0 + TT], start=(k == 0), stop=(k == 7),
                                      r=[wg, hh], w=[pg])
                            for k in range(8):
                                kb.mm(pu[:, :], wg[:, k, FS + c * 128:FS + (c + 1) * 128], hh[:, k, tl0:tl0 + TT], start=(k == 0), stop=(k == 7),
                                      r=[wg, hh], w=[pu])
                            s_ = sg[c]
                            a_ = a16[(tile % 2) * 2 + c]
                            kb.act(s_[:], pg[:, :], AF.Silu, r=[pg], w=[s_])
                            kb.tt("dve", a_[:], s_[:], pu[:, :], ALU.mult, r=[s_, pu], w=[a_])
                            aa.append(a_)
                        for sub in range(4):
                            sl = tile * 4 + sub
                            sgl = half * 16 + sl
                            for dh in range(2):
                                po = X.pb[n % 8]; n += 1
                                for c in range(2):
                                    kb.mm(po[:, :], aa[c][:, sub * 128:(sub + 1) * 128], wdn[:, c, dh * 512:(dh + 1) * 512],
                                          start=(c == 0), stop=(c == 1), r=[aa[c], wdn], w=[po])
                                if first:
                                    kb.ts("dve", acc[:, sl, dh * 512:(dh + 1) * 512], po[:, :], gate_all[:, sgl, e:e + 1], None, ALU.mult,
                                          r=[po, gate_all], w=[acc])
                                else:
                                    kb.stt("dve", acc[:, sl, dh * 512:(dh + 1) * 512], po[:, :], gate_all[:, sgl, e:e + 1],
                                           acc[:, sl, dh * 512:(dh + 1) * 512], ALU.mult, ALU.add, r=[po, gate_all, acc], w=[acc])
            for sl in range(16):
                s0 = h0 + sl * 128
                kb.ld(x3s[:], x3src[:, :, s0:s0 + 128], x3s)
                px = [X.pb[n % 8], X.pb[(n + 1) % 8]]; n += 2
                for c in range(8):
                    kb.tr(px[c // 4][:, (c % 4) * 128:(c % 4 + 1) * 128], x3s[:, c, :], X.ident_f[:, :], r=[x3s, X.ident_f], w=[px[c // 4]])
                kb.tt("dve", ytmp[:], acc[:, sl, :], gm_bc[:], ALU.mult, r=[acc, gm_bc], w=[ytmp])
                for dh in range(2):
                    kb.tt("dve", ytmp[:, dh * 512:(dh + 1) * 512], ytmp[:, dh * 512:(dh + 1) * 512], px[dh][:, :], ALU.add,
                          r=[ytmp, px[dh]], w=[ytmp])
                kb.act(junk[:], ytmp[:], AF.Square, accum=ssq[:, 0:1], r=[ytmp], w=[junk, ssq])
                kb.act(ssq[:], ssq[:], AF.Ln, bias=X.epsc[:, 0:1], scale=1.0 / D, r=[ssq, X.epsc], w=[ssq])
                kb.act(ssq[:], ssq[:], AF.Exp, scale=-0.5, r=[ssq], w=[ssq])
                o = ost[sl % 2]
                kb.stt("dve", o[:], ytmp[:], ssq[:, 0:1], fw_bc[:], ALU.mult, ALU.mult, r=[ytmp, ssq, fw_bc], w=[o])
                kb.st(X.out[s0:s0 + 128, :], o[:], o)
        P.barrier()


def col_layout(v, nchunk):
    return np.ascontiguousarray(np.asarray(v, np.float32).reshape(nchunk, 128).T)


def make_in_maps(inputs, cores):
    consts = make_consts()
    mods = [("hyb_mod_w", "hyb_mod_b", "hyb_norm"), ("dense_mod_w", "dense_mod_b", "dense_norm"),
            ("ssm_mod_w", "ssm_mod_b", "ssm_norm"), ("moe_mod_w", "moe_mod_b", "moe_norm")]
    shared = {}
    for k, v in consts.items():
        shared["c_" + k] = v
    for j, (w, b, n) in enumerate(mods):
        shared["mod_w%d" % j] = np.ascontiguousarray(inputs[w][0])
        shared["mod_b%d" % j] = col_layout(inputs[b][0], 24)
        shared["norm_w%d" % j] = col_layout(inputs[n][0], 8)
    shared["hyb_w_in"] = np.ascontiguousarray(inputs["hyb_w_in"][0])
    shared["gk_up"] = np.ascontiguousarray(inputs["gla_gk_up"][0])
    shared["gk_bias_col"] = col_layout(inputs["gla_gk_bias"][0], 2)
    shared["gk_bias_rep"] = np.ascontiguousarray(np.broadcast_to(inputs["gla_gk_bias"][0][None, :], (64, 256)))
    shared["gla_norm"] = np.ascontiguousarray(inputs["gla_out_norm"][0][:, None])
    shared["hyb_w_out"] = np.ascontiguousarray(inputs["hyb_w_out"][0])
    shared["dense_w_gu"] = np.ascontiguousarray(inputs["dense_w_gu"][0])
    shared["dense_w_down"] = np.ascontiguousarray(inputs["dense_w_down"][0])
    shared["ssm_w_in"] = np.ascontiguousarray(inputs["ssm_w_in"][0])
    shared["ssm_w_out"] = np.ascontiguousarray(inputs["ssm_w_out"][0])
    cw = inputs["ssm_conv_w"][0]
    shared["conv_w_l"] = np.ascontiguousarray(cw.reshape(4, 24, 128).transpose(2, 1, 0))
    shared["conv_b_l"] = col_layout(inputs["ssm_conv_b"][0], 24)
    shared["dt_bias_rep"] = np.ascontiguousarray(np.broadcast_to(inputs["ssm_dt_bias"][0][None, :], (128, 32)))
    shared["a_log_rep"] = np.ascontiguousarray(np.broadcast_to(inputs["ssm_a_log"][0][None, :], (64, 32)))
    shared["d_rep"] = np.ascontiguousarray(np.broadcast_to(np.repeat(inputs["ssm_d"][0], 64)[None, :], (64, 2048)))
    shared["gate_norm_rep"] = np.ascontiguousarray(np.broadcast_to(inputs["ssm_gate_norm"][0][None, :], (128, 2048)))
    shared["router"] = np.ascontiguousarray(inputs["moe_router"][0])
    shared["moe_w_gu"] = np.ascontiguousarray(inputs["moe_w_gu"][0])
    shared["moe_w_down"] = np.ascontiguousarray(inputs["moe_w_down"][0])
    shared["final_w_rep"] = np.ascontiguousarray(np.broadcast_to(inputs["final_norm"][None, :], (128, D)))
    shared["cmp_peT"] = np.ascontiguousarray(inputs["nsa_cmp_pe"][0].transpose(0, 2, 1))
    shared["cmp_w1"] = np.ascontiguousarray(inputs["nsa_cmp_w1"][0])
    shared["cmp_w2"] = np.ascontiguousarray(inputs["nsa_cmp_w2"][0])
    maps = []
    for b in cores:
        m = dict(shared)
        m["xT"] = np.ascontiguousarray(inputs["x"][b].T)
        m["cvec"] = col_layout(inputs["c"][b], 8)
        maps.append(m)
    return maps


ALL_PHASES = ("adaln", "l0proj", "gla", "nsa", "l0out", "dense", "ssm_in", "ssm_conv", "ssm_scan", "ssm_out", "moe")
_CACHE = {}


def kernel(**inputs):
    inputs = {k: np.asarray(v) for k, v in inputs.items()}
    n = 8
    if "prog" not in _CACHE:
        _CACHE["prog"] = build_program(set(ALL_PHASES))
    nc, ext_in = _CACHE["prog"]
    maps = make_in_maps(inputs, list(range(n)))
    in_maps = [{k: v for k, v in m.items() if k in ext_in} for m in maps]
    res = run_bass_kernel_spmd(nc, in_maps, core_ids=list(range(n)))
    out = np.stack([np.asarray(res.results[i]["out"], dtype=np.float32) for i in range(n)], axis=0)
    return out
```
